# Optimizing a Trainium2 kernel written in Bass

```python
import jax, jax.numpy as jnp
from jax import lax
import numpy as np

D_MODEL = 1024
BATCH = 16
SEQ = 2048
DEPTH = 1

HEAD_DIM = 64
MIX_WIDTH = D_MODEL
RWKV_WIDTH = MIX_WIDTH // 2
MOBA_WIDTH = MIX_WIDTH - RWKV_WIDTH
RWKV_HEADS = RWKV_WIDTH // HEAD_DIM
MOBA_HEADS = MOBA_WIDTH // HEAD_DIM
DECAY_LORA = max(32, int(round(1.8 * D_MODEL ** 0.5 / 32)) * 32)
AAA_LORA = max(32, int(round(1.8 * D_MODEL ** 0.5 / 32)) * 32)
GATE_LORA = max(32, int(round(0.6 * D_MODEL ** 0.8 / 32)) * 32)
RWKV_LN_EPS = 64e-5
RWKV_PROJ = 3 * RWKV_WIDTH + DECAY_LORA + AAA_LORA + GATE_LORA
MOBA_PROJ = 3 * MOBA_WIDTH
IN_COLS = RWKV_PROJ + MOBA_PROJ
MOBA_BLOCK = 256
MOBA_TOPK = 3
MOBA_QUERY_CHUNK = 16
N_EXPERTS = 32
TOP_K = 4
D_FF = D_MODEL
SWIGLU_LIMIT = 7.0
SWIGLU_ALPHA = 1.702
EXPERT_ROWS = 128
NORM_EPS = 1e-6

kernel_name = 'hybrid_rwkv7_moba_moe_adaln_block'


def rms_norm(x, g):
    xf = x.astype(jnp.float32)
    y = xf * lax.rsqrt(jnp.mean(xf * xf, axis=-1, keepdims=True) + NORM_EPS)
    return (y * g.astype(jnp.float32)).astype(x.dtype)


def token_shift(p):
    return jnp.pad(p, ((0, 0), (1, 0), (0, 0)))[:, :-1]


def rwkv7_time_mix(p, mu, w0, w_up, a0, a_up, g_up, k_k, k_a, r_k, ln_g, ln_b):
    B, S, _ = p.shape
    H, N, W = RWKV_HEADS, HEAD_DIM, RWKV_WIDTH
    f32 = jnp.float32
    p = p + (token_shift(p) - p) * mu
    r, k, v, xw, xa, xg = jnp.split(
        p, [W, 2 * W, 3 * W, 3 * W + DECAY_LORA, 3 * W + DECAY_LORA + AAA_LORA], axis=-1)
    w_log = -jax.nn.softplus(-(w0 + jnp.tanh(xw) @ w_up)) - 0.5
    decay = jnp.exp(-jnp.exp(w_log.astype(f32)))
    a = jax.nn.sigmoid(a0 + xa @ a_up)
    g = jax.nn.sigmoid(xg) @ g_up
    heads = lambda t: t.reshape(B, S, H, N).astype(f32)
    kk = heads(k * k_k)
    kk = kk / jnp.maximum(jnp.linalg.norm(kk, axis=-1, keepdims=True), 1e-12)
    k = k * (1.0 + (a - 1.0) * k_a)
    r_h, k_h, v_h, w_h, a_h = heads(r), heads(k), heads(v), heads(decay), heads(a)

    def step(state, inp):
        r_t, w_t, k_t, v_t, kk_t, a_t = inp
        sa = jnp.einsum('bhij,bhj->bhi', state, -kk_t)
        state = (state * w_t[:, :, None, :]
                 + sa[..., None] * (kk_t * a_t)[:, :, None, :]
                 + v_t[..., None] * k_t[:, :, None, :])
        y_t = jnp.einsum('bhij,bhj->bhi', state, r_t)
        return state, y_t

    sf = lambda t: jnp.moveaxis(t, 1, 0)
    state0 = jnp.zeros((B, H, N, N), f32)
    _, y = lax.scan(step, state0, (sf(r_h), sf(w_h), sf(k_h), sf(v_h), sf(kk), sf(a_h)))
    y = jnp.moveaxis(y, 0, 1)
    mean = jnp.mean(y, axis=-1, keepdims=True)
    var = jnp.mean(jnp.square(y - mean), axis=-1, keepdims=True)
    y = ((y - mean) * lax.rsqrt(var + RWKV_LN_EPS)).reshape(B, S, W) * ln_g + ln_b
    bonus = jnp.sum(r_h * k_h * r_k, axis=-1, keepdims=True) * v_h
    y = (y + bonus.reshape(B, S, W)) * g
    return y.astype(p.dtype)


def moba_attention(p, q_norm_g, k_norm_g):
    B, S, _ = p.shape
    H, N, BLK, QC = MOBA_HEADS, HEAD_DIM, MOBA_BLOCK, MOBA_QUERY_CHUNK
    f32 = jnp.float32
    to_heads = lambda t: t.reshape(B, S, H, N).transpose(0, 2, 1, 3)
    q, k, v = [to_heads(t) for t in jnp.split(p, 3, axis=-1)]
    q = rms_norm(q, q_norm_g)
    k = rms_norm(k, k_norm_g)
    NB = -(-S // BLK)
    n_sel = min(MOBA_TOPK, NB)
    pad = NB * BLK - S
    kb = jnp.pad(k, ((0, 0), (0, 0), (0, pad), (0, 0))).reshape(B, H, NB, BLK, N)
    vb = jnp.pad(v, ((0, 0), (0, 0), (0, pad), (0, 0))).reshape(B, H, NB, BLK, N)
    kmean = jnp.mean(kb.astype(f32), axis=3)
    slopes = jnp.exp2(-8.0 * (jnp.arange(H, dtype=f32) + 1.0) / H)
    scale = HEAD_DIM ** -0.5
    bi = jnp.arange(B)[:, None, None, None]
    hi = jnp.arange(H)[None, :, None, None]
    n_chunks = S // QC
    q_chunks = jnp.moveaxis(q.reshape(B, H, n_chunks, QC, N), 2, 0)

    def attend_chunk(args):
        qi, ci = args
        t0 = ci * QC
        j = t0 // BLK
        tq = t0 + jnp.arange(QC)
        gate = jnp.einsum('bhqd,bhnd->bhqn', qi.astype(f32), kmean)
        gate = jnp.where(jnp.arange(NB) < j, gate, -jnp.inf)
        _, sel = lax.top_k(gate, n_sel)
        ksel = kb[bi, hi, sel]
        vsel = vb[bi, hi, sel]
        kpos_sel = sel[..., None] * BLK + jnp.arange(BLK)
        s_sel = (jnp.einsum('bhqd,bhqmkd->bhqmk', qi, ksel).astype(f32) * scale
                 - slopes[:, None, None, None] * (tq[:, None, None] - kpos_sel))
        s_sel = jnp.where((jnp.arange(n_sel) < j)[:, None], s_sel, -jnp.inf)
        k_own = lax.dynamic_index_in_dim(kb, j, axis=2, keepdims=False)
        v_own = lax.dynamic_index_in_dim(vb, j, axis=2, keepdims=False)
        kpos_own = j * BLK + jnp.arange(BLK)
        s_own = (jnp.einsum('bhqd,bhkd->bhqk', qi, k_own).astype(f32) * scale
                 - slopes[:, None, None] * (tq[:, None] - kpos_own[None, :]))
        s_own = jnp.where(kpos_own[None, :] <= tq[:, None], s_own, -jnp.inf)
        s = jnp.concatenate([s_sel.reshape(B, H, QC, n_sel * BLK), s_own], axis=-1)
        prob = jax.nn.softmax(s, axis=-1)
        p_sel = prob[..., :n_sel * BLK].reshape(B, H, QC, n_sel, BLK)
        p_own = prob[..., n_sel * BLK:]
        out = (jnp.einsum('bhqmk,bhqmkd->bhqd', p_sel, vsel)
               + jnp.einsum('bhqk,bhkd->bhqd', p_own, v_own))
        return out.astype(qi.dtype)

    out = lax.map(attend_chunk, (q_chunks, jnp.arange(n_chunks, dtype=jnp.int32)))
    out = jnp.moveaxis(out, 0, 2).reshape(B, H, S, N)
    return out.transpose(0, 2, 1, 3).reshape(B, S, MOBA_WIDTH)


def moe_ffn(h, w_router, b_router, w_gate_up, b_gate_up, w_down, b_down):
    T, D = h.shape
    logits = (h @ w_router + b_router).astype(jnp.float32)
    top_val, top_idx = lax.top_k(logits, TOP_K)
    weights = jax.nn.softmax(top_val, axis=-1)
    M = T * TOP_K
    slot_expert = top_idx.reshape(M)
    slot_token = jnp.arange(M, dtype=jnp.int32) // TOP_K
    slot_weight = weights.reshape(M)
    order = jnp.argsort(slot_expert)
    e_sorted = slot_expert[order]
    counts = jnp.bincount(slot_expert, length=N_EXPERTS)
    padded = (counts + EXPERT_ROWS - 1) // EXPERT_ROWS * EXPERT_ROWS
    pad_end = jnp.cumsum(padded)
    pad_start = pad_end - padded
    start = jnp.cumsum(counts) - counts
    dest = pad_start[e_sorted] + jnp.arange(M) - start[e_sorted]
    P = M + N_EXPERTS * EXPERT_ROWS
    n_blocks = P // EXPERT_ROWS
    buf_token = jnp.zeros((P,), jnp.int32).at[dest].set(slot_token[order])
    buf_weight = jnp.zeros((P,), jnp.float32).at[dest].set(slot_weight[order])
    blk_expert = jnp.minimum(
        jnp.searchsorted(pad_end, jnp.arange(n_blocks) * EXPERT_ROWS, side='right'), N_EXPERTS - 1)

    def expert_block(args):
        tok, e = args
        xb = h[tok]
        gu = xb @ w_gate_up[e] + b_gate_up[e]
        gate = jnp.minimum(gu[:, :D_FF], SWIGLU_LIMIT)
        up = jnp.clip(gu[:, D_FF:], -SWIGLU_LIMIT, SWIGLU_LIMIT)
        act = (up + 1.0) * gate * jax.nn.sigmoid(SWIGLU_ALPHA * gate)
        return act @ w_down[e] + b_down[e]

    y = lax.map(expert_block, (buf_token.reshape(n_blocks, EXPERT_ROWS), blk_expert)).reshape(P, D)
    return jax.ops.segment_sum(y * buf_weight[:, None], buf_token, num_segments=T)


def hybrid_layer(x, c, w_ada, b_ada, norm1_g, w_in, rwkv_mu, rwkv_w0, rwkv_w_up, rwkv_a0,
                 rwkv_a_up, rwkv_g_up, rwkv_k_k, rwkv_k_a, rwkv_r_k, rwkv_ln_g, rwkv_ln_b,
                 q_norm_g, k_norm_g, w_out, norm2_g, w_router, b_router, w_gate_up,
                 b_gate_up, w_down, b_down):
    B, S, D = x.shape
    mods = jax.nn.silu(c) @ w_ada + b_ada
    shift1, scale1, gate1, shift2, scale2, gate2 = jnp.split(mods[:, None, :], 6, axis=-1)
    h = rms_norm(x, norm1_g) * (1.0 + scale1) + shift1
    proj = h @ w_in
    y_rwkv = rwkv7_time_mix(proj[..., :RWKV_PROJ], rwkv_mu, rwkv_w0, rwkv_w_up, rwkv_a0,
                            rwkv_a_up, rwkv_g_up, rwkv_k_k, rwkv_k_a, rwkv_r_k,
                            rwkv_ln_g, rwkv_ln_b)
    y_moba = moba_attention(proj[..., RWKV_PROJ:], q_norm_g, k_norm_g)
    x = x + gate1 * (jnp.concatenate([y_rwkv, y_moba], axis=-1) @ w_out)
    h = rms_norm(x, norm2_g) * (1.0 + scale2) + shift2
    y = moe_ffn(h.reshape(B * S, D), w_router, b_router, w_gate_up, b_gate_up, w_down, b_down)
    return x + gate2 * y.reshape(B, S, D).astype(x.dtype)


def setup_inputs(seed: int = 0) -> dict:
    key = jax.random.key(seed)
    ks = jax.random.split(key, 27)
    nrm = lambda k, shape, s: jax.random.normal(k, shape, jnp.float32) * s
    L = DEPTH
    return {
        'x': nrm(ks[0], (BATCH, SEQ, D_MODEL), 1.0),
        'c': nrm(ks[1], (BATCH, D_MODEL), 1.0),
        'w_ada': nrm(ks[2], (L, D_MODEL, 6 * D_MODEL), 0.5 * D_MODEL ** -0.5),
        'b_ada': nrm(ks[3], (L, 6 * D_MODEL), 0.02),
        'norm1_g': 1.0 + nrm(ks[4], (L, D_MODEL), 0.05),
        'w_in': nrm(ks[5], (L, D_MODEL, IN_COLS), D_MODEL ** -0.5),
        'rwkv_mu': jax.random.uniform(ks[6], (L, RWKV_PROJ), jnp.float32),
        'rwkv_w0': jax.random.uniform(ks[7], (L, RWKV_WIDTH), jnp.float32, minval=-6.0, maxval=0.0),
        'rwkv_w_up': nrm(ks[8], (L, DECAY_LORA, RWKV_WIDTH), 0.3 * DECAY_LORA ** -0.5),
        'rwkv_a0': nrm(ks[9], (L, RWKV_WIDTH), 0.5),
        'rwkv_a_up': nrm(ks[10], (L, AAA_LORA, RWKV_WIDTH), 0.5 * AAA_LORA ** -0.5),
        'rwkv_g_up': nrm(ks[11], (L, GATE_LORA, RWKV_WIDTH), GATE_LORA ** -0.5),
        'rwkv_k_k': 0.85 + nrm(ks[12], (L, RWKV_WIDTH), 0.05),
        'rwkv_k_a': 1.0 + nrm(ks[13], (L, RWKV_WIDTH), 0.05),
        'rwkv_r_k': nrm(ks[14], (L, RWKV_HEADS, HEAD_DIM), 0.1),
        'rwkv_ln_g': 1.0 + nrm(ks[15], (L, RWKV_WIDTH), 0.05),
        'rwkv_ln_b': nrm(ks[16], (L, RWKV_WIDTH), 0.02),
        'q_norm_g': 1.0 + nrm(ks[17], (L, HEAD_DIM), 0.05),
        'k_norm_g': 1.0 + nrm(ks[18], (L, HEAD_DIM), 0.05),
        'w_out': nrm(ks[19], (L, MIX_WIDTH, D_MODEL), MIX_WIDTH ** -0.5),
        'norm2_g': 1.0 + nrm(ks[20], (L, D_MODEL), 0.05),
        'w_router': nrm(ks[21], (L, D_MODEL, N_EXPERTS), D_MODEL ** -0.5),
        'b_router': nrm(ks[22], (L, N_EXPERTS), 0.01),
        'w_gate_up': nrm(ks[23], (L, N_EXPERTS, D_MODEL, 2 * D_FF), D_MODEL ** -0.5),
        'b_gate_up': nrm(ks[24], (L, N_EXPERTS, 2 * D_FF), 0.01),
        'w_down': nrm(ks[25], (L, N_EXPERTS, D_FF, D_MODEL), D_FF ** -0.5),
        'b_down': nrm(ks[26], (L, N_EXPERTS, D_MODEL), 0.01),
    }


def reference(x, c, w_ada, b_ada, norm1_g, w_in, rwkv_mu, rwkv_w0, rwkv_w_up, rwkv_a0,
              rwkv_a_up, rwkv_g_up, rwkv_k_k, rwkv_k_a, rwkv_r_k, rwkv_ln_g, rwkv_ln_b,
              q_norm_g, k_norm_g, w_out, norm2_g, w_router, b_router, w_gate_up,
              b_gate_up, w_down, b_down):
    for l in range(DEPTH):
        x = hybrid_layer(x, c, w_ada[l], b_ada[l], norm1_g[l], w_in[l], rwkv_mu[l], rwkv_w0[l],
                         rwkv_w_up[l], rwkv_a0[l], rwkv_a_up[l], rwkv_g_up[l], rwkv_k_k[l],
                         rwkv_k_a[l], rwkv_r_k[l], rwkv_ln_g[l], rwkv_ln_b[l], q_norm_g[l],
                         k_norm_g[l], w_out[l], norm2_g[l], w_router[l], b_router[l],
                         w_gate_up[l], b_gate_up[l], w_down[l], b_down[l])
    return x
```

```python
import contextlib
import numpy as np
import concourse.bass as bass
import concourse.mybir as mybir
from concourse.bass_utils import run_bass_kernel_spmd

F32 = mybir.dt.float32
BF16 = mybir.dt.bfloat16
ALU = mybir.AluOpType
AF = mybir.ActivationFunctionType
AX = mybir.AxisListType

NCORES = 8
NB = 2
S = 2048
D = 1024
NT = S // 128
CH = 32
NCH = 128 // CH
KLV = CH.bit_length() - 1
NEXP = 32
CAP = 2048
SPARSE = True
WARM = 0
U32 = mybir.dt.uint32
LAST_NOPS = 0


class Prog:
    ROT = 12000
    NDMA = 24
    SAME_ENGINE_SYNC = True

    def __init__(self, nc, stack):
        self.nc = nc
        self.stack = stack
        self.ops = {e: [] for e in ("pe", "dve", "act", "pool", "sp")}
        self.sems = []
        self.cur_sem = {}
        self.cnt = {}
        self.allsems = {e: [] for e in ("pe", "dve", "act", "pool")}
        for e in ("pe", "dve", "act", "pool"):
            self.cur_sem[e] = self._new_sem(e)
            self.allsems[e].append(self.cur_sem[e])
            self.cnt[e] = 0
        self.dma_sems = [self._new_sem("dma%d" % i) for i in range(self.NDMA)]
        self.dma_cnt = [0] * self.NDMA
        self.dma_rr = 0
        self.dma_rr_sw = 0
        self.last_write = {}
        self.readers = {}
        self.waited = {e: {} for e in self.ops}
        self.nops = 0

    def _new_sem(self, name):
        s = self.stack.enter_context(self.nc.semaphore("s_%s_%d" % (name, len(self.sems))))
        self.sems.append(s)
        return len(self.sems) - 1

    LIMIT = None

    def _add(self, eng, fn, reads, writes, dma):
        if Prog.LIMIT is not None and self.nops >= Prog.LIMIT:
            return None
        waits = []
        deps = []
        for t in reads:
            if t in self.last_write:
                deps.append(self.last_write[t])
        for t in writes:
            if t in self.last_write:
                deps.append(self.last_write[t])
            deps.extend(self.readers.get(t, ()))
        if dma:
            if eng == "pool":
                s = self.dma_rr_sw % 8
                self.dma_rr_sw += 1
            else:
                s = 8 + self.dma_rr % (self.NDMA - 8)
                self.dma_rr += 1
            if self.dma_cnt[s] > 0:
                deps.append((self.dma_sems[s], self.dma_cnt[s] * 16, "dma"))
            self.dma_cnt[s] += 1
            ticket = (self.dma_sems[s], self.dma_cnt[s] * 16, "dma")
            inc = (self.dma_sems[s], 16)
        else:
            if self.cnt[eng] >= self.ROT:
                self.cur_sem[eng] = self._new_sem(eng)
                self.allsems[eng].append(self.cur_sem[eng])
                self.cnt[eng] = 0
            self.cnt[eng] += 1
            ticket = (self.cur_sem[eng], self.cnt[eng], eng)
            inc = (self.cur_sem[eng], 1)
        w = self.waited[eng]
        for (sem, val, src) in deps:
            if src == eng and not dma:
                if eng == "pe" or not self.SAME_ENGINE_SYNC:
                    continue
            if w.get(sem, 0) >= val:
                continue
            w[sem] = val
            waits.append((sem, val))
        self.ops[eng].append((waits, fn, inc))
        self.nops += 1
        for t in reads:
            self.readers.setdefault(t, []).append(ticket)
        for t in writes:
            self.last_write[t] = ticket
            self.readers[t] = []
        return ticket

    @staticmethod
    def _is_psum(t):
        return isinstance(t, str) and (t.startswith(("bk", "pps", "PS")) or t.endswith("pt") or "pp" in t)

    def op(self, eng, fn, reads=(), writes=()):
        reads = list(reads)
        writes = list(writes)
        for t in list(reads):
            if self._is_psum(t):
                reads.remove(t)
                if t not in writes:
                    writes.append(t)
        return self._add(eng, fn, reads, writes, False)

    def dma(self, eng, fn, reads=(), writes=()):
        return self._add(eng, fn, list(reads), list(writes), True)

    def barrier(self):
        allw = []
        for e in ("pe", "dve", "act", "pool"):
            if self.cnt[e] > 0:
                allw.append((self.cur_sem[e], self.cnt[e]))
        for s in range(self.NDMA):
            if self.dma_cnt[s] > 0:
                allw.append((self.dma_sems[s], self.dma_cnt[s] * 16))
        for e in self.ops:
            w = self.waited[e]
            waits = []
            for (sem, val) in allw:
                if w.get(sem, 0) >= val:
                    continue
                w[sem] = val
                waits.append((sem, val))
            if waits:
                self.ops[e].append((waits, None, None))
        self.last_write = {}
        self.readers = {}

    def emit(self):
        nc = self.nc
        sems = self.sems

        def run(engname, eng):
            for (waits, fn, inc) in self.ops[engname]:
                for (s, v) in waits:
                    eng.wait_ge(sems[s], v)
                if fn is None:
                    continue
                ins = fn(eng)
                ins.then_inc(sems[inc[0]], inc[1])

        with nc.Block() as block:
            @block.tensor
            def _(e):
                run("pe", e)

            @block.vector
            def _(e):
                run("dve", e)

            @block.scalar
            def _(e):
                run("act", e)

            @block.gpsimd
            def _(e):
                run("pool", e)

            @block.sync
            def _(e):
                run("sp", e)


def host_consts():
    c = {}
    c["ident"] = np.eye(128, dtype=np.float32)
    t = np.arange(128)
    same = (t[:, None] // CH) == (t[None, :] // CH)
    mS = (same & (t[:, None] < t[None, :])).astype(np.float32)
    mI = (same & (t[:, None] <= t[None, :])).astype(np.float32)
    c["mask_si"] = np.concatenate([mS, mI], axis=1)
    c["mask_l"] = np.ascontiguousarray(mS.T)
    cm = np.zeros((128, NCH, 64), np.float32)
    for n in range(NCH):
        cm[n * CH:(n + 1) * CH, n, :] = 1.0
    c["cmask"] = cm.reshape(128, NCH * 64)
    rm = np.ones((128, S), np.float32)
    rm[:, ::CH] = 0.0
    c["resetm"] = rm
    bo = np.zeros((128, 128), np.float32)
    bo[:64, :64] = 1.0
    bo[64:, 64:] = 1.0
    c["blkones"] = bo
    slopes = 2.0 ** (-8.0 * (np.arange(8) + 1.0) / 8.0)
    al = np.zeros((128, 8, 16), np.float32)
    for h in range(8):
        for dl in range(16):
            al[:, h, dl] = slopes[h] * (-dl * 128.0 + np.arange(128))
    c["alibi"] = al.reshape(128, 128)
    c["causal"] = (t[:, None] <= t[None, :]).astype(np.float32)
    pos = np.arange(S)
    alk = np.zeros((8, 3, S), np.float32)
    alq = np.zeros((8, 3, S), np.float32)
    for h in range(8):
        alk[h, 0] = slopes[h] * (pos % 128); alq[h, 0] = 1.0
        alk[h, 1] = 1.0;                      alq[h, 1] = -slopes[h] * 128.0 * (pos // 128)
        alk[h, 2] = slopes[h] * 128.0 * (pos // 128); alq[h, 2] = 1.0
    c["alk"] = alk.reshape(24, S)
    c["alq"] = alq.reshape(24, S)
    pm = np.zeros((128, 8, 8), np.float32)
    pi = np.zeros((128, 8, 8), np.float32)
    for j in range(8):
        pm[:, j, j:] = -1e30
        pi[:, j, :j] = 1.0
    c["tri"] = (t[:, None] < t[None, :]).astype(np.float32)
    c["iota32"] = np.tile(np.arange(32, dtype=np.float32), (128, 1))
    c["ecap"] = np.tile(np.arange(32, dtype=np.float32) * CAP, (128, 1))
    c["pastm"] = pm.reshape(128, 64)
    c["pasti"] = pi.reshape(128, 64)
    return c


CONST_SHAPES = {"ident": [128, 128], "mask_si": [128, 256], "mask_l": [128, 128], "cmask": [128, NCH * 64],
                "resetm": [128, S], "blkones": [128, 128], "alibi": [128, 128], "causal": [128, 128],
                "pastm": [128, 64], "pasti": [128, 64], "tri": [128, 128], "iota32": [128, 32], "ecap": [128, 32], "alk": [24, S], "alq": [24, S]}


IN_SHAPES = {
    "x": [NB, S, D], "cT": [NB, 128, 8], "w_ada": [D, 6 * D], "b_ada": [6 * D], "norm1_g": [D],
    "win_l": [27, 128, 8, 128], "mu_p": [1920], "pvec": [128, 4, 7],
    "w_up": [64, 512], "a_up": [64, 512], "g_up": [160, 512],
    "qkg": [128, 2], "w_out": [D, D], "norm2_g": [D], "w_router": [D, NEXP], "b_router": [NEXP],
    "wgu_l": [NEXP, 8, 128, 8, 256], "bgu_l": [128, NEXP, 16], "w_down": [NEXP, D, D], "b_down": [NEXP, D],
}


def build_program(stages=99, debug=False, small=False, t2=None, t3=None, t5=None):
    nc = bass.Bass("TRN2", target_bir_lowering=False)
    I = {}
    for k, shp in IN_SHAPES.items():
        if small and k in ("wgu_l", "w_down"):
            shp = [1] + list(shp[1:])
        I[k] = nc.dram_tensor(k, shp, F32, kind="ExternalInput").ap()
    for k, shp in CONST_SHAPES.items():
        I[k] = nc.dram_tensor("c_" + k, shp, F32, kind="ExternalInput").ap()
    out = nc.dram_tensor("out", [NB, S, D], F32, kind="ExternalOutput").ap()
    projF = nc.dram_tensor("projF", [NB, 27 * 128, S], F32, kind=("ExternalInput" if (t2 or t3 is not None) else "Internal")).ap()
    t3only = t3 is not None
    t2 = t2 or {}
    t3 = t3 or {}
    t5 = t5 or {}
    T2_NHP = t2.get("nhp", 4); T2_NBLK = t2.get("nblk", S // 128); T2_UPTO = t2.get("upto", 99)
    ymixT = nc.dram_tensor("ymixT", [NB, 512, S], BF16, kind="Internal").ap()
    ymoba = nc.dram_tensor("ymoba", [NB, S, 512], BF16, kind="Internal").ap()
    xmid = nc.dram_tensor("xmid", [NB, S, D], F32, kind="Internal").ap()
    XS = nc.dram_tensor("XS", [NEXP * CAP, D], BF16, kind="Internal").ap()
    YS = nc.dram_tensor("YS", [NEXP * CAP, D], F32, kind="Internal").ap()
    dbg = {}
    if debug:
        dbg["d_proj"] = nc.dram_tensor("d_proj", [27 * 128, S], F32, kind="ExternalOutput").ap()
        dbg["d_yr"] = nc.dram_tensor("d_yr", [512, S], F32, kind="ExternalOutput").ap()
        dbg["d_ym"] = nc.dram_tensor("d_ym", [S, 512], BF16, kind="ExternalOutput").ap()
        dbg["d_xm"] = nc.dram_tensor("d_xm", [S, D], F32, kind="ExternalOutput").ap()
        dbg["d_gw"] = nc.dram_tensor("d_gw", [S, NEXP], F32, kind="ExternalOutput").ap()
        dbg["d_yraw"] = nc.dram_tensor("d_yraw", [512, S], F32, kind="ExternalOutput").ap()

    with contextlib.ExitStack() as gst:
        P = Prog(nc, gst)
        uid = [0]

        def mk(st, space, shape, dt, name=None):
            uid[0] += 1
            nm = "%s_%d" % (name or "t", uid[0])
            if space == "sb":
                return st.enter_context(nc.sbuf_tensor(nm, shape, dt))
            return st.enter_context(nc.psum_tensor(nm, shape, dt))

        C = {}
        for k, shp in CONST_SHAPES.items():
            if k in ("resetm", "alk", "alq"):
                continue
            C[k] = mk(gst, "sb", shp, F32, "c" + k)
            P.dma("sp", (lambda e, k=k: e.dma_start(out=C[k][:], in_=I[k])), writes=["c_" + k])
        identb = mk(gst, "sb", [128, 128], BF16, "identb")
        P.op("dve", lambda e: e.tensor_copy(out=identb[:], in_=C["ident"][:]), reads=["c_ident"], writes=["identb"])
        msi_b = mk(gst, "sb", [128, 256], BF16, "msib")
        P.op("dve", lambda e: e.tensor_copy(out=msi_b[:], in_=C["mask_si"][:]), reads=["c_mask_si"], writes=["msib"])
        blk_b = mk(gst, "sb", [128, 128], BF16, "blkb")
        P.op("dve", lambda e: e.tensor_copy(out=blk_b[:], in_=C["blkones"][:]), reads=["c_blkones"], writes=["blkb"])
        pvec = mk(gst, "sb", [128, 4, 7], F32, "pvec")
        P.dma("sp", lambda e: e.dma_start(out=pvec[:], in_=I["pvec"]), writes=["pvec"])
        qkg = mk(gst, "sb", [128, 2], F32, "qkg")
        P.dma("sp", lambda e: e.dma_start(out=qkg[:], in_=I["qkg"]), writes=["qkg"])
        P.barrier()

        def mods_piece(st, b, slot, dest, destname, mode, gvec=None):
            cb = mk(st, "sb", [128, 8], F32, "cb")
            cbs = mk(st, "sb", [128, 8], F32, "cbs")
            CB = mk(st, "sb", [128, 8, 128], BF16, "CB")
            wp = [mk(st, "sb", [128, 8, 512], BF16, "wadap") for _ in range(2)]
            bb = [mk(st, "sb", [128, 512], F32, "badab") for _ in range(2)]
            pp = [mk(st, "ps", [128, 512], F32, "modps") for _ in range(2)]
            u = "m%d_%d_" % (b, slot)
            P.dma("sp", lambda e: e.dma_start(out=cb[:], in_=I["cT"][b]), writes=[u + "cb"])
            P.op("act", lambda e: e.activation(out=cbs[:], in_=cb[:], func=AF.Silu), reads=[u + "cb"], writes=[u + "cbs"])
            P.op("dve", lambda e: e.tensor_copy(out=CB[:], in_=cbs[:].unsqueeze(2).to_broadcast([128, 8, 128])),
                 reads=[u + "cbs"], writes=[u + "CB"])
            gb = None
            if mode == "scale":
                gb = mk(st, "sb", [128, 1024], F32, "gvecb")
                P.dma("sp", lambda e: e.dma_start(out=gb[:], in_=gvec.partition_broadcast(128)), writes=[u + "gb"])
            wv = I["w_ada"].rearrange("(k p) n -> p k n", p=128)
            for half in range(2):
                c0 = slot * 1024 + half * 512
                P.dma("pool", (lambda e, half=half, c0=c0: e.dma_start(out=wp[half][:], in_=wv[:, :, c0:c0 + 512])),
                      writes=[u + "wp%d" % half])
                P.dma("sp", (lambda e, half=half, c0=c0: e.dma_start(out=bb[half][:],
                                                                      in_=I["b_ada"][c0:c0 + 512].partition_broadcast(128))),
                      writes=[u + "bb%d" % half])
                for k in range(8):
                    P.op("pe", (lambda e, half=half, k=k: e.matmul(pp[half][:], lhsT=CB[:, k, :], rhs=wp[half][:, k, :],
                                                                   start=(k == 0), stop=(k == 7))),
                         reads=[u + "CB", u + "wp%d" % half], writes=[u + "pp%d" % half])
                dsl = dest[:, half * 512:(half + 1) * 512]
                if mode == "plain":
                    P.op("dve", (lambda e, half=half, dsl=dsl: e.tensor_tensor(out=dsl, in0=pp[half][:], in1=bb[half][:], op=ALU.add)),
                         reads=[u + "pp%d" % half, u + "bb%d" % half], writes=[destname])
                else:
                    P.op("dve", (lambda e, half=half: e.tensor_tensor(out=bb[half][:], in0=pp[half][:], in1=bb[half][:], op=ALU.add)),
                         reads=[u + "pp%d" % half, u + "bb%d" % half], writes=[u + "bb%d" % half])
                    P.op("dve", (lambda e, half=half, dsl=dsl: e.scalar_tensor_tensor(
                        out=dsl, in0=bb[half][:], scalar=1.0, in1=gb[:, half * 512:(half + 1) * 512], op0=ALU.add, op1=ALU.mult)),
                         reads=[u + "bb%d" % half, u + "gb"], writes=[destname])

        def norm_tile(st_bufs, src_ap, Gt, Bt, Gn, Bn, tag, hb_out, extra_reads=()):
            xt, junk, ss, t1 = st_bufs
            P.dma("sp", lambda e: e.dma_start(out=xt[:], in_=src_ap), reads=list(extra_reads), writes=[tag + "xt"])
            P.op("act", lambda e: e.activation(out=junk[:], in_=xt[:], func=AF.Square, accum_out=ss[:, 0:1]),
                 reads=[tag + "xt"], writes=[tag + "junk", tag + "ss"])
            P.op("dve", lambda e: e.tensor_scalar(out=ss[:, 1:2], in0=ss[:, 0:1], scalar1=1.0 / D, scalar2=1e-6,
                                                  op0=ALU.mult, op1=ALU.add), reads=[tag + "ss"], writes=[tag + "ss"])
            P.op("act", lambda e: e.activation(out=ss[:, 1:2], in_=ss[:, 1:2], func=AF.Sqrt), reads=[tag + "ss"], writes=[tag + "ss"])
            P.op("dve", lambda e: e.reciprocal(out=ss[:, 1:2], in_=ss[:, 1:2]), reads=[tag + "ss"], writes=[tag + "ss"])
            P.op("dve", lambda e: e.scalar_tensor_tensor(out=t1[:], in0=xt[:], scalar=ss[:, 1:2], in1=Gt[:], op0=ALU.mult, op1=ALU.mult),
                 reads=[tag + "xt", tag + "ss", Gn], writes=[tag + "t1"])
            P.op("pool", lambda e: e.tensor_tensor(out=hb_out[0][:], in0=t1[:], in1=Bt[:], op=ALU.add),
                 reads=[tag + "t1", Bn], writes=[hb_out[1]])

        HM8 = mk(gst, "sb", [128, 2], F32, "HM8")
        for hh_ in range(2):
            P.op("dve", (lambda e, hh_=hh_: e.tensor_scalar(out=HM8[:, hh_:hh_ + 1], in0=C["blkones"][:, hh_ * 64:hh_ * 64 + 1], scalar1=0.125, scalar2=None,
                                                            op0=ALU.mult)), reads=["c_blkones"], writes=["HM8"])
        idsel = mk(gst, "sb", [128, 64], F32, "idsel")
        P.op("dve", lambda e: e.tensor_tensor(out=idsel[:], in0=C["ident"][:, 0:64], in1=C["ident"][:, 64:128], op=ALU.add),
             reads=["c_ident"], writes=["idsel"])
        P.barrier()

        def interleave(*gens):
            gens = list(gens)
            while gens:
                for g in list(gens):
                    try:
                        next(g)
                    except StopIteration:
                        gens.remove(g)

        def st2_rwkv(b):
            with contextlib.ExitStack() as st:
                RM = mk(st, "sb", [128, S], F32, "RM")
                P.dma("sp", lambda e: e.dma_start(out=RM[:], in_=I["resetm"]), writes=["c_resetm"])
                LA = mk(st, "sb", [128, S], BF16, "LA")
                SG1 = mk(st, "sb", [128, S], BF16, "SG1")
                SG2 = mk(st, "sb", [32, S], BF16, "SG2")
                WUP = mk(st, "sb", [128, 512], BF16, "WUP")
                GUP1 = mk(st, "sb", [128, 512], BF16, "GUP1")
                GUP2 = mk(st, "sb", [32, 512], BF16, "GUP2")
                T = [mk(st, "sb", [128, S], F32, "T%d" % i) for i in range(8)]
                T1, T2, T3, T4, T5, T6, T7 = T[1:8]
                AFt, BFt, KFt, RFt, VFt, Qt = [mk(st, "sb", [128, S], BF16, n) for n in ("AFt", "BFt", "KFt", "RFt", "VFt", "Qt")]
                bank = {n: mk(st, "ps", [128, 512], F32, "bk" + n) for n in "BCDEFGH"}
                pp = [bank["E"], bank["F"]]
                ppn = ["bkE", "bkF"]
                bankA = mk(st, "ps", [128, 1024], BF16, "bkA")
                ppi = [0]

                def mm_tg(lhsT_fn, rhs_fn, reads, evac_fn, nacc=1):
                    for tg in range(4):
                        i = ppi[0] % 2
                        ppi[0] += 1
                        for a in range(nacc):
                            P.op("pe", (lambda e, i=i, a=a, tg=tg: e.matmul(pp[i][:], lhsT=lhsT_fn(a), rhs=rhs_fn(a, tg),
                                                                             start=(a == 0), stop=(a == nacc - 1))),
                                 reads=reads, writes=[ppn[i]])
                        evac_fn(pp[i], ppn[i], tg)

                sl = lambda tg: slice(tg * 512, (tg + 1) * 512)
                P.dma("sp", lambda e: e.dma_start(out=T1[:], in_=projF[b, 1536:1664, :]), reads=[], writes=["T1"])
                P.op("act", lambda e: e.activation(out=LA[0:64, :], in_=T1[0:64, :], func=AF.Tanh), reads=["T1"], writes=["LA"])
                P.op("act", lambda e: e.copy(out=LA[64:128, :], in_=T1[64:128, :]), reads=["T1"], writes=["LA"])
                P.dma("sp", lambda e: e.dma_start(out=T2[:], in_=projF[b, 1664:1792, :]), reads=[], writes=["T2"])
                P.op("act", lambda e: e.activation(out=SG1[:], in_=T2[:], func=AF.Sigmoid), reads=["T2"], writes=["SG1"])
                P.dma("sp", lambda e: e.dma_start(out=T3[0:32, :], in_=projF[b, 1792:1824, :]), reads=[], writes=["T3"])
                P.op("act", lambda e: e.activation(out=SG2[:], in_=T3[0:32, :], func=AF.Sigmoid), reads=["T3"], writes=["SG2"])
                WUP2 = mk(st, "sb", [128, 512], BF16, "WUP2")
                P.op("pool", lambda e: e.memset(WUP[:], 0.0), writes=["WUP"])
                P.op("pool", lambda e: e.memset(WUP2[:], 0.0), writes=["WUP2"])
                P.dma("pool", lambda e: e.dma_start(out=WUP[0:64, :], in_=I["w_up"]), writes=["WUP"])
                P.dma("pool", lambda e: e.dma_start(out=WUP2[64:128, :], in_=I["a_up"]), writes=["WUP2"])
                P.dma("pool", lambda e: e.dma_start(out=GUP1[:], in_=I["g_up"][0:128, :]), writes=["GUP1"])
                P.dma("pool", lambda e: e.dma_start(out=GUP2[:], in_=I["g_up"][128:160, :]), writes=["GUP2"])

                def do_hp(hp):
                    cs = slice(hp * 128, (hp + 1) * 128)
                    pv = lambda i: pvec[:, hp, i:i + 1]
                    mm_tg(lambda a: WUP[:, cs], lambda a, tg: LA[:, sl(tg)], ["WUP", "LA"],
                          lambda ps, pn, tg: P.op("act", lambda e: e.activation(out=T1[:, sl(tg)], in_=ps[:], func=AF.Sigmoid, bias=pv(0)),
                                                  reads=[pn, "pvec"], writes=["T1"]))
                    P.op("act", lambda e: e.activation(out=T1[:], in_=T1[:], func=AF.Identity, scale=-0.6065306597126334),
                         reads=["T1"], writes=["T1"])
                    P.op("dve", lambda e: e.tensor_tensor_scan(out=T2[:], data0=RM[:], data1=T1[:], initial=0.0,
                                                               op0=ALU.mult, op1=ALU.add), reads=["T1", "c_resetm"], writes=["T2"])
                    P.op("dve", lambda e: e.tensor_tensor(out=T3[:], in0=T2[:], in1=T1[:], op=ALU.subtract), reads=["T1", "T2"], writes=["T3"])
                    P.op("act", lambda e: e.activation(out=T3[:], in_=T3[:], func=AF.Exp), reads=["T3"], writes=["T3"])
                    P.op("act", lambda e: e.activation(out=T4[:], in_=T2[:], func=AF.Exp), reads=["T2"], writes=["T4"])
                    P.op("act", lambda e: e.activation(out=T2[:], in_=T2[:], func=AF.Exp, scale=-1.0), reads=["T2"], writes=["T2"])
                    mm_tg(lambda a: WUP2[:, cs], lambda a, tg: LA[:, sl(tg)], ["WUP2", "LA"],
                          lambda ps, pn, tg: P.op("act", lambda e: e.activation(out=T1[:, sl(tg)], in_=ps[:], func=AF.Sigmoid, bias=pv(1)),
                                                  reads=[pn, "pvec"], writes=["T1"]))
                    P.dma("sp", lambda e: e.dma_start(out=T5[:], in_=projF[b, 512 + hp * 128:512 + (hp + 1) * 128, :]),
                          reads=[], writes=["T5"])
                    P.op("dve", lambda e: e.tensor_scalar(out=T6[:], in0=T5[:], scalar1=pv(2), scalar2=None, op0=ALU.mult),
                         reads=["T5", "pvec"], writes=["T6"])
                    P.op("dve", lambda e: e.tensor_tensor(out=Qt[:], in0=T6[:], in1=T6[:], op=ALU.mult), reads=["T6"], writes=["Qt"])
                    mm_tg(lambda a: blk_b[:], lambda a, tg: Qt[:, sl(tg)], ["blkb", "Qt"],
                          lambda ps, pn, tg: P.op("dve", lambda e: e.tensor_scalar(out=T7[:, sl(tg)], in0=ps[:], scalar1=1e-24, scalar2=None, op0=ALU.max),
                                                  reads=[pn], writes=["T7"]))
                    P.op("act", lambda e: e.activation(out=T7[:], in_=T7[:], func=AF.Ln), reads=["T7"], writes=["T7"])
                    P.op("act", lambda e: e.activation(out=T7[:], in_=T7[:], func=AF.Exp, scale=-0.5), reads=["T7"], writes=["T7"])
                    P.op("dve", lambda e: e.tensor_tensor(out=T6[:], in0=T6[:], in1=T7[:], op=ALU.mult), reads=["T6", "T7"], writes=["T6"])
                    P.op("dve", lambda e: e.scalar_tensor_tensor(out=AFt[:], in0=T6[:], scalar=-1.0, in1=T3[:], op0=ALU.mult, op1=ALU.mult),
                         reads=["T6", "T3"], writes=["AFt"])
                    P.op("dve", lambda e: e.tensor_tensor(out=T7[:], in0=T6[:], in1=T1[:], op=ALU.mult), reads=["T6", "T1"], writes=["T7"])
                    P.op("dve", lambda e: e.tensor_tensor(out=BFt[:], in0=T7[:], in1=T2[:], op=ALU.mult), reads=["T7", "T2"], writes=["BFt"])
                    P.op("dve", lambda e: e.tensor_scalar(out=T7[:], in0=T1[:], scalar1=-1.0, scalar2=pv(3), op0=ALU.add, op1=ALU.mult),
                         reads=["T1", "pvec"], writes=["T7"])
                    P.op("dve", lambda e: e.scalar_tensor_tensor(out=T5[:], in0=T7[:], scalar=1.0, in1=T5[:], op0=ALU.add, op1=ALU.mult),
                         reads=["T7", "T5"], writes=["T5"])
                    P.op("dve", lambda e: e.tensor_tensor(out=KFt[:], in0=T5[:], in1=T2[:], op=ALU.mult), reads=["T5", "T2"], writes=["KFt"])
                    P.dma("sp", lambda e: e.dma_start(out=T1[:], in_=projF[b, hp * 128:(hp + 1) * 128, :]), reads=[], writes=["T1"])
                    P.op("dve", lambda e: e.tensor_tensor(out=RFt[:], in0=T1[:], in1=T4[:], op=ALU.mult), reads=["T1", "T4"], writes=["RFt"])
                    P.op("dve", lambda e: e.tensor_tensor(out=T6[:], in0=T1[:], in1=T5[:], op=ALU.mult), reads=["T1", "T5"], writes=["T6"])
                    P.op("dve", lambda e: e.tensor_scalar(out=Qt[:], in0=T6[:], scalar1=pv(4), scalar2=None, op0=ALU.mult),
                         reads=["T6", "pvec"], writes=["Qt"])
                    P.dma("sp", lambda e: e.dma_start(out=T3[:], in_=projF[b, 1024 + hp * 128:1024 + (hp + 1) * 128, :]),
                          reads=[], writes=["T3"])
                    P.op("act", lambda e: e.copy(out=VFt[:], in_=T3[:]), reads=["T3"], writes=["VFt"])
                    mm_tg(lambda a: blk_b[:], lambda a, tg: Qt[:, sl(tg)], ["blkb", "Qt"],
                          lambda ps, pn, tg: P.op("dve", lambda e: e.tensor_tensor(out=T7[:, sl(tg)], in0=ps[:], in1=T3[:, sl(tg)], op=ALU.mult),
                                                  reads=[pn, "T3"], writes=["T7"]))
                    mm_tg(lambda a: (GUP1[:, cs] if a == 0 else GUP2[:, cs]), lambda a, tg: (SG1[:, sl(tg)] if a == 0 else SG2[:, sl(tg)]),
                          ["GUP1", "GUP2", "SG1", "SG2"],
                          lambda ps, pn, tg: P.op("act", lambda e: e.copy(out=T2[:, sl(tg)], in_=ps[:]), reads=[pn], writes=["T2"]),
                          nacc=2)

                    with contextlib.ExitStack() as sc:
                        sbc = lambda shape, dt, n: mk(sc, "sb", shape, dt, n)
                        hm = lambda hh: C["blkones"][:, hh * 64:hh * 64 + 1]
                        AFh = [sbc([128, S], BF16, "AFh") for _ in range(2)]
                        RFh = [sbc([128, S], BF16, "RFh") for _ in range(2)]
                        BFh = [sbc([128, S], BF16, "BFh") for _ in range(2)]
                        for hh in range(2):
                            P.op("act", (lambda e, hh=hh: e.activation(out=AFh[hh][:], in_=AFt[:], func=AF.Identity, scale=hm(hh))),
                                 reads=["AFt", "c_blkones"], writes=["AFh%d" % hh])
                            P.op("act", (lambda e, hh=hh: e.activation(out=RFh[hh][:], in_=RFt[:], func=AF.Identity, scale=hm(hh))),
                                 reads=["RFt", "c_blkones"], writes=["RFh%d" % hh])
                            P.op("act", (lambda e, hh=hh: e.activation(out=BFh[hh][:], in_=BFt[:], func=AF.Identity, scale=hm(hh))),
                                 reads=["BFt", "c_blkones"], writes=["BFh%d" % hh])
                        TT = sbc([128, 4, 128], BF16, "TT")
                        PADT = sbc([128, 3, 384], BF16, "PADT")
                        ZZ = sbc([128, 2, 384], BF16, "ZZ")
                        XM1 = sbc([128, 2, 256], BF16, "XM1")
                        XM2 = sbc([128, 2, 256], BF16, "XM2")
                        LM = sbc([128, 2, 128], BF16, "LM")
                        Z = [sbc([128, 2, 128], BF16, "Z%d" % i) for i in range(KLV + 1)]
                        XPw = {k: sbc([128, 2, 128], BF16, "XP%d" % k) for k in range(1, KLV)}
                        LPw = {k: sbc([128, 2, 128], BF16, "LP%d" % k) for k in range(1, KLV - 1)}
                        BX, UX, VX = [sbc([128, 2, NCH * 64], BF16, n) for n in ("BX", "UX", "VX")]
                        RH = [sbc([128, 128], F32, "RH") for _ in range(2)]
                        YH = [sbc([128, 128], F32, "YH") for _ in range(2)]
                        GT = [sbc([128, NCH, 128], F32, "GT") for _ in range(2)]
                        HG = [sbc([128, NCH, 128], F32, "HG") for _ in range(2)]
                        STt = [sbc([128, 128], F32, "ST") for _ in range(2)]
                        P.op("pool", lambda e: e.memset(STt[0][:], 0.0), writes=["ST0"])
                        P.op("pool", lambda e: e.memset(PADT[:], 0.0), writes=["PADT"])
                        P.op("pool", lambda e: e.memset(ZZ[:], 0.0), writes=["ZZ"])
                        for q_ in range(2):
                            P.op("pool", (lambda e, q_=q_: e.memset(GT[q_][:], 0.0)), writes=["GT%d" % q_])
                            P.op("pool", (lambda e, q_=q_: e.memset(HG[q_][:], 0.0)), writes=["HG%d" % q_])
                        PT = bankA[:].rearrange("p (a c) -> p a c", c=128)[:, 0:4, :]
                        P1 = bank["B"][:].rearrange("p (h c) -> p h c", c=256)
                        P2 = bank["C"][:].rearrange("p (h c) -> p h c", c=256)
                        P3 = bank["D"][:, 0:256].rearrange("p (h c) -> p h c", c=128)
                        P4 = bank["D"][:, 256:384].rearrange("p (h c) -> p h c", c=64)
                        rot = [bank["E"], bank["F"], bank["G"]]
                        rotn = ["bkE", "bkF", "bkG"]
                        ri = [0]
                        cm3 = C["cmask"][:].rearrange("p (n j) -> p n j", j=64)
                        HS = [slice(0, 64), slice(64, 128)]
                        HB = [slice(0, 128), slice(128, 256)]

                        def pre(bl):
                            blk = slice(bl * 128, (bl + 1) * 128)
                            q = bl % 2
                            for a, src in enumerate((AFt, BFt, KFt, VFt)):
                                P.op("pe", (lambda e, a=a, src=src: e.transpose(PT[:, a, :], src[:, blk], identb[:])),
                                     reads=[("AFt", "BFt", "KFt", "VFt")[a], "identb"], writes=["bkA"])
                            P.op("act", lambda e: e.copy(out=TT[:], in_=PT), reads=["bkA"], writes=["TT"])
                            P.op("dve", lambda e: e.tensor_copy(
                                out=PADT[:].rearrange("p a (h c) -> p a h c", c=192)[:, :, :, 0:64],
                                in_=TT[:, 1:4, :].rearrange("p a (h c) -> p a h c", c=64)), reads=["TT"], writes=["PADT"])
                            for hh in range(2):
                                hc = slice(hh * 64, (hh + 1) * 64)
                                P.op("pool", (lambda e, hh=hh, hc=hc: e.tensor_tensor(
                                    out=BX[:, hh, :].rearrange("p (n j) -> p n j", j=64),
                                    in0=TT[:, 1, hc].unsqueeze(1).to_broadcast([128, NCH, 64]), in1=cm3, op=ALU.mult)),
                                     reads=["TT", "c_cmask"], writes=["BX"])
                                P.op("pool", (lambda e, hh=hh, hc=hc: e.tensor_tensor(
                                    out=VX[:, hh, :].rearrange("p (n j) -> p n j", j=64),
                                    in0=TT[:, 3, hc].unsqueeze(1).to_broadcast([128, NCH, 64]), in1=cm3, op=ALU.mult)),
                                     reads=["TT", "c_cmask"], writes=["VX"])
                            yield
                            for hh in range(2):
                                P.op("pe", (lambda e, hh=hh: e.matmul(P1[:, hh, 0:128], lhsT=BFt[:, blk], rhs=AFh[hh][:, blk], start=True, stop=True)),
                                     reads=["BFt", "AFh%d" % hh], writes=["bkB"])
                                P.op("pe", (lambda e, hh=hh: e.matmul(P1[:, hh, 128:256], lhsT=BFt[:, blk], rhs=RFh[hh][:, blk], start=True, stop=True)),
                                     reads=["BFt", "RFh%d" % hh], writes=["bkB"])
                                P.op("pe", (lambda e, hh=hh: e.matmul(P2[:, hh, 0:128], lhsT=KFt[:, blk], rhs=AFh[hh][:, blk], start=True, stop=True)),
                                     reads=["KFt", "AFh%d" % hh], writes=["bkC"])
                                P.op("pe", (lambda e, hh=hh: e.matmul(P2[:, hh, 128:256], lhsT=KFt[:, blk], rhs=RFh[hh][:, blk], start=True, stop=True)),
                                     reads=["KFt", "RFh%d" % hh], writes=["bkC"])
                                P.op("pe", (lambda e, hh=hh: e.matmul(P3[:, hh, :], lhsT=AFt[:, blk], rhs=BFh[hh][:, blk], start=True, stop=True)),
                                     reads=["AFt", "BFh%d" % hh], writes=["bkD"])
                            msk = C["mask_si"][:].unsqueeze(1).to_broadcast([128, 2, 256])
                            P.op("dve", lambda e: e.tensor_tensor(out=XM1[:], in0=P1, in1=msk, op=ALU.mult), reads=["bkB", "c_mask_si"], writes=["XM1"])
                            P.op("dve", lambda e: e.tensor_tensor(out=XM2[:], in0=P2, in1=msk, op=ALU.mult), reads=["bkC", "c_mask_si"], writes=["XM2"])
                            P.op("dve", lambda e: e.tensor_tensor(out=LM[:], in0=P3, in1=C["mask_l"][:].unsqueeze(1).to_broadcast([128, 2, 128]),
                                                                  op=ALU.mult), reads=["bkD", "c_mask_l"], writes=["LM"])
                            yield
                            Xab = lambda hh: XM1[:, hh, 0:128]
                            Mrb = lambda hh: XM1[:, hh, 128:256]
                            Xak = lambda hh: XM2[:, hh, 0:128]
                            Mrk = lambda hh: XM2[:, hh, 128:256]
                            VT = lambda hh: TT[:, 3, hh * 64:(hh + 1) * 64]
                            for hh in range(2):
                                P.op("pe", (lambda e, hh=hh: e.matmul(P4[:, hh, :], lhsT=Xak(hh), rhs=VT(hh), start=True, stop=True)),
                                     reads=["XM2", "TT"], writes=["bkD"])
                            P.op("dve", lambda e: e.tensor_copy(out=Z[0][:, :, 0:64], in_=TT[:, 0, :].rearrange("p (h c) -> p h c", c=64)),
                                 reads=["TT"], writes=["Z0a"])
                            P.op("act", lambda e: e.copy(out=Z[0][:, :, 64:128], in_=P4), reads=["bkD"], writes=["Z0b"])
                            yield

                            def mm2(dst_i, lfn, rfn, reads):
                                for hh in range(2):
                                    P.op("pe", (lambda e, hh=hh: e.matmul(rot[dst_i][:, hh * 128:(hh + 1) * 128], lhsT=lfn(hh), rhs=rfn(hh), start=True, stop=True)),
                                         reads=reads, writes=[rotn[dst_i]])

                            def rv(i):
                                return rot[i][:, 0:256].rearrange("p (h c) -> p h c", c=128)

                            def tl(Xt):
                                return Xt if callable(Xt) else (lambda hh: Xt[:, hh, :])

                            def zstep(k, Xt, xn):
                                i = ri[0] % len(rot)
                                ri[0] += 1
                                zr = ["Z%da" % k, "Z%db" % k] if k == 0 else ["Z%d" % k]
                                mm2(i, tl(Xt), lambda hh: Z[k][:, hh, :], [xn] + zr)
                                P.op("dve", (lambda e, i=i: e.tensor_tensor(out=Z[k + 1][:], in0=rv(i), in1=Z[k][:], op=ALU.add)),
                                     reads=[rotn[i]] + zr, writes=["Z%d" % (k + 1)])

                            def sq(dst, dn, Lt, ln_, Rt, rn_):
                                i = ri[0] % len(rot)
                                ri[0] += 1
                                mm2(i, tl(Lt), tl(Rt), [ln_, rn_])
                                P.op("act", (lambda e, i=i: e.copy(out=dst[:], in_=rv(i))), reads=[rotn[i]], writes=[dn])

                            Lab = lambda hh: LM[:, hh, :]
                            Xk, Xn_, Lk, Ln_ = Xab, "XM1", Lab, "LM"
                            for k in range(KLV):
                                zstep(k, Xk, Xn_)
                                if k < KLV - 1:
                                    sq(XPw[k + 1], "XP%d" % (k + 1), Lk, Ln_, Xk, Xn_)
                                    if k < KLV - 2:
                                        sq(LPw[k + 1], "LP%d" % (k + 1), Xk, Xn_, Lk, Ln_)
                                        Lk, Ln_ = LPw[k + 1], "LP%d" % (k + 1)
                                    Xk, Xn_ = XPw[k + 1], "XP%d" % (k + 1)
                                if k < KLV - 1:
                                    yield
                            P.op("act", lambda e: e.copy(
                                out=ZZ[:].rearrange("p a (h c) -> p a h c", c=192)[:, :, :, 0:64],
                                in_=Z[KLV][:].rearrange("p h (a c) -> p a h c", c=64)), reads=["Z%d" % KLV], writes=["ZZ"])
                            for hh in range(2):
                                P.op("dve", (lambda e, hh=hh: e.tensor_tensor(
                                    out=UX[:, hh, :].rearrange("p (n j) -> p n j", j=64),
                                    in0=Z[KLV][:, hh, 64:128].unsqueeze(1).to_broadcast([128, NCH, 64]), in1=cm3, op=ALU.mult)),
                                     reads=["Z%d" % KLV, "c_cmask"], writes=["UX"])
                            yield
                            Uh = lambda hh: Z[KLV][:, hh, 64:128]
                            PR = bank["B"][:, 0:128]
                            PY = bank["B"][:, 128:256]
                            for hh in range(2):
                                P.op("pe", (lambda e, hh=hh: e.matmul(PR, lhsT=ZZ[:, 0, HB[hh]], rhs=Mrb(hh), start=(hh == 0), stop=(hh == 1))),
                                     reads=["ZZ", "XM1"], writes=["bkB"])
                            for hh in range(2):
                                P.op("pe", (lambda e, hh=hh: e.matmul(PY, lhsT=ZZ[:, 1, HB[hh]], rhs=Mrb(hh), start=(hh == 0), stop=False)),
                                     reads=["ZZ", "XM1"], writes=["bkB"])
                                P.op("pe", (lambda e, hh=hh: e.matmul(PY, lhsT=PADT[:, 2, HB[hh]], rhs=Mrk(hh), start=False, stop=(hh == 1))),
                                     reads=["PADT", "XM2"], writes=["bkB"])
                            P.op("dve", lambda e: e.tensor_tensor(out=RH[q][:], in0=PR, in1=RFt[:, blk], op=ALU.add),
                                 reads=["bkB", "RFt"], writes=["RH%d" % q])
                            P.op("act", lambda e: e.copy(out=YH[q][:], in_=PY), reads=["bkB"], writes=["YH%d" % q])
                            yield
                            PG = bankA[:].bitcast(F32)[:, 0:NCH * 64]
                            PH = bank["C"][:, 0:NCH * 64]
                            for hh in range(2):
                                P.op("pe", (lambda e, hh=hh: e.matmul(PG, lhsT=ZZ[:, 0, HB[hh]], rhs=BX[:, hh, :], start=(hh == 0), stop=(hh == 1))),
                                     reads=["ZZ", "BX"], writes=["bkA"])
                            for hh in range(2):
                                P.op("pe", (lambda e, hh=hh: e.matmul(PH[:], lhsT=PADT[:, 0, HB[hh]], rhs=UX[:, hh, :], start=(hh == 0), stop=False)),
                                     reads=["PADT", "UX"], writes=["bkC"])
                                P.op("pe", (lambda e, hh=hh: e.matmul(PH[:], lhsT=PADT[:, 1, HB[hh]], rhs=VX[:, hh, :], start=False, stop=(hh == 1))),
                                     reads=["PADT", "VX"], writes=["bkC"])
                            gc = T4[:, blk].rearrange("p (n c) -> p n c", c=CH)[:, :, CH - 1]
                            for hh in range(2):
                                hs = HS[hh]
                                P.op("dve", (lambda e, hs=hs: e.tensor_tensor(out=GT[q][hs, :, hs], in0=PG[hs, :].rearrange("p (n j) -> p n j", j=64),
                                                                              in1=idsel[hs, :].unsqueeze(1).to_broadcast([64, NCH, 64]), op=ALU.add)),
                                     reads=["bkA", "idsel"], writes=["GT%d" % q])
                                P.op("dve", (lambda e, hs=hs: e.tensor_tensor(out=HG[q][hs, :, hs], in0=PH[hs, :].rearrange("p (n j) -> p n j", j=64),
                                                                              in1=gc[hs, :].unsqueeze(2).to_broadcast([64, NCH, 64]), op=ALU.mult)),
                                     reads=["bkC", "T4"], writes=["HG%d" % q])
                            yield

                        def chain(bl):
                            blk = slice(bl * 128, (bl + 1) * 128)
                            q = bl % 2
                            PYc = bank["H"][:, 0:128]
                            PS = bank["H"][:, 128:256]
                            for n in range(NCH):
                                g = bl * NCH + n
                                cur, nxt = STt[g % 2], STt[(g + 1) % 2]
                                cn, nn = "ST%d" % (g % 2), "ST%d" % ((g + 1) % 2)
                                P.op("pe", (lambda e, n=n, cur=cur: e.matmul(PYc[:, n * CH:(n + 1) * CH], lhsT=cur[:], rhs=RH[q][:, n * CH:(n + 1) * CH],
                                                                             start=True, stop=True)),
                                     reads=[cn, "RH%d" % q], writes=["bkH"])
                                P.op("pe", (lambda e, n=n, cur=cur: e.matmul(PS, lhsT=GT[q][:, n, :], rhs=cur[:], start=True, stop=True)),
                                     reads=[cn, "GT%d" % q], writes=["bkH"])
                                for _w in range(WARM):
                                    P.op("pe", lambda e: e.matmul(bank["G"][:], lhsT=identb[:], rhs=AFt[:, 0:512], start=True, stop=True),
                                         reads=[], writes=["bkG"])
                                gcol = T4[:, bl * 128 + n * CH + CH - 1:bl * 128 + n * CH + CH]
                                P.op("dve", (lambda e, n=n, nxt=nxt, gcol=gcol: e.scalar_tensor_tensor(out=nxt[:], in0=PS, scalar=gcol, in1=HG[q][:, n, :],
                                                                                                        op0=ALU.mult, op1=ALU.add)),
                                     reads=["bkH", "T4", "HG%d" % q], writes=[nn])
                                yield
                            P.op("dve", lambda e: e.tensor_tensor(out=T5[:, blk], in0=PYc, in1=YH[q][:], op=ALU.add),
                                 reads=["bkH", "YH%d" % q], writes=["T5"])
                            yield

                        nblk = T2_NBLK
                        if T2_UPTO == 2:
                            g_ = pre(0)
                            for _ in range(t2.get("pre", 99)):
                                try:
                                    next(g_)
                                except StopIteration:
                                    break
                        elif T2_UPTO >= 2:
                            interleave(pre(0))
                        if T2_UPTO >= 3:
                            for bl in range(nblk):
                                if bl + 1 < nblk:
                                    interleave(chain(bl), pre(bl + 1))
                                else:
                                    interleave(chain(bl))

                    if T2_UPTO < 4:
                        P.barrier()
                        return
                    P.op("act", lambda e: e.copy(out=Qt[:], in_=T5[:]), reads=["T5"], writes=["Qt"])
                    mm_tg(lambda a: blk_b[:], lambda a, tg: Qt[:, sl(tg)], ["blkb", "Qt"],
                          lambda ps, pn, tg: P.op("dve", lambda e: e.scalar_tensor_tensor(out=T3[:, sl(tg)], in0=ps[:], scalar=-1.0 / 64, in1=T5[:, sl(tg)],
                                                                                          op0=ALU.mult, op1=ALU.add),
                                                  reads=[pn, "T5"], writes=["T3"]))
                    P.op("dve", lambda e: e.tensor_tensor(out=Qt[:], in0=T3[:], in1=T3[:], op=ALU.mult), reads=["T3"], writes=["Qt"])
                    mm_tg(lambda a: blk_b[:], lambda a, tg: Qt[:, sl(tg)], ["blkb", "Qt"],
                          lambda ps, pn, tg: P.op("dve", lambda e: e.tensor_scalar(out=T6[:, sl(tg)], in0=ps[:], scalar1=1.0 / 64, scalar2=64e-5,
                                                                                   op0=ALU.mult, op1=ALU.add),
                                                  reads=[pn], writes=["T6"]))
                    P.op("act", lambda e: e.activation(out=T6[:], in_=T6[:], func=AF.Ln), reads=["T6"], writes=["T6"])
                    P.op("act", lambda e: e.activation(out=T6[:], in_=T6[:], func=AF.Exp, scale=-0.5), reads=["T6"], writes=["T6"])
                    P.op("dve", lambda e: e.tensor_tensor(out=T3[:], in0=T3[:], in1=T6[:], op=ALU.mult), reads=["T3", "T6"], writes=["T3"])
                    P.op("dve", lambda e: e.tensor_scalar(out=T3[:], in0=T3[:], scalar1=pv(5), scalar2=pv(6), op0=ALU.mult, op1=ALU.add),
                         reads=["T3", "pvec"], writes=["T3"])
                    P.op("dve", lambda e: e.tensor_tensor(out=T3[:], in0=T3[:], in1=T7[:], op=ALU.add), reads=["T3", "T7"], writes=["T3"])
                    P.op("dve", lambda e: e.tensor_tensor(out=Qt[:], in0=T3[:], in1=T2[:], op=ALU.mult), reads=["T3", "T2"], writes=["Qt"])
                    P.dma("sp", lambda e: e.dma_start(out=ymixT[b, hp * 128:(hp + 1) * 128, :], in_=Qt[:]), reads=["Qt"], writes=[])
                    if debug and b == 0:
                        P.dma("sp", lambda e: e.dma_start(out=dbg["d_yr"][hp * 128:(hp + 1) * 128, :], in_=T3[:]), reads=["T3"])
                        P.dma("sp", lambda e: e.dma_start(out=dbg["d_yraw"][hp * 128:(hp + 1) * 128, :], in_=T5[:]), reads=["T5"])
                    P.barrier()

                for hp in range(T2_NHP):
                    do_hp(hp)
            P.barrier()

        def st3_moba(b):
            with contextlib.ExitStack() as st:
                TQ, TK, TV, RR = [mk(st, "sb", [128, S], F32, n) for n in ("TQ", "TK", "TV", "RR")]
                SQ = mk(st, "sb", [128, S], BF16, "SQ")
                KNBh = [mk(st, "sb", [128, S], BF16, "KNBh") for _ in range(2)]
                QH = [mk(st, "sb", [128, S], BF16, "QH") for _ in range(2)]
                VA = mk(st, "sb", [128, 16, 2, 65], BF16, "VA")
                KM = mk(st, "sb", [128, 8], F32, "KM")
                KMH = [mk(st, "sb", [128, 8], F32, "KMH") for _ in range(2)]
                GATE = mk(st, "sb", [128, 32, 8], F32, "GATE")
                MX = mk(st, "sb", [128, 32, 8], F32, "MX")
                SEL = mk(st, "sb", [128, 32, 8], F32, "SEL")
                ET = [mk(st, "sb", [128, 2, 128], BF16, "ET") for _ in range(4)]
                ACC = [mk(st, "sb", [128, 65], F32, "ACC") for _ in range(2)]
                RC = [mk(st, "sb", [128, 1], F32, "RC") for _ in range(2)]
                YT = mk(st, "sb", [128, 16, 128], BF16, "YT")
                ppx = [mk(st, "ps", [128, 512], F32, "PSpp") for _ in range(2)]
                PTR = mk(st, "ps", [128, 8, 128], BF16, "PStr")
                PGt = mk(st, "ps", [128, 512], F32, "PSg")
                PSs = [mk(st, "ps", [128, 512], F32, "PSs") for _ in range(2)]
                POs = [mk(st, "ps", [128, 512], F32, "PSo") for _ in range(2)]
                sl = lambda tg: slice(tg * 512, (tg + 1) * 512)
                hm = lambda hh: C["blkones"][:, hh * 64:hh * 64 + 1]
                P.op("pool", lambda e: e.memset(VA[:, :, :, 64:65], 1.0), writes=["VA1"])
                cnt = {"pp": 0, "s": 0, "o": 0, "e": 0}

                def rmsn(Tt, tn, gcol):
                    P.op("pool", lambda e: e.tensor_tensor(out=SQ[:], in0=Tt[:], in1=Tt[:], op=ALU.mult), reads=[tn], writes=["SQ"])
                    for tg in range(4):
                        i = cnt["pp"] % 2
                        cnt["pp"] += 1
                        P.op("pe", (lambda e, i=i, tg=tg: e.matmul(ppx[i][:], lhsT=blk_b[:], rhs=SQ[:, sl(tg)], start=True, stop=True)),
                             reads=["blkb", "SQ"], writes=["PSpp%d" % i])
                        P.op("dve", (lambda e, i=i, tg=tg: e.tensor_scalar(out=RR[:, sl(tg)], in0=ppx[i][:], scalar1=1.0 / 64, scalar2=1e-6,
                                                                         op0=ALU.mult, op1=ALU.add)), reads=["PSpp%d" % i], writes=["RR"])
                    P.op("act", lambda e: e.activation(out=RR[:], in_=RR[:], func=AF.Ln), reads=["RR"], writes=["RR"])
                    P.op("act", lambda e: e.activation(out=RR[:], in_=RR[:], func=AF.Exp, scale=-0.5), reads=["RR"], writes=["RR"])
                    P.op("dve", lambda e: e.scalar_tensor_tensor(out=Tt[:], in0=Tt[:], scalar=gcol, in1=RR[:], op0=ALU.mult, op1=ALU.mult),
                         reads=[tn, "RR", "qkg"], writes=[tn])

                def do_hp(hp):
                    r0 = 1920 + hp * 128
                    P.dma("sp", lambda e: e.dma_start(out=TQ[:], in_=projF[b, r0:r0 + 128, :]), reads=[], writes=["TQ"])
                    P.dma("sp", lambda e: e.dma_start(out=TK[:], in_=projF[b, r0 + 512:r0 + 640, :]), reads=[], writes=["TK"])
                    P.dma("sp", lambda e: e.dma_start(out=TV[:], in_=projF[b, r0 + 1024:r0 + 1152, :]), reads=[], writes=["TV"])
                    rmsn(TQ, "TQ", qkg[:, 0:1])
                    for hh in range(2):
                        P.op("act", (lambda e, hh=hh: e.activation(out=QH[hh][:], in_=TQ[:], func=AF.Identity, scale=HM8[:, hh:hh + 1])),
                             reads=["TQ", "HM8"], writes=["QH%d" % hh])
                    rmsn(TK, "TK", qkg[:, 1:2])
                    for hh in range(2):
                        hg_ = hp * 2 + hh
                        o_ = 64 * (1 - hh)
                        P.op("act", (lambda e, hh=hh: e.copy(out=KNBh[hh][:], in_=TK[:])), reads=["TK"], writes=["KNB%d" % hh])
                        P.dma("pool", (lambda e, hh=hh, hg_=hg_, o_=o_: e.dma_start(out=KNBh[hh][o_:o_ + 3, :], in_=I["alk"][hg_ * 3:hg_ * 3 + 3, :])),
                              writes=["KNB%d" % hh])
                        P.dma("pool", (lambda e, hh=hh, hg_=hg_, o_=o_: e.dma_start(out=QH[hh][o_:o_ + 3, :], in_=I["alq"][hg_ * 3:hg_ * 3 + 3, :])),
                              writes=["QH%d" % hh])
                    P.op("dve", lambda e: e.tensor_reduce(out=KM[:], in_=TK[:].rearrange("p (n k) -> p n k", k=256), axis=AX.X, op=ALU.add),
                         reads=["TK"], writes=["KM"])
                    for hh in range(2):
                        P.op("dve", (lambda e, hh=hh: e.tensor_scalar(out=KMH[hh][:], in0=KM[:], scalar1=hm(hh), scalar2=1.0 / 256, op0=ALU.mult, op1=ALU.mult)),
                             reads=["KM", "c_blkones"], writes=["KMH%d" % hh])
                    P.op("act", lambda e: e.copy(out=SQ[:], in_=TV[:]), reads=["TV"], writes=["SQ"])
                    for g8 in range(2):
                        for k8 in range(8):
                            kt = g8 * 8 + k8
                            P.op("pe", (lambda e, kt=kt, k8=k8: e.transpose(PTR[:, k8, :], SQ[:, kt * 128:(kt + 1) * 128], identb[:])),
                                 reads=["SQ", "identb"], writes=["PStr"])
                        P.op("act", (lambda e, g8=g8: e.copy(out=VA[:, g8 * 8:(g8 + 1) * 8, :, 0:64],
                                                             in_=PTR[:].rearrange("p k (h c) -> p k h c", c=64))),
                             reads=["PStr"], writes=["VA"])
                    PG3 = PGt[:, 0:256].rearrange("p (g n) -> p g n", n=8)
                    for qt in range(NT):
                        for hh in range(2):
                            P.op("pe", (lambda e, qt=qt, hh=hh: e.matmul(PG3[:, qt * 2 + hh, :], lhsT=TQ[:, qt * 128:(qt + 1) * 128], rhs=KMH[hh][:],
                                                                         start=True, stop=True)),
                                 reads=["TQ", "KMH%d" % hh], writes=["PSg"])
                    pm4 = C["pastm"][:].rearrange("p (j n) -> p j n", n=8).unsqueeze(2).to_broadcast([128, 8, 4, 8])
                    pi4 = C["pasti"][:].rearrange("p (j n) -> p j n", n=8).unsqueeze(2).to_broadcast([128, 8, 4, 8])
                    g4 = lambda T_: T_[:].rearrange("p (j f) n -> p j f n", f=4)
                    P.op("dve", lambda e: e.tensor_tensor(out=g4(GATE), in0=PGt[:, 0:256].rearrange("p (j f n) -> p j f n", f=4, n=8), in1=pm4, op=ALU.add),
                         reads=["PSg", "c_pastm"], writes=["GATE"])
                    for g in range(32):
                        P.op("dve", (lambda e, g=g: e.max(out=MX[:, g, :], in_=GATE[:, g, :])), reads=["GATE"], writes=["MX"])
                    P.op("dve", lambda e: e.tensor_tensor(out=SEL[:], in0=GATE[:], in1=MX[:, :, 2:3].to_broadcast([128, 32, 8]), op=ALU.is_ge),
                         reads=["GATE", "MX"], writes=["SEL"])
                    P.op("dve", lambda e: e.tensor_tensor(out=g4(SEL), in0=g4(SEL), in1=pi4, op=ALU.mult), reads=["SEL", "c_pasti"], writes=["SEL"])

                    Sb = [(PSs[0], "PSs0"), (PSs[1], "PSs1"), (ppx[0], "PSpp0"), (ppx[1], "PSpp1")]
                    Ob = [(POs[0], "PSo0"), (POs[1], "PSo1"), (PGt, "PSg"), (PTR[:].rearrange("p k c -> p (k c)").bitcast(F32), "PStr")]

                    def front(hh, qt, kts):
                        si = cnt["s"] % 4
                        cnt["s"] += 1
                        ei = cnt["e"] % 4
                        cnt["e"] += 1
                        PSs_, sn = Sb[si]
                        qs = slice(qt * 128, (qt + 1) * 128)
                        nk = len(kts)
                        for idx, kt in enumerate(kts):
                            P.op("pe", (lambda e, idx=idx, kt=kt: e.matmul(PSs_[:, idx * 128:(idx + 1) * 128], lhsT=KNBh[hh][:, kt * 128:(kt + 1) * 128],
                                                                           rhs=QH[hh][:, qs], start=True, stop=True)),
                                 reads=["KNB%d" % hh, "QH%d" % hh], writes=[sn])
                        P.op("act", lambda e: e.activation(out=ET[ei][:, 0:nk, :], in_=PSs_[:, 0:nk * 128].rearrange("p (k c) -> p k c", c=128), func=AF.Exp),
                             reads=[sn], writes=["ET%d" % ei])
                        for idx, kt in enumerate(kts):
                            if kt == qt:
                                P.op("pool", (lambda e, idx=idx: e.tensor_tensor(out=ET[ei][:, idx, :], in0=ET[ei][:, idx, :], in1=C["causal"][:], op=ALU.mult)),
                                     reads=["ET%d" % ei, "c_causal"], writes=["ET%d" % ei])
                        return ei

                    def back(hh, qt, kts, sel_col, first, last, ei):
                        oi = cnt["o"] % 4
                        cnt["o"] += 1
                        POs_, on = Ob[oi]
                        a = qt % 2
                        nk = len(kts)
                        for idx, kt in enumerate(kts):
                            P.op("pe", (lambda e, idx=idx, kt=kt: e.matmul(POs_[:, 0:65], lhsT=ET[ei][:, idx, :], rhs=VA[:, kt, hh, :],
                                                                           start=(idx == 0), stop=(idx == nk - 1))),
                                 reads=["ET%d" % ei, "VA", "VA1"], writes=[on])
                        if first:
                            P.op("act", lambda e: e.copy(out=ACC[a][:], in_=POs_[:, 0:65]), reads=[on], writes=["ACC%d" % a])
                        else:
                            P.op("dve", lambda e: e.scalar_tensor_tensor(out=ACC[a][:], in0=POs_[:, 0:65], scalar=sel_col, in1=ACC[a][:],
                                                                         op0=ALU.mult, op1=ALU.add),
                                 reads=[on, "SEL", "ACC%d" % a], writes=["ACC%d" % a])
                        if last:
                            P.op("dve", lambda e: e.reciprocal(out=RC[a][:], in_=ACC[a][:, 64:65]), reads=["ACC%d" % a], writes=["RC%d" % a])
                            P.op("dve", lambda e: e.tensor_scalar(out=YT[:, qt, hh * 64:(hh + 1) * 64], in0=ACC[a][:, 0:64],
                                                                  scalar1=RC[a][:, 0:1], scalar2=None, op0=ALU.mult),
                                 reads=["ACC%d" % a, "RC%d" % a], writes=["YT"])

                    work = []
                    for hh in range(2):
                        for qt in range(NT):
                            j = qt // 2
                            items = [([qt] if qt % 2 == 0 else [qt - 1, qt], None, True)]
                            for n in range(j):
                                items.append(([2 * n, 2 * n + 1], SEL[:, qt * 2 + hh, n:n + 1], False))
                            for ii, (kts, sc, first) in enumerate(items):
                                work.append((hh, qt, kts, sc, first, ii == len(items) - 1))
                    SKEW = 2
                    pend = []
                    for wi in range(len(work) + SKEW):
                        if wi < len(work):
                            hh, qt, kts, sc, first, last = work[wi]
                            pend.append((work[wi], front(hh, qt, kts)))
                        if wi >= SKEW:
                            (hh, qt, kts, sc, first, last), ei = pend.pop(0)
                            back(hh, qt, kts, sc, first, last, ei)
                    P.dma("sp", lambda e: e.dma_start(out=ymoba[b].rearrange("(t p) c -> p t c", p=128)[:, :, hp * 128:(hp + 1) * 128], in_=YT[:]),
                          reads=["YT"], writes=[])
                    if debug and b == 0:
                        P.dma("sp", lambda e: e.dma_start(out=dbg["d_ym"].rearrange("(t p) c -> p t c", p=128)[:, :, hp * 128:(hp + 1) * 128], in_=YT[:]),
                              reads=["YT"])

                for hp in range(t3.get("nhp", 4)):
                    do_hp(hp)
            P.barrier()

        def st456(b):
            with contextlib.ExitStack() as st:
                h2T = mk(st, "sb", [128, 8, S], BF16, "h2T")
                ACCM = mk(st, "sb", [128, NT, 1024], F32, "ACCM")
                GW = mk(st, "sb", [128, NT, NEXP], F32, "GW")
                GT1, G2, B2, GT2 = [mk(st, "sb", [128, 1024], F32, n) for n in ("GT1", "G2", "B2", "GT2")]
                for slot, dest, dn, mode, gv in ((2, GT1, "GT1", "plain", None), (4, G2, "G2", "scale", I["norm2_g"]),
                                                 (3, B2, "B2", "plain", None), (5, GT2, "GT2", "plain", None)):
                    with contextlib.ExitStack() as st2:
                        mods_piece(st2, b, slot, dest, dn, mode, gv)
                        P.barrier()
                with contextlib.ExitStack() as s4:
                    YR = mk(s4, "sb", [128, 4, S], BF16, "YR")
                    YM = mk(s4, "sb", [128, NT, 512], BF16, "YM")
                    WO = mk(s4, "sb", [128, 8, 1024], BF16, "WO")
                    WR = mk(s4, "sb", [128, 8, NEXP], F32, "WR")
                    BRb = mk(s4, "sb", [128, NEXP], F32, "BRb")
                    BD = mk(s4, "sb", [32, 1024], F32, "BD")
                    P.dma("sp", lambda e: e.dma_start(out=YR[:], in_=ymixT[b].rearrange("(c p) t -> p c t", p=128)), reads=[], writes=["YR"])
                    P.dma("sp", lambda e: e.dma_start(out=YM[:], in_=ymoba[b].rearrange("(t p) c -> p t c", p=128)), reads=[], writes=["YM"])
                    P.dma("pool", lambda e: e.dma_start(out=WO[:], in_=I["w_out"].rearrange("(k p) n -> p k n", p=128)), writes=["WO"])
                    P.dma("sp", lambda e: e.dma_start(out=WR[:], in_=I["w_router"].rearrange("(k p) n -> p k n", p=128)), writes=["WR"])
                    P.dma("sp", lambda e: e.dma_start(out=BRb[:], in_=I["b_router"].partition_broadcast(128)), writes=["BRb"])
                    P.dma("sp", lambda e: e.dma_start(out=BD[:], in_=I["b_down"]), writes=["BD"])
                    nb2 = 2
                    xt1 = mk(s4, "sb", [128, 1024], F32, "xt4")
                    xt = [xt1, xt1]
                    xm = [mk(s4, "sb", [128, 1024], F32, "xm4") for _ in range(nb2)]
                    h2f = [mk(s4, "sb", [128, 1024], F32, "h2f") for _ in range(nb2)]
                    ss = [mk(s4, "sb", [128, 2], F32, "ss4") for _ in range(nb2)]
                    YMT = [mk(s4, "sb", [128, 4, 128], BF16, "YMT") for _ in range(nb2)]
                    h2Tf1 = mk(s4, "sb", [128, 8, 128], F32, "h2Tf")
                    h2Tf = [h2Tf1, h2Tf1]
                    LG = [mk(s4, "sb", [128, NEXP], F32, "LG") for _ in range(nb2)]
                    EX = [mk(s4, "sb", [128, NEXP], F32, "EX") for _ in range(nb2)]
                    MK = [mk(s4, "sb", [128, NEXP], F32, "MK") for _ in range(nb2)]
                    MX8 = [mk(s4, "sb", [128, 8], F32, "MX8") for _ in range(nb2)]
                    sm = [mk(s4, "sb", [128, 4], F32, "sm4") for _ in range(nb2)]
                    gTs = [mk(s4, "sb", [32, 128], F32, "gTs") for _ in range(nb2)]
                    PO = [mk(s4, "ps", [128, 512], F32, "PSo4") for _ in range(2)]
                    PTm = mk(s4, "ps", [128, 8, 128], BF16, "PStm")
                    PTf = [mk(s4, "ps", [128, 4, 128], F32, "PStf") for _ in range(2)]
                    PL = mk(s4, "ps", [128, 512], F32, "PSl")
                    PGt_ = mk(s4, "ps", [128, 512], F32, "PSgt")
                    hsl = lambda h: slice(h * 512, (h + 1) * 512)
                    for tt in range(NT):
                        i = tt % nb2
                        u = "s4_%d_" % i
                        tsl = slice(tt * 128, (tt + 1) * 128)
                        for c in range(4):
                            P.op("pe", (lambda e, c=c, tt=tt: e.transpose(PTm[:, c, :], YM[:, tt, c * 128:(c + 1) * 128], identb[:])),
                                 reads=["YM", "identb"], writes=["PStm"])
                        P.op("act", (lambda e, i=i: e.copy(out=YMT[i][:], in_=PTm[:, 0:4, :])), reads=["PStm"], writes=[u + "YMT"])
                        P.dma("sp", (lambda e, i=i, tt=tt: e.dma_start(out=xt[i][:], in_=I["x"][b, tt * 128:(tt + 1) * 128, :])), writes=["s4_xt"])
                        for h in range(2):
                            for kc in range(8):
                                P.op("pe", (lambda e, i=i, h=h, kc=kc, tsl=tsl: e.matmul(
                                    PO[h][:], lhsT=(YR[:, kc, tsl] if kc < 4 else YMT[i][:, kc - 4, :]), rhs=WO[:, kc, hsl(h)],
                                    start=(kc == 0), stop=(kc == 7))), reads=["YR", u + "YMT", "WO"], writes=["PSo4%d" % h])
                            P.op("dve", (lambda e, i=i, h=h: e.tensor_tensor(out=xm[i][:, hsl(h)], in0=PO[h][:], in1=GT1[:, hsl(h)], op=ALU.mult)),
                                 reads=["PSo4%d" % h, "GT1"], writes=[u + "xm"])
                        P.op("pool", (lambda e, i=i: e.tensor_tensor(out=xm[i][:], in0=xm[i][:], in1=xt[i][:], op=ALU.add)),
                             reads=[u + "xm", "s4_xt"], writes=[u + "xm"])
                        P.dma("sp", (lambda e, i=i, tt=tt: e.dma_start(out=xmid[b, tt * 128:(tt + 1) * 128, :], in_=xm[i][:])),
                              reads=[u + "xm"], writes=[])
                        P.op("act", (lambda e, i=i: e.activation(out=h2f[i][:], in_=xm[i][:], func=AF.Square, accum_out=ss[i][:, 0:1])),
                             reads=[u + "xm"], writes=[u + "h2f", u + "ss"])
                        P.op("dve", (lambda e, i=i: e.tensor_scalar(out=ss[i][:, 1:2], in0=ss[i][:, 0:1], scalar1=1.0 / D, scalar2=1e-6,
                                                                    op0=ALU.mult, op1=ALU.add)), reads=[u + "ss"], writes=[u + "ss"])
                        P.op("act", (lambda e, i=i: e.activation(out=ss[i][:, 1:2], in_=ss[i][:, 1:2], func=AF.Sqrt)), reads=[u + "ss"], writes=[u + "ss"])
                        P.op("dve", (lambda e, i=i: e.reciprocal(out=ss[i][:, 1:2], in_=ss[i][:, 1:2])), reads=[u + "ss"], writes=[u + "ss"])
                        P.op("dve", (lambda e, i=i: e.scalar_tensor_tensor(out=h2f[i][:], in0=xm[i][:], scalar=ss[i][:, 1:2], in1=G2[:],
                                                                           op0=ALU.mult, op1=ALU.mult)), reads=[u + "xm", u + "ss", "G2"], writes=[u + "h2f"])
                        P.op("pool", (lambda e, i=i: e.tensor_tensor(out=h2f[i][:], in0=h2f[i][:], in1=B2[:], op=ALU.add)),
                             reads=[u + "h2f", "B2"], writes=[u + "h2f"])
                        for k in range(8):
                            P.op("pe", (lambda e, i=i, k=k: e.transpose(PTf[k // 4][:, k % 4, :], h2f[i][:, k * 128:(k + 1) * 128], C["ident"][:])),
                                 reads=[u + "h2f", "c_ident"], writes=["PStf%d" % (k // 4)])
                        for g in range(2):
                            P.op("act", (lambda e, i=i, g=g: e.copy(out=h2Tf[i][:, g * 4:(g + 1) * 4, :], in_=PTf[g][:])),
                                 reads=["PStf%d" % g], writes=["s4_h2Tf"])
                            P.op("dve", (lambda e, g=g, tsl=tsl: e.tensor_copy(out=h2T[:, g * 4:(g + 1) * 4, tsl], in_=PTf[g][:])),
                                 reads=["PStf%d" % g], writes=["h2T"])
                        for k in range(8):
                            P.op("pe", (lambda e, i=i, k=k: e.matmul(PL[:, 0:NEXP], lhsT=h2Tf[i][:, k, :], rhs=WR[:, k, :], start=(k == 0), stop=(k == 7))),
                                 reads=["s4_h2Tf", "WR"], writes=["PSl"])
                        P.op("dve", (lambda e, i=i: e.tensor_tensor(out=LG[i][:], in0=PL[:, 0:NEXP], in1=BRb[:], op=ALU.add)),
                             reads=["PSl", "BRb"], writes=[u + "LG"])
                        P.op("dve", (lambda e, i=i: e.max(out=MX8[i][:], in_=LG[i][:])), reads=[u + "LG"], writes=[u + "MX8"])
                        P.op("dve", (lambda e, i=i: e.tensor_scalar(out=sm[i][:, 0:1], in0=MX8[i][:, 0:1], scalar1=-1.0, scalar2=None, op0=ALU.mult)),
                             reads=[u + "MX8"], writes=[u + "sm"])
                        P.op("act", (lambda e, i=i: e.activation(out=EX[i][:], in_=LG[i][:], func=AF.Exp, bias=sm[i][:, 0:1])),
                             reads=[u + "LG", u + "sm"], writes=[u + "EX"])
                        P.op("dve", (lambda e, i=i: e.tensor_scalar(out=MK[i][:], in0=LG[i][:], scalar1=MX8[i][:, 3:4], scalar2=None, op0=ALU.is_ge)),
                             reads=[u + "LG", u + "MX8"], writes=[u + "MK"])
                        P.op("dve", (lambda e, i=i: e.tensor_tensor(out=EX[i][:], in0=EX[i][:], in1=MK[i][:], op=ALU.mult)),
                             reads=[u + "EX", u + "MK"], writes=[u + "EX"])
                        P.op("dve", (lambda e, i=i: e.tensor_reduce(out=sm[i][:, 1:2], in_=EX[i][:], axis=AX.X, op=ALU.add)),
                             reads=[u + "EX"], writes=[u + "sm"])
                        P.op("dve", (lambda e, i=i: e.reciprocal(out=sm[i][:, 2:3], in_=sm[i][:, 1:2])), reads=[u + "sm"], writes=[u + "sm"])
                        P.op("dve", (lambda e, i=i, tt=tt: e.tensor_scalar(out=GW[:, tt, :], in0=EX[i][:], scalar1=sm[i][:, 2:3], scalar2=None, op0=ALU.mult)),
                             reads=[u + "EX", u + "sm"], writes=["GW"])
                        P.op("pe", (lambda e, tt=tt: e.transpose(PGt_[0:32, 0:128], GW[:, tt, :], C["ident"][:])), reads=["GW", "c_ident"], writes=["PSgt"])
                        P.op("act", (lambda e, i=i: e.copy(out=gTs[i][:], in_=PGt_[0:32, 0:128])), reads=["PSgt"], writes=[u + "gTs"])
                        for h in range(2):
                            P.op("pe", (lambda e, i=i, h=h: e.matmul(PO[h][:], lhsT=gTs[i][:], rhs=BD[:, hsl(h)], start=True, stop=True)),
                                 reads=[u + "gTs", "BD"], writes=["PSo4%d" % h])
                            P.op("act", (lambda e, h=h, tt=tt: e.copy(out=ACCM[:, tt, hsl(h)], in_=PO[h][:])), reads=["PSo4%d" % h], writes=["ACCM%d" % tt])
                        if debug and b == 0:
                            P.dma("sp", (lambda e, i=i, tt=tt: e.dma_start(out=dbg["d_xm"][tt * 128:(tt + 1) * 128, :], in_=xm[i][:])), reads=[u + "xm"])
                            P.dma("sp", (lambda e, tt=tt: e.dma_start(out=dbg["d_gw"][tt * 128:(tt + 1) * 128, :], in_=GW[:, tt, :])), reads=["GW"])
                    P.barrier()
                if stages <= 4:
                    P.barrier()
                    return
                with contextlib.ExitStack() as s5:
                    ACT_T = mk(s5, "sb", [128, 8, S], BF16, "ACT_T")
                    NG, NDR = 3, 11
                    WGUr = [mk(s5, "sb", [128, 8, 256], BF16, "WGUr") for _ in range(NG)]
                    WDr = [mk(s5, "sb", [128, 1024], BF16, "WDr") for _ in range(NDR)]
                    BGU = mk(s5, "sb", [128, NEXP, 16], F32, "BGU")
                    BGS = mk(s5, "sb", [128, NEXP, 16], F32, "BGS")
                    P.dma("sp", lambda e: e.dma_start(out=BGU[:], in_=I["bgu_l"]), writes=["BGU"])
                    P.op("dve", lambda e: e.tensor_scalar(out=BGS[:], in0=BGU[:], scalar1=1.702, scalar2=None, op0=ALU.mult), reads=["BGU"], writes=["BGS"])
                    sgt = [mk(s5, "sb", [128, 512], F32, "sgt") for _ in range(2)]
                    gtt = [mk(s5, "sb", [128, 512], F32, "gtt") for _ in range(2)]
                    upt = [mk(s5, "sb", [128, 512], F32, "upt") for _ in range(2)]
                    PGm = [mk(s5, "ps", [128, 512], F32, "PSgm") for _ in range(2)]
                    PUm = [mk(s5, "ps", [128, 512], F32, "PSum") for _ in range(2)]
                    POm = [mk(s5, "ps", [128, 512], F32, "PSom") for _ in range(2)]
                    SIGC = 0.9999933243
                    uc = [0]
                    oc = [0]
                    nexp = t5.get("nexp", NEXP)

                    def load_piece(idx):
                        e_, fc = idx // 8, idx % 8
                        if e_ >= nexp:
                            return
                        P.dma("pool", (lambda e, e_=e_, fc=fc, idx=idx: e.dma_start(out=WGUr[idx % NG][:], in_=I["wgu_l"][e_, fc])),
                              writes=["WGU%d" % (idx % NG)])
                        P.dma("pool", (lambda e, e_=e_, fc=fc, idx=idx: e.dma_start(out=WDr[idx % NDR][:], in_=I["w_down"][e_, fc * 128:(fc + 1) * 128, :])),
                              writes=["WD%d" % (idx % NDR)])
                    LOOK = 2
                    for idx in range(LOOK):
                        load_piece(idx)
                    for ex in range(nexp):
                        for fc in range(8):
                            idx = ex * 8 + fc
                            load_piece(idx + LOOK)
                            wg = WGUr[idx % NG]
                            wn = "WGU%d" % (idx % NG)
                            for tg in range(4):
                                j = uc[0] % 2
                                uc[0] += 1
                                tsl = slice(tg * 512, (tg + 1) * 512)
                                for k in range(8):
                                    P.op("pe", (lambda e, wg=wg, k=k, tsl=tsl, j=j: e.matmul(PGm[j][:], lhsT=wg[:, k, 0:128], rhs=h2T[:, k, tsl],
                                                                                           start=(k == 0), stop=(k == 7))),
                                         reads=[wn, "h2T"], writes=["PSgm%d" % j])
                                for k in range(8):
                                    P.op("pe", (lambda e, wg=wg, k=k, tsl=tsl, j=j: e.matmul(PUm[j][:], lhsT=wg[:, k, 128:256], rhs=h2T[:, k, tsl],
                                                                                           start=(k == 0), stop=(k == 7))),
                                         reads=[wn, "h2T"], writes=["PSum%d" % j])
                                bg = BGU[:, ex, fc:fc + 1]
                                bu = BGU[:, ex, 8 + fc:9 + fc]
                                bs = BGS[:, ex, fc:fc + 1]
                                P.op("act", (lambda e, j=j, bs=bs: e.activation(out=sgt[j][:], in_=PGm[j][:], func=AF.Sigmoid, bias=bs, scale=1.702)),
                                     reads=["PSgm%d" % j, "BGS"], writes=["sgt%d" % j])
                                P.op("dve", (lambda e, j=j, bg=bg: e.tensor_scalar(out=gtt[j][:], in0=PGm[j][:], scalar1=bg, scalar2=7.0, op0=ALU.add, op1=ALU.min)),
                                     reads=["PSgm%d" % j, "BGU"], writes=["gtt%d" % j])
                                P.op("dve", (lambda e, j=j, bu=bu: e.tensor_scalar(out=upt[j][:], in0=PUm[j][:], scalar1=bu, scalar2=-7.0, op0=ALU.add, op1=ALU.max)),
                                     reads=["PSum%d" % j, "BGU"], writes=["upt%d" % j])
                                P.op("dve", (lambda e, j=j: e.tensor_scalar(out=upt[j][:], in0=upt[j][:], scalar1=7.0, scalar2=1.0, op0=ALU.min, op1=ALU.add)),
                                     reads=["upt%d" % j], writes=["upt%d" % j])
                                P.op("dve", (lambda e, j=j: e.scalar_tensor_tensor(out=gtt[j][:], in0=sgt[j][:], scalar=SIGC, in1=gtt[j][:], op0=ALU.min, op1=ALU.mult)),
                                     reads=["sgt%d" % j, "gtt%d" % j], writes=["gtt%d" % j])
                                P.op("dve", (lambda e, j=j, fc=fc, tsl=tsl: e.tensor_tensor(out=ACT_T[:, fc, tsl], in0=gtt[j][:], in1=upt[j][:], op=ALU.mult)),
                                     reads=["gtt%d" % j, "upt%d" % j], writes=["ACT_T%d" % fc])
                        for tt in range(NT):
                            tsl = slice(tt * 128, (tt + 1) * 128)
                            for h in range(2):
                                j = oc[0] % 2
                                oc[0] += 1
                                for fc in range(8):
                                    wd = WDr[(ex * 8 + fc) % NDR]
                                    P.op("pe", (lambda e, wd=wd, fc=fc, tsl=tsl, j=j, h=h: e.matmul(POm[j][:], lhsT=ACT_T[:, fc, tsl], rhs=wd[:, hsl(h)],
                                                                                                  start=(fc == 0), stop=(fc == 7))),
                                         reads=["ACT_T%d" % fc, "WD%d" % ((ex * 8 + fc) % NDR)], writes=["PSom%d" % j])
                                P.op("dve", (lambda e, j=j, tt=tt, h=h, ex=ex: e.scalar_tensor_tensor(out=ACCM[:, tt, hsl(h)], in0=POm[j][:], scalar=GW[:, tt, ex:ex + 1],
                                                                                                     in1=ACCM[:, tt, hsl(h)], op0=ALU.mult, op1=ALU.add)),
                                     reads=["PSom%d" % j, "GW", "ACCM%d" % tt], writes=["ACCM%d" % tt])
                    P.barrier()
                with contextlib.ExitStack() as s6:
                    xm6 = [mk(s6, "sb", [128, 1024], F32, "xm6") for _ in range(2)]
                    o6 = [mk(s6, "sb", [128, 1024], F32, "o6") for _ in range(2)]
                    for tt in range(NT):
                        i = tt % 2
                        P.dma("sp", (lambda e, i=i, tt=tt: e.dma_start(out=xm6[i][:], in_=xmid[b, tt * 128:(tt + 1) * 128, :])), reads=[], writes=["xm6%d" % i])
                        P.op("dve", (lambda e, i=i, tt=tt: e.tensor_tensor(out=o6[i][:], in0=ACCM[:, tt, :], in1=GT2[:], op=ALU.mult)),
                             reads=["ACCM%d" % tt, "GT2"], writes=["o6%d" % i])
                        P.op("pool", (lambda e, i=i: e.tensor_tensor(out=o6[i][:], in0=o6[i][:], in1=xm6[i][:], op=ALU.add)),
                             reads=["o6%d" % i, "xm6%d" % i], writes=["o6%d" % i])
                        P.dma("sp", (lambda e, i=i, tt=tt: e.dma_start(out=out[b, tt * 128:(tt + 1) * 128, :], in_=o6[i][:])), reads=["o6%d" % i], writes=[])
                    P.barrier()
            P.barrier()

        BASE = mk(gst, "sb", [128, NEXP], F32, "BASE")
        GWa = mk(gst, "sb", [128, NB * NT, NEXP], F32, "GWa")
        SLu = mk(gst, "sb", [128, NB * NT, 4], U32, "SLu")
        GKa = mk(gst, "sb", [128, NB * NT, 4], F32, "GKa")
        GT2a = [mk(gst, "sb", [128, 1024], F32, "GT2a") for _ in range(NB)]
        trib = mk(gst, "sb", [128, 128], BF16, "trib")
        onesb = mk(gst, "sb", [128, 128], BF16, "onesb")
        P.op("dve", lambda e: e.tensor_copy(out=trib[:], in_=C["tri"][:]), reads=["c_tri"], writes=["trib"])
        P.op("pool", lambda e: e.memset(onesb[:], 1.0), writes=["onesb"])
        P.op("dve", lambda e: e.tensor_copy(out=BASE[:], in_=C["ecap"][:]), reads=["c_ecap"], writes=["BASE"])
        ECMAX = mk(gst, "sb", [128, NEXP], F32, "ECMAX")
        P.op("dve", lambda e: e.tensor_scalar(out=ECMAX[:], in0=C["ecap"][:], scalar1=float(CAP - 1), scalar2=None, op0=ALU.add), reads=["c_ecap"], writes=["ECMAX"])
        P.barrier()

        def st4s(b):
            with contextlib.ExitStack() as st:
                GT1, G2, B2 = [mk(st, "sb", [128, 1024], F32, n) for n in ("GT1", "G2", "B2")]
                with contextlib.ExitStack() as st2:
                    for slot, dest, dn, mode, gv in ((2, GT1, "GT1", "plain", None), (4, G2, "G2", "scale", I["norm2_g"]),
                                                     (3, B2, "B2", "plain", None), (5, GT2a[b], "GT2a%d" % b, "plain", None)):
                        mods_piece(st2, b, slot, dest, dn, mode, gv)
                    P.barrier()
                with contextlib.ExitStack() as s4:
                    YR = mk(s4, "sb", [128, 4, S], BF16, "YR")
                    YM = mk(s4, "sb", [128, NT, 512], BF16, "YM")
                    WO = mk(s4, "sb", [128, 8, 1024], BF16, "WO")
                    WR = mk(s4, "sb", [128, 8, NEXP], F32, "WR")
                    BRb = mk(s4, "sb", [128, NEXP], F32, "BRb")
                    P.dma("sp", lambda e: e.dma_start(out=YR[:], in_=ymixT[b].rearrange("(c p) t -> p c t", p=128)), reads=[], writes=["YR"])
                    P.dma("sp", lambda e: e.dma_start(out=YM[:], in_=ymoba[b].rearrange("(t p) c -> p t c", p=128)), reads=[], writes=["YM"])
                    P.dma("pool", lambda e: e.dma_start(out=WO[:], in_=I["w_out"].rearrange("(k p) n -> p k n", p=128)), writes=["WO"])
                    P.dma("sp", lambda e: e.dma_start(out=WR[:], in_=I["w_router"].rearrange("(k p) n -> p k n", p=128)), writes=["WR"])
                    P.dma("sp", lambda e: e.dma_start(out=BRb[:], in_=I["b_router"].partition_broadcast(128)), writes=["BRb"])
                    nb2 = 2
                    xt1 = mk(s4, "sb", [128, 1024], F32, "xt4")
                    xm = [mk(s4, "sb", [128, 1024], F32, "xm4") for _ in range(nb2)]
                    h2f = [mk(s4, "sb", [128, 1024], F32, "h2f") for _ in range(nb2)]
                    h2b = [mk(s4, "sb", [128, 1024], BF16, "h2b") for _ in range(nb2)]
                    ss = [mk(s4, "sb", [128, 2], F32, "ss4") for _ in range(nb2)]
                    YMT = [mk(s4, "sb", [128, 4, 128], BF16, "YMT") for _ in range(nb2)]
                    h2Tf = mk(s4, "sb", [128, 8, 128], F32, "h2Tf")
                    LG = [mk(s4, "sb", [128, NEXP], F32, "LG") for _ in range(nb2)]
                    EX = [mk(s4, "sb", [128, NEXP], F32, "EX") for _ in range(nb2)]
                    MK = [mk(s4, "sb", [128, NEXP], F32, "MK") for _ in range(nb2)]
                    MKb = [mk(s4, "sb", [128, NEXP], BF16, "MKb") for _ in range(nb2)]
                    POSE = [mk(s4, "sb", [128, NEXP], F32, "POSE") for _ in range(nb2)]
                    TMP = [mk(s4, "sb", [128, NEXP], F32, "TMP") for _ in range(nb2)]
                    MX8 = [mk(s4, "sb", [128, 8], F32, "MX8") for _ in range(nb2)]
                    IX8 = [mk(s4, "sb", [128, 8], U32, "IX8") for _ in range(nb2)]
                    IXf = [mk(s4, "sb", [128, 8], F32, "IXf") for _ in range(nb2)]
                    SLf = [mk(s4, "sb", [128, 4], F32, "SLf") for _ in range(nb2)]
                    sm = [mk(s4, "sb", [128, 4], F32, "sm4") for _ in range(nb2)]
                    PO = [mk(s4, "ps", [128, 512], F32, "PSo4") for _ in range(2)]
                    PTm = mk(s4, "ps", [128, 8, 128], BF16, "PStm")
                    PTf = [mk(s4, "ps", [128, 4, 128], F32, "PStf") for _ in range(2)]
                    PL = mk(s4, "ps", [128, 512], F32, "PSl")
                    PC = mk(s4, "ps", [128, 512], F32, "PSc")
                    hsl = lambda h: slice(h * 512, (h + 1) * 512)
                    def phaseA(tt):
                        i = tt % nb2
                        gt = b * NT + tt
                        u = "s4_%d_" % i
                        tsl = slice(tt * 128, (tt + 1) * 128)
                        for c in range(4):
                            P.op("pe", (lambda e, c=c, tt=tt: e.transpose(PTm[:, c, :], YM[:, tt, c * 128:(c + 1) * 128], identb[:])),
                                 reads=["YM", "identb"], writes=["PStm"])
                        P.op("act", (lambda e, i=i: e.copy(out=YMT[i][:], in_=PTm[:, 0:4, :])), reads=["PStm"], writes=[u + "YMT"])
                        P.dma("sp", (lambda e, tt=tt: e.dma_start(out=xt1[:], in_=I["x"][b, tt * 128:(tt + 1) * 128, :])), writes=["s4_xt"])
                        for h in range(2):
                            for kc in range(8):
                                P.op("pe", (lambda e, i=i, h=h, kc=kc, tsl=tsl: e.matmul(
                                    PO[h][:], lhsT=(YR[:, kc, tsl] if kc < 4 else YMT[i][:, kc - 4, :]), rhs=WO[:, kc, hsl(h)],
                                    start=(kc == 0), stop=(kc == 7))), reads=["YR", u + "YMT", "WO"], writes=["PSo4%d" % h])
                            P.op("dve", (lambda e, i=i, h=h: e.tensor_tensor(out=xm[i][:, hsl(h)], in0=PO[h][:], in1=GT1[:, hsl(h)], op=ALU.mult)),
                                 reads=["PSo4%d" % h, "GT1"], writes=[u + "xm"])
                        P.op("dve", (lambda e, i=i: e.tensor_tensor(out=xm[i][:], in0=xm[i][:], in1=xt1[:], op=ALU.add)),
                             reads=[u + "xm", "s4_xt"], writes=[u + "xm"])
                        P.dma("sp", (lambda e, i=i, tt=tt: e.dma_start(out=xmid[b, tt * 128:(tt + 1) * 128, :], in_=xm[i][:])),
                              reads=[u + "xm"], writes=[])
                        P.op("act", (lambda e, i=i: e.activation(out=h2f[i][:], in_=xm[i][:], func=AF.Square, accum_out=ss[i][:, 0:1])),
                             reads=[u + "xm"], writes=[u + "h2f", u + "ss"])
                        P.op("dve", (lambda e, i=i: e.tensor_scalar(out=ss[i][:, 1:2], in0=ss[i][:, 0:1], scalar1=1.0 / D, scalar2=1e-6,
                                                                    op0=ALU.mult, op1=ALU.add)), reads=[u + "ss"], writes=[u + "ss"])
                        P.op("act", (lambda e, i=i: e.activation(out=ss[i][:, 1:2], in_=ss[i][:, 1:2], func=AF.Sqrt)), reads=[u + "ss"], writes=[u + "ss"])
                        P.op("dve", (lambda e, i=i: e.reciprocal(out=ss[i][:, 1:2], in_=ss[i][:, 1:2])), reads=[u + "ss"], writes=[u + "ss"])
                        P.op("dve", (lambda e, i=i: e.scalar_tensor_tensor(out=h2f[i][:], in0=xm[i][:], scalar=ss[i][:, 1:2], in1=G2[:],
                                                                           op0=ALU.mult, op1=ALU.mult)), reads=[u + "xm", u + "ss", "G2"], writes=[u + "h2f"])
                        P.op("dve", (lambda e, i=i: e.tensor_tensor(out=h2f[i][:], in0=h2f[i][:], in1=B2[:], op=ALU.add)),
                             reads=[u + "h2f", "B2"], writes=[u + "h2f"])
                        P.op("act", (lambda e, i=i: e.copy(out=h2b[i][:], in_=h2f[i][:])), reads=[u + "h2f"], writes=[u + "h2b"])
                        for k in range(8):
                            P.op("pe", (lambda e, i=i, k=k: e.transpose(PTf[k // 4][:, k % 4, :], h2f[i][:, k * 128:(k + 1) * 128], C["ident"][:])),
                                 reads=[u + "h2f", "c_ident"], writes=["PStf%d" % (k // 4)])
                        for g in range(2):
                            P.op("act", (lambda e, g=g: e.copy(out=h2Tf[:, g * 4:(g + 1) * 4, :], in_=PTf[g][:])),
                                 reads=["PStf%d" % g], writes=["s4_h2Tf"])
                        for k in range(8):
                            P.op("pe", (lambda e, k=k: e.matmul(PL[:, 0:NEXP], lhsT=h2Tf[:, k, :], rhs=WR[:, k, :], start=(k == 0), stop=(k == 7))),
                                 reads=["s4_h2Tf", "WR"], writes=["PSl"])
                        P.op("dve", (lambda e, i=i: e.tensor_tensor(out=LG[i][:], in0=PL[:, 0:NEXP], in1=BRb[:], op=ALU.add)),
                             reads=["PSl", "BRb"], writes=[u + "LG"])

                    def phaseB(tt):
                        i = tt % nb2
                        gt = b * NT + tt
                        u = "s4_%d_" % i
                        P.op("dve", (lambda e, i=i: e.max(out=MX8[i][:], in_=LG[i][:])), reads=[u + "LG"], writes=[u + "MX8"])
                        P.op("dve", (lambda e, i=i: e.max_index(out=IX8[i][:], in_max=MX8[i][:], in_values=LG[i][:])),
                             reads=[u + "LG", u + "MX8"], writes=[u + "IX8"])
                        P.op("dve", (lambda e, i=i: e.tensor_copy(out=IXf[i][:], in_=IX8[i][:])), reads=[u + "IX8"], writes=[u + "IXf"])
                        P.op("dve", (lambda e, i=i: e.tensor_scalar(out=sm[i][:, 0:1], in0=MX8[i][:, 0:1], scalar1=-1.0, scalar2=None, op0=ALU.mult)),
                             reads=[u + "MX8"], writes=[u + "sm"])
                        P.op("act", (lambda e, i=i: e.activation(out=EX[i][:], in_=LG[i][:], func=AF.Exp, bias=sm[i][:, 0:1])),
                             reads=[u + "LG", u + "sm"], writes=[u + "EX"])
                        P.op("dve", (lambda e, i=i: e.tensor_scalar(out=MK[i][:], in0=LG[i][:], scalar1=MX8[i][:, 3:4], scalar2=None, op0=ALU.is_ge)),
                             reads=[u + "LG", u + "MX8"], writes=[u + "MK"])
                        P.op("dve", (lambda e, i=i: e.tensor_copy(out=MKb[i][:], in_=MK[i][:])), reads=[u + "MK"], writes=[u + "MKb"])
                        P.op("dve", (lambda e, i=i: e.tensor_tensor(out=EX[i][:], in0=EX[i][:], in1=MK[i][:], op=ALU.mult)),
                             reads=[u + "EX", u + "MK"], writes=[u + "EX"])
                        P.op("dve", (lambda e, i=i: e.tensor_reduce(out=sm[i][:, 1:2], in_=EX[i][:], axis=AX.X, op=ALU.add)),
                             reads=[u + "EX"], writes=[u + "sm"])
                        P.op("dve", (lambda e, i=i: e.reciprocal(out=sm[i][:, 2:3], in_=sm[i][:, 1:2])), reads=[u + "sm"], writes=[u + "sm"])
                        P.op("dve", (lambda e, i=i, gt=gt: e.tensor_scalar(out=GWa[:, gt, :], in0=EX[i][:], scalar1=sm[i][:, 2:3], scalar2=None, op0=ALU.mult)),
                             reads=[u + "EX", u + "sm"], writes=["GWa"])
                        P.op("pe", (lambda e, i=i: e.matmul(PC[:, 0:NEXP], lhsT=trib[:], rhs=MKb[i][:], start=True, stop=True)),
                             reads=["trib", u + "MKb"], writes=["PSc"])
                        P.op("pe", (lambda e, i=i: e.matmul(PC[:, NEXP:2 * NEXP], lhsT=onesb[:], rhs=MKb[i][:], start=True, stop=True)),
                             reads=["onesb", u + "MKb"], writes=["PSc"])
                        P.op("dve", (lambda e, i=i: e.tensor_tensor(out=POSE[i][:], in0=PC[:, 0:NEXP], in1=BASE[:], op=ALU.add)),
                             reads=["PSc", "BASE"], writes=[u + "POSE"])
                        P.op("dve", lambda e: e.tensor_tensor(out=BASE[:], in0=PC[:, NEXP:2 * NEXP], in1=BASE[:], op=ALU.add),
                             reads=["PSc", "BASE"], writes=["BASE"])
                        P.op("dve", (lambda e, i=i: e.tensor_tensor(out=POSE[i][:], in0=POSE[i][:], in1=ECMAX[:], op=ALU.min)),
                             reads=[u + "POSE", "ECMAX"], writes=[u + "POSE"])
                        P.op("dve", (lambda e, i=i: e.memset(SLf[i][:], 0.0)), writes=[u + "SLf"])
                        P.op("dve", (lambda e, gt=gt: e.memset(GKa[:, gt, :], 0.0)), writes=["GKa"])
                        for k in range(4):
                            P.op("dve", (lambda e, i=i, k=k: e.scalar_tensor_tensor(out=TMP[i][:], in0=C["iota32"][:], scalar=IXf[i][:, k:k + 1], in1=POSE[i][:],
                                                                                    op0=ALU.is_equal, op1=ALU.mult, accum_out=SLf[i][:, k:k + 1])),
                                 reads=["c_iota32", u + "IXf", u + "POSE", u + "SLf"], writes=[u + "TMP", u + "SLf"])
                            P.op("dve", (lambda e, i=i, k=k, gt=gt: e.scalar_tensor_tensor(out=TMP[i][:], in0=C["iota32"][:], scalar=IXf[i][:, k:k + 1], in1=GWa[:, gt, :],
                                                                                           op0=ALU.is_equal, op1=ALU.mult, accum_out=GKa[:, gt, k:k + 1])),
                                 reads=["c_iota32", u + "IXf", "GWa", "GKa"], writes=[u + "TMP", "GKa"])
                        P.op("dve", (lambda e, i=i, gt=gt: e.tensor_copy(out=SLu[:, gt, :], in_=SLf[i][:])), reads=[u + "SLf"], writes=["SLu"])
                        for k in range(4):
                            P.dma("pool", (lambda e, i=i, k=k, gt=gt: e.indirect_dma_start(
                                out=XS, out_offset=bass.IndirectOffsetOnAxis(ap=SLu[:, gt, k:k + 1], axis=0), in_=h2b[i][:], in_offset=None)),
                                  reads=[u + "h2b", "SLu"], writes=[])
                        if debug and b == 0:
                            P.dma("sp", (lambda e, i=i, tt=tt: e.dma_start(out=dbg["d_xm"][tt * 128:(tt + 1) * 128, :], in_=xm[i][:])), reads=[u + "xm"])
                            P.dma("sp", (lambda e, gt=gt, tt=tt: e.dma_start(out=dbg["d_gw"][tt * 128:(tt + 1) * 128, :], in_=GWa[:, gt, :])), reads=["GWa"])

                    phaseA(0)
                    for tt in range(NT):
                        if tt + 1 < NT:
                            phaseA(tt + 1)
                        phaseB(tt)
                    P.barrier()
            P.barrier()

        def moe_sparse():
            with contextlib.ExitStack() as s5:
                XT = mk(s5, "sb", [128, 8, CAP], BF16, "XT")
                ACT_T = mk(s5, "sb", [128, 8, CAP], BF16, "ACT_T")
                NG, NDR = 3, 11
                WGUr = [mk(s5, "sb", [128, 8, 256], BF16, "WGUr") for _ in range(NG)]
                WDr = [mk(s5, "sb", [128, 1024], BF16, "WDr") for _ in range(NDR)]
                BGU = mk(s5, "sb", [128, NEXP, 16], F32, "BGU")
                BGS = mk(s5, "sb", [128, NEXP, 16], F32, "BGS")
                P.dma("sp", lambda e: e.dma_start(out=BGU[:], in_=I["bgu_l"]), writes=["BGU"])
                P.op("dve", lambda e: e.tensor_scalar(out=BGS[:], in0=BGU[:], scalar1=1.702, scalar2=None, op0=ALU.mult), reads=["BGU"], writes=["BGS"])
                BG1 = mk(s5, "sb", [128, NEXP, 16], F32, "BG1")
                P.op("dve", lambda e: e.tensor_scalar(out=BG1[:], in0=BGU[:], scalar1=1.0, scalar2=None, op0=ALU.add), reads=["BGU"], writes=["BG1"])
                XR = [mk(s5, "sb", [128, 1024], BF16, "XR") for _ in range(2)]
                YO = [mk(s5, "sb", [128, 1024], F32, "YO") for _ in range(2)]
                sgt = [mk(s5, "sb", [128, 512], F32, "sgt") for _ in range(2)]
                gtt = [mk(s5, "sb", [128, 512], F32, "gtt") for _ in range(2)]
                upt = [mk(s5, "sb", [128, 512], F32, "upt") for _ in range(2)]
                PGm = [mk(s5, "ps", [128, 512], F32, "PSgm") for _ in range(2)]
                PUm = [mk(s5, "ps", [128, 512], F32, "PSum") for _ in range(2)]
                POm = [mk(s5, "ps", [128, 512], F32, "PSom") for _ in range(2)]
                PTx = mk(s5, "ps", [128, 8, 128], BF16, "PStx")
                SIGC = 0.9999933243
                hsl = lambda h: slice(h * 512, (h + 1) * 512)
                uc = [0]
                oc = [0]
                xc = [0]
                nexp = t5.get("nexp", NEXP)
                NST = CAP // 128

                def load_piece(idx):
                    e_, fc = idx // 8, idx % 8
                    if e_ >= nexp:
                        return
                    P.dma("pool", (lambda e, e_=e_, fc=fc, idx=idx: e.dma_start(out=WGUr[idx % NG][:], in_=I["wgu_l"][e_, fc])),
                          writes=["WGU%d" % (idx % NG)])
                    P.dma("pool", (lambda e, e_=e_, fc=fc, idx=idx: e.dma_start(out=WDr[idx % NDR][:], in_=I["w_down"][e_, fc * 128:(fc + 1) * 128, :])),
                          writes=["WD%d" % (idx % NDR)])
                LOOK = 2
                for idx in range(LOOK):
                    load_piece(idx)
                def build_xt(ex, st_):
                    i = xc[0] % 2
                    xc[0] += 1
                    r0 = ex * CAP + st_ * 128
                    P.dma("sp", (lambda e, i=i, r0=r0: e.dma_start(out=XR[i][:], in_=XS[r0:r0 + 128, :])), reads=[], writes=["XR%d" % i])
                    for k in range(8):
                        P.op("pe", (lambda e, i=i, k=k: e.transpose(PTx[:, k, :], XR[i][:, k * 128:(k + 1) * 128], identb[:])),
                             reads=["XR%d" % i, "identb"], writes=["PStx"])
                    P.op("act", (lambda e, st_=st_: e.copy(out=XT[:, :, st_ * 128:(st_ + 1) * 128], in_=PTx[:])), reads=["PStx"], writes=["XT"])

                for st_ in range(NST):
                    build_xt(0, st_)
                for ex in range(nexp):
                    for fc in range(8):
                        idx = ex * 8 + fc
                        load_piece(idx + LOOK)
                        wg = WGUr[idx % NG]
                        wn = "WGU%d" % (idx % NG)
                        for tg in range(CAP // 512):
                            j = uc[0] % 2
                            uc[0] += 1
                            tsl = slice(tg * 512, (tg + 1) * 512)
                            for k in range(8):
                                P.op("pe", (lambda e, wg=wg, k=k, tsl=tsl, j=j: e.matmul(PGm[j][:], lhsT=wg[:, k, 0:128], rhs=XT[:, k, tsl],
                                                                                       start=(k == 0), stop=(k == 7))),
                                     reads=[wn, "XT"], writes=["PSgm%d" % j])
                            for k in range(8):
                                P.op("pe", (lambda e, wg=wg, k=k, tsl=tsl, j=j: e.matmul(PUm[j][:], lhsT=wg[:, k, 128:256], rhs=XT[:, k, tsl],
                                                                                       start=(k == 0), stop=(k == 7))),
                                     reads=[wn, "XT"], writes=["PSum%d" % j])
                            bg = BGU[:, ex, fc:fc + 1]
                            bu = BG1[:, ex, 8 + fc:9 + fc]
                            bs = BGS[:, ex, fc:fc + 1]
                            P.op("act", (lambda e, j=j, bs=bs: e.activation(out=sgt[j][:], in_=PGm[j][:], func=AF.Sigmoid, bias=bs, scale=1.702)),
                                 reads=["PSgm%d" % j, "BGS"], writes=["sgt%d" % j])
                            P.op("dve", (lambda e, j=j, bg=bg: e.tensor_scalar(out=gtt[j][:], in0=PGm[j][:], scalar1=bg, scalar2=7.0, op0=ALU.add, op1=ALU.min)),
                                 reads=["PSgm%d" % j, "BGU"], writes=["gtt%d" % j])
                            P.op("act", (lambda e, j=j, bu=bu: e.activation(out=upt[j][:], in_=PUm[j][:], func=AF.Identity, bias=bu)),
                                 reads=["PSum%d" % j, "BG1"], writes=["upt%d" % j])
                            P.op("dve", (lambda e, j=j: e.tensor_scalar(out=upt[j][:], in0=upt[j][:], scalar1=-6.0, scalar2=8.0, op0=ALU.max, op1=ALU.min)),
                                 reads=["upt%d" % j], writes=["upt%d" % j])
                            P.op("dve", (lambda e, j=j: e.scalar_tensor_tensor(out=gtt[j][:], in0=sgt[j][:], scalar=SIGC, in1=gtt[j][:], op0=ALU.min, op1=ALU.mult)),
                                 reads=["sgt%d" % j, "gtt%d" % j], writes=["gtt%d" % j])
                            P.op("dve", (lambda e, j=j, fc=fc, tsl=tsl: e.tensor_tensor(out=ACT_T[:, fc, tsl], in0=gtt[j][:], in1=upt[j][:], op=ALU.mult)),
                                 reads=["gtt%d" % j, "upt%d" % j], writes=["ACT_T%d" % fc])
                    for st_ in range(NST):
                        tsl = slice(st_ * 128, (st_ + 1) * 128)
                        yi = st_ % 2
                        for h in range(2):
                            j = oc[0] % 2
                            oc[0] += 1
                            for fc in range(8):
                                wd = WDr[(ex * 8 + fc) % NDR]
                                P.op("pe", (lambda e, wd=wd, fc=fc, tsl=tsl, j=j, h=h: e.matmul(POm[j][:], lhsT=ACT_T[:, fc, tsl], rhs=wd[:, hsl(h)],
                                                                                              start=(fc == 0), stop=(fc == 7))),
                                     reads=["ACT_T%d" % fc, "WD%d" % ((ex * 8 + fc) % NDR)], writes=["PSom%d" % j])
                            P.op("act", (lambda e, j=j, h=h, yi=yi: e.copy(out=YO[yi][:, hsl(h)], in_=POm[j][:])), reads=["PSom%d" % j], writes=["YO%d" % yi])
                        r0 = ex * CAP + st_ * 128
                        P.dma("act", (lambda e, yi=yi, r0=r0: e.dma_start(out=YS[r0:r0 + 128, :], in_=YO[yi][:])), reads=["YO%d" % yi], writes=[])
                        if ex + 1 < nexp:
                            build_xt(ex + 1, st_)
                P.barrier()

        def final_sparse():
            with contextlib.ExitStack() as s6:
                BD = mk(s6, "sb", [32, 1024], F32, "BD")
                P.dma("sp", lambda e: e.dma_start(out=BD[:], in_=I["b_down"]), writes=["BD"])
                gTs = [mk(s6, "sb", [32, 128], F32, "gTs") for _ in range(2)]
                acc = [mk(s6, "sb", [128, 1024], F32, "acc6") for _ in range(2)]
                GB = [mk(s6, "sb", [128, 1024], F32, "GB6") for _ in range(8)]
                xm6 = [mk(s6, "sb", [128, 1024], F32, "xm6") for _ in range(2)]
                PGt_ = mk(s6, "ps", [128, 512], F32, "PSgt")
                PO = [mk(s6, "ps", [128, 512], F32, "PSo6") for _ in range(2)]
                hsl = lambda h: slice(h * 512, (h + 1) * 512)
                gc_ = [0]
                for gt in range(NB * NT):
                    b, tt = gt // NT, gt % NT
                    i = gt % 2
                    P.op("pe", (lambda e, gt=gt: e.transpose(PGt_[0:32, 0:128], GWa[:, gt, :], C["ident"][:])), reads=["GWa", "c_ident"], writes=["PSgt"])
                    P.op("act", (lambda e, i=i: e.copy(out=gTs[i][:], in_=PGt_[0:32, 0:128])), reads=["PSgt"], writes=["gTs%d" % i])
                    for h in range(2):
                        P.op("pe", (lambda e, i=i, h=h: e.matmul(PO[h][:], lhsT=gTs[i][:], rhs=BD[:, hsl(h)], start=True, stop=True)),
                             reads=["gTs%d" % i, "BD"], writes=["PSo6%d" % h])
                        P.op("act", (lambda e, i=i, h=h: e.copy(out=acc[i][:, hsl(h)], in_=PO[h][:])), reads=["PSo6%d" % h], writes=["acc%d" % i])
                    P.dma("sp", (lambda e, i=i, b=b, tt=tt: e.dma_start(out=xm6[i][:], in_=xmid[b, tt * 128:(tt + 1) * 128, :])), reads=[], writes=["xm6%d" % i])
                    for k in range(4):
                        g = gc_[0] % 8
                        gc_[0] += 1
                        P.dma("pool", (lambda e, g=g, gt=gt, k=k: e.indirect_dma_start(
                            out=GB[g][:], out_offset=None, in_=YS, in_offset=bass.IndirectOffsetOnAxis(ap=SLu[:, gt, k:k + 1], axis=0))),
                              reads=["SLu"], writes=["GB%d" % g])
                        P.op("dve", (lambda e, g=g, i=i, gt=gt, k=k: e.scalar_tensor_tensor(out=acc[i][:], in0=GB[g][:], scalar=GKa[:, gt, k:k + 1], in1=acc[i][:],
                                                                                            op0=ALU.mult, op1=ALU.add)),
                             reads=["GB%d" % g, "GKa", "acc%d" % i], writes=["acc%d" % i])
                    P.op("dve", (lambda e, i=i, b=b: e.tensor_tensor(out=acc[i][:], in0=acc[i][:], in1=GT2a[b][:], op=ALU.mult)),
                         reads=["acc%d" % i, "GT2a%d" % b], writes=["acc%d" % i])
                    P.op("dve", (lambda e, i=i: e.tensor_tensor(out=acc[i][:], in0=acc[i][:], in1=xm6[i][:], op=ALU.add)),
                         reads=["acc%d" % i, "xm6%d" % i], writes=["acc%d" % i])
                    P.dma("act", (lambda e, i=i, b=b, tt=tt: e.dma_start(out=out[b, tt * 128:(tt + 1) * 128, :], in_=acc[i][:])), reads=["acc%d" % i], writes=[])
                P.barrier()

        def st1(b):
            with contextlib.ExitStack() as st:
                G1 = mk(st, "sb", [128, 1024], F32, "G1")
                B1 = mk(st, "sb", [128, 1024], F32, "B1")
                with contextlib.ExitStack() as st2:
                    mods_piece(st2, b, 1, G1, "G1", "scale", I["norm1_g"])
                    mods_piece(st2, b, 0, B1, "B1", "plain")
                    P.barrier()
                hT = mk(st, "sb", [128, 8, S + 1], BF16, "hT")
                P.op("pool", lambda e: e.memset(hT[:, :, 0:1], 0.0), writes=["hT0"])
                if True:
                    bufs = [(mk(st, "sb", [128, 1024], F32, "xt"), mk(st, "sb", [128, 1024], BF16, "junk"),
                             mk(st, "sb", [128, 2], F32, "ss"), mk(st, "sb", [128, 1024], F32, "t1")) for _ in range(2)]
                    hbs = [mk(st, "sb", [128, 1024], BF16, "hb") for _ in range(2)]
                    pts = [mk(st, "ps", [128, 8, 128], BF16, "pt") for _ in range(2)]
                    for tt in range(NT):
                        i = tt % 2
                        tag = "n1_%d_" % i
                        norm_tile(bufs[i], I["x"][b, tt * 128:(tt + 1) * 128, :], G1, B1, "G1", "B1", tag, (hbs[i], tag + "hb"))
                        for k in range(8):
                            P.op("pe", (lambda e, i=i, k=k: e.transpose(pts[i][:, k, :], hbs[i][:, k * 128:(k + 1) * 128], identb[:])),
                                 reads=[tag + "hb", "identb"], writes=[tag + "pt"])
                        P.op("act", (lambda e, i=i, tt=tt: e.copy(out=hT[:, :, 1 + tt * 128:1 + (tt + 1) * 128], in_=pts[i][:])),
                             reads=[tag + "pt"], writes=["hT_%d" % (tt // 4)])
                with contextlib.ExitStack() as st2:
                    mub = mk(st2, "sb", [128, 1920], F32, "mub")
                    P.dma("sp", lambda e: e.dma_start(out=mub[:], in_=I["mu_p"].partition_broadcast(128)), writes=["mub"])
                    wf = [mk(st2, "sb", [128, 8, 128], F32, "wf") for _ in range(2)]
                    w1 = [mk(st2, "sb", [128, 8, 128], BF16, "w1") for _ in range(2)]
                    w2 = [mk(st2, "sb", [128, 8, 128], BF16, "w2") for _ in range(2)]
                    stg = [mk(st2, "sb", [128, S], F32, "stg") for _ in range(2)]
                    pps = [mk(st2, "ps", [128, 512], F32, "pps") for _ in range(4)]
                    for cc in range(27):
                        i = cc % 2
                        tg_ = "pj%d_" % i
                        rw = cc < 15
                        if rw:
                            P.dma("sp", (lambda e, i=i, cc=cc: e.dma_start(out=wf[i][:], in_=I["win_l"][cc])), writes=[tg_ + "wf"])
                            P.op("pool", (lambda e, i=i, cc=cc: e.tensor_tensor(
                                out=w2[i][:], in0=wf[i][:], in1=mub[:, cc * 128:(cc + 1) * 128].unsqueeze(1).to_broadcast([128, 8, 128]),
                                op=ALU.mult)), reads=[tg_ + "wf", "mub"], writes=[tg_ + "w2"])
                            P.op("dve", (lambda e, i=i: e.tensor_tensor(out=w1[i][:], in0=wf[i][:], in1=w2[i][:], op=ALU.subtract)),
                                 reads=[tg_ + "wf", tg_ + "w2"], writes=[tg_ + "w1"])
                        else:
                            P.dma("pool", (lambda e, i=i, cc=cc: e.dma_start(out=w1[i][:], in_=I["win_l"][cc])), writes=[tg_ + "w1"])
                        for tg in range(4):
                            pi_ = (cc * 4 + tg) % 4
                            nmm = 16 if rw else 8
                            for k in range(8):
                                P.op("pe", (lambda e, i=i, k=k, tg=tg, pi_=pi_, nmm=nmm: e.matmul(
                                    pps[pi_][:], lhsT=w1[i][:, k, :], rhs=hT[:, k, 1 + tg * 512:1 + (tg + 1) * 512],
                                    start=(k == 0), stop=(k == 7 and nmm == 8))),
                                     reads=[tg_ + "w1", "hT_%d" % tg], writes=["pps%d" % pi_])
                            if rw:
                                rd = ["hT_%d" % tg, "hT0"] + (["hT_%d" % (tg - 1)] if tg > 0 else [])
                                for k in range(8):
                                    P.op("pe", (lambda e, i=i, k=k, tg=tg, pi_=pi_: e.matmul(
                                        pps[pi_][:], lhsT=w2[i][:, k, :], rhs=hT[:, k, tg * 512:(tg + 1) * 512],
                                        start=False, stop=(k == 7))),
                                         reads=[tg_ + "w2"] + rd, writes=["pps%d" % pi_])
                            P.op("act", (lambda e, i=i, tg=tg, pi_=pi_: e.copy(out=stg[i][:, tg * 512:(tg + 1) * 512], in_=pps[pi_][:])),
                                 reads=["pps%d" % pi_], writes=[tg_ + "stg"])
                        P.dma("act", (lambda e, i=i, cc=cc: e.dma_start(out=projF[b, cc * 128:(cc + 1) * 128, :], in_=stg[i][:])),
                              reads=[tg_ + "stg"], writes=[])
                        if debug and b == 0:
                            P.dma("sp", (lambda e, i=i, cc=cc: e.dma_start(out=dbg["d_proj"][cc * 128:(cc + 1) * 128, :], in_=stg[i][:])),
                                  reads=[tg_ + "stg"])
                    P.barrier()
        if t2:
            st2_rwkv(0)
        if t3only:
            st3_moba(0)
        for b in range(NB if not (t2 or t3only) else 0):
            st1(b)
            if stages <= 1:
                continue
            st2_rwkv(b)
            if stages <= 2:
                continue
            st3_moba(b)
            if stages <= 3:
                continue
            if SPARSE:
                st4s(b)
            else:
                st456(b)
        if SPARSE and not (t2 or t3only) and stages > 4:
            moe_sparse()
            final_sparse()
        P.barrier()
        global LAST_NOPS
        LAST_NOPS = P.nops
        P.emit()
    return nc


def host_layout(inputs):
    f = lambda k: np.ascontiguousarray(np.asarray(inputs[k], dtype=np.float32)[0])
    x = np.asarray(inputs["x"], dtype=np.float32)
    c = np.asarray(inputs["c"], dtype=np.float32)
    w_in = f("w_in")
    win_p = np.zeros((D, 27 * 128), np.float32)
    win_p[:, 0:1824] = w_in[:, 0:1824]
    win_p[:, 1920:1920 + 1536] = w_in[:, 1824:3360]
    win_l = np.ascontiguousarray(win_p.reshape(8, 128, 27, 128).transpose(2, 1, 0, 3))
    mu_p = np.zeros((1920,), np.float32)
    mu_p[0:1824] = f("rwkv_mu")
    names = ["rwkv_w0", "rwkv_a0", "rwkv_k_k", "rwkv_k_a", "rwkv_r_k", "rwkv_ln_g", "rwkv_ln_b"]
    pvec = np.stack([f(n).reshape(4, 128) for n in names], axis=-1)
    pvec = np.ascontiguousarray(pvec.transpose(1, 0, 2))
    qkg = np.stack([np.tile(f("q_norm_g"), 2), np.tile(f("k_norm_g"), 2)], axis=-1)
    wgu = f("w_gate_up")
    wg = wgu[:, :, :1024].reshape(NEXP, 8, 128, 8, 128)
    wu = wgu[:, :, 1024:].reshape(NEXP, 8, 128, 8, 128)
    wgu_l = np.ascontiguousarray(np.concatenate([wg, wu], axis=-1).transpose(0, 3, 2, 1, 4))
    bgu = f("b_gate_up")
    bgu_l = np.ascontiguousarray(bgu.reshape(NEXP, 16, 128).transpose(2, 0, 1))
    shared = {
        "w_ada": f("w_ada"), "b_ada": f("b_ada"), "norm1_g": f("norm1_g"), "win_l": win_l, "mu_p": mu_p, "pvec": pvec,
        "w_up": f("rwkv_w_up"), "a_up": f("rwkv_a_up"), "g_up": f("rwkv_g_up"), "qkg": np.ascontiguousarray(qkg),
        "w_out": f("w_out"), "norm2_g": f("norm2_g"), "w_router": f("w_router"), "b_router": f("b_router"),
        "wgu_l": wgu_l, "bgu_l": bgu_l, "w_down": f("w_down"), "b_down": f("b_down"),
    }
    for k, v in host_consts().items():
        shared["c_" + k] = v
    in_maps = []
    for i in range(NCORES):
        m = dict(shared)
        m["x"] = np.ascontiguousarray(x[i * NB:(i + 1) * NB])
        cc = c[i * NB:(i + 1) * NB]
        m["cT"] = np.ascontiguousarray(cc.reshape(NB, 8, 128).transpose(0, 2, 1))
        in_maps.append(m)
    return in_maps


def kernel(**inputs):
    in_maps = host_layout(inputs)
    nc = build_program()
    res = run_bass_kernel_spmd(nc, in_maps, core_ids=list(range(NCORES)))
    return np.concatenate([r["out"] for r in res.results], axis=0)
```

```python
import contextlib
import numpy as np
import concourse.bass as bass
import concourse.mybir as mybir
from concourse.bass_utils import run_bass_kernel_spmd

F32 = mybir.dt.float32
BF16 = mybir.dt.bfloat16
ALU = mybir.AluOpType
AF = mybir.ActivationFunctionType
AX = mybir.AxisListType

NCORES = 8
NB = 2
S = 2048
D = 1024
NT = S // 128
CH = 32
NCH = 128 // CH
KLV = CH.bit_length() - 1
NEXP = 32
CAP = 2048
SPARSE = True
WARM = 0
U32 = mybir.dt.uint32
LAST_NOPS = 0


class Prog:
    ROT = 12000
    NDMA = 24
    SAME_ENGINE_SYNC = True

    def __init__(self, nc, stack):
        self.nc = nc
        self.stack = stack
        self.ops = {e: [] for e in ("pe", "dve", "act", "pool", "sp")}
        self.sems = []
        self.cur_sem = {}
        self.cnt = {}
        self.allsems = {e: [] for e in ("pe", "dve", "act", "pool")}
        for e in ("pe", "dve", "act", "pool"):
            self.cur_sem[e] = self._new_sem(e)
            self.allsems[e].append(self.cur_sem[e])
            self.cnt[e] = 0
        self.dma_sems = [self._new_sem("dma%d" % i) for i in range(self.NDMA)]
        self.dma_cnt = [0] * self.NDMA
        self.dma_rr = 0
        self.dma_rr_sw = 0
        self.last_write = {}
        self.readers = {}
        self.waited = {e: {} for e in self.ops}
        self.nops = 0

    def _new_sem(self, name):
        s = self.stack.enter_context(self.nc.semaphore("s_%s_%d" % (name, len(self.sems))))
        self.sems.append(s)
        return len(self.sems) - 1

    LIMIT = None

    def _add(self, eng, fn, reads, writes, dma):
        if Prog.LIMIT is not None and self.nops >= Prog.LIMIT:
            return None
        waits = []
        deps = []
        for t in reads:
            if t in self.last_write:
                deps.append(self.last_write[t])
        for t in writes:
            if t in self.last_write:
                deps.append(self.last_write[t])
            deps.extend(self.readers.get(t, ()))
        if dma:
            if eng == "pool":
                s = self.dma_rr_sw % 8
                self.dma_rr_sw += 1
            else:
                s = 8 + self.dma_rr % (self.NDMA - 8)
                self.dma_rr += 1
            if self.dma_cnt[s] > 0:
                deps.append((self.dma_sems[s], self.dma_cnt[s] * 16, "dma"))
            self.dma_cnt[s] += 1
            ticket = (self.dma_sems[s], self.dma_cnt[s] * 16, "dma")
            inc = (self.dma_sems[s], 16)
        else:
            if self.cnt[eng] >= self.ROT:
                self.cur_sem[eng] = self._new_sem(eng)
                self.allsems[eng].append(self.cur_sem[eng])
                self.cnt[eng] = 0
            self.cnt[eng] += 1
            ticket = (self.cur_sem[eng], self.cnt[eng], eng)
            inc = (self.cur_sem[eng], 1)
        w = self.waited[eng]
        for (sem, val, src) in deps:
            if src == eng and not dma:
                if eng == "pe" or not self.SAME_ENGINE_SYNC:
                    continue
            if w.get(sem, 0) >= val:
                continue
            w[sem] = val
            waits.append((sem, val))
        self.ops[eng].append((waits, fn, inc))
        self.nops += 1
        for t in reads:
            self.readers.setdefault(t, []).append(ticket)
        for t in writes:
            self.last_write[t] = ticket
            self.readers[t] = []
        return ticket

    @staticmethod
    def _is_psum(t):
        return isinstance(t, str) and (t.startswith(("bk", "pps", "PS")) or t.endswith("pt") or "pp" in t)

    def op(self, eng, fn, reads=(), writes=()):
        reads = list(reads)
        writes = list(writes)
        for t in list(reads):
            if self._is_psum(t):
                reads.remove(t)
                if t not in writes:
                    writes.append(t)
        return self._add(eng, fn, reads, writes, False)

    def dma(self, eng, fn, reads=(), writes=()):
        return self._add(eng, fn, list(reads), list(writes), True)

    def barrier(self):
        allw = []
        for e in ("pe", "dve", "act", "pool"):
            if self.cnt[e] > 0:
                allw.append((self.cur_sem[e], self.cnt[e]))
        for s in range(self.NDMA):
            if self.dma_cnt[s] > 0:
                allw.append((self.dma_sems[s], self.dma_cnt[s] * 16))
        for e in self.ops:
            w = self.waited[e]
            waits = []
            for (sem, val) in allw:
                if w.get(sem, 0) >= val:
                    continue
                w[sem] = val
                waits.append((sem, val))
            if waits:
                self.ops[e].append((waits, None, None))
        self.last_write = {}
        self.readers = {}

    def emit(self):
        nc = self.nc
        sems = self.sems

        def run(engname, eng):
            for (waits, fn, inc) in self.ops[engname]:
                for (s, v) in waits:
                    eng.wait_ge(sems[s], v)
                if fn is None:
                    continue
                ins = fn(eng)
                ins.then_inc(sems[inc[0]], inc[1])

        with nc.Block() as block:
            @block.tensor
            def _(e):
                run("pe", e)

            @block.vector
            def _(e):
                run("dve", e)

            @block.scalar
            def _(e):
                run("act", e)

            @block.gpsimd
            def _(e):
                run("pool", e)

            @block.sync
            def _(e):
                run("sp", e)


def host_consts():
    c = {}
    c["ident"] = np.eye(128, dtype=np.float32)
    t = np.arange(128)
    same = (t[:, None] // CH) == (t[None, :] // CH)
    mS = (same & (t[:, None] < t[None, :])).astype(np.float32)
    mI = (same & (t[:, None] <= t[None, :])).astype(np.float32)
    c["mask_si"] = np.concatenate([mS, mI], axis=1)
    c["mask_l"] = np.ascontiguousarray(mS.T)
    cm = np.zeros((128, NCH, 64), np.float32)
    for n in range(NCH):
        cm[n * CH:(n + 1) * CH, n, :] = 1.0
    c["cmask"] = cm.reshape(128, NCH * 64)
    rm = np.ones((128, S), np.float32)
    rm[:, ::CH] = 0.0
    c["resetm"] = rm
    bo = np.zeros((128, 128), np.float32)
    bo[:64, :64] = 1.0
    bo[64:, 64:] = 1.0
    c["blkones"] = bo
    slopes = 2.0 ** (-8.0 * (np.arange(8) + 1.0) / 8.0)
    al = np.zeros((128, 8, 16), np.float32)
    for h in range(8):
        for dl in range(16):
            al[:, h, dl] = slopes[h] * (-dl * 128.0 + np.arange(128))
    c["alibi"] = al.reshape(128, 128)
    c["causal"] = (t[:, None] <= t[None, :]).astype(np.float32)
    pos = np.arange(S)
    alk = np.zeros((8, 3, S), np.float32)
    alq = np.zeros((8, 3, S), np.float32)
    for h in range(8):
        alk[h, 0] = slopes[h] * (pos % 128); alq[h, 0] = 1.0
        alk[h, 1] = 1.0;                      alq[h, 1] = -slopes[h] * 128.0 * (pos // 128)
        alk[h, 2] = slopes[h] * 128.0 * (pos // 128); alq[h, 2] = 1.0
    c["alk"] = alk.reshape(24, S)
    c["alq"] = alq.reshape(24, S)
    pm = np.zeros((128, 8, 8), np.float32)
    pi = np.zeros((128, 8, 8), np.float32)
    for j in range(8):
        pm[:, j, j:] = -1e30
        pi[:, j, :j] = 1.0
    c["tri"] = (t[:, None] < t[None, :]).astype(np.float32)
    c["iota32"] = np.tile(np.arange(32, dtype=np.float32), (128, 1))
    c["ecap"] = np.tile(np.arange(32, dtype=np.float32) * CAP, (128, 1))
    c["pastm"] = pm.reshape(128, 64)
    c["pasti"] = pi.reshape(128, 64)
    return c


CONST_SHAPES = {"ident": [128, 128], "mask_si": [128, 256], "mask_l": [128, 128], "cmask": [128, NCH * 64],
                "resetm": [128, S], "blkones": [128, 128], "alibi": [128, 128], "causal": [128, 128],
                "pastm": [128, 64], "pasti": [128, 64], "tri": [128, 128], "iota32": [128, 32], "ecap": [128, 32], "alk": [24, S], "alq": [24, S]}


IN_SHAPES = {
    "x": [NB, S, D], "cT": [NB, 128, 8], "w_ada": [D, 6 * D], "b_ada": [6 * D], "norm1_g": [D],
    "win_l": [27, 128, 8, 128], "mu_p": [1920], "pvec": [128, 4, 7],
    "w_up": [64, 512], "a_up": [64, 512], "g_up": [160, 512],
    "qkg": [128, 2], "w_out": [D, D], "norm2_g": [D], "w_router": [D, NEXP], "b_router": [NEXP],
    "wgu_l": [NEXP, 8, 128, 8, 256], "bgu_l": [128, NEXP, 16], "w_down": [NEXP, D, D], "b_down": [NEXP, D],
}


def build_program(stages=99, debug=False, small=False, t2=None, t3=None, t5=None):
    nc = bass.Bass("TRN2", target_bir_lowering=False)
    I = {}
    for k, shp in IN_SHAPES.items():
        if small and k in ("wgu_l", "w_down"):
            shp = [1] + list(shp[1:])
        I[k] = nc.dram_tensor(k, shp, F32, kind="ExternalInput").ap()
    for k, shp in CONST_SHAPES.items():
        I[k] = nc.dram_tensor("c_" + k, shp, F32, kind="ExternalInput").ap()
    out = nc.dram_tensor("out", [NB, S, D], F32, kind="ExternalOutput").ap()
    projF = nc.dram_tensor("projF", [NB, 27 * 128, S], F32, kind=("ExternalInput" if (t2 or t3 is not None) else "Internal")).ap()
    t3only = t3 is not None
    t2 = t2 or {}
    t3 = t3 or {}
    t5 = t5 or {}
    T2_NHP = t2.get("nhp", 4); T2_NBLK = t2.get("nblk", S // 128); T2_UPTO = t2.get("upto", 99)
    ymixT = nc.dram_tensor("ymixT", [NB, 512, S], BF16, kind="Internal").ap()
    ymoba = nc.dram_tensor("ymoba", [NB, S, 512], BF16, kind="Internal").ap()
    xmid = nc.dram_tensor("xmid", [NB, S, D], F32, kind="Internal").ap()
    XS = nc.dram_tensor("XS", [NEXP * CAP, D], BF16, kind="Internal").ap()
    YS = nc.dram_tensor("YS", [NEXP * CAP, D], F32, kind="Internal").ap()
    dbg = {}
    if debug:
        dbg["d_proj"] = nc.dram_tensor("d_proj", [27 * 128, S], F32, kind="ExternalOutput").ap()
        dbg["d_yr"] = nc.dram_tensor("d_yr", [512, S], F32, kind="ExternalOutput").ap()
        dbg["d_ym"] = nc.dram_tensor("d_ym", [S, 512], BF16, kind="ExternalOutput").ap()
        dbg["d_xm"] = nc.dram_tensor("d_xm", [S, D], F32, kind="ExternalOutput").ap()
        dbg["d_gw"] = nc.dram_tensor("d_gw", [S, NEXP], F32, kind="ExternalOutput").ap()
        dbg["d_yraw"] = nc.dram_tensor("d_yraw", [512, S], F32, kind="ExternalOutput").ap()

    with contextlib.ExitStack() as gst:
        P = Prog(nc, gst)
        uid = [0]

        def mk(st, space, shape, dt, name=None):
            uid[0] += 1
            nm = "%s_%d" % (name or "t", uid[0])
            if space == "sb":
                return st.enter_context(nc.sbuf_tensor(nm, shape, dt))
            return st.enter_context(nc.psum_tensor(nm, shape, dt))

        C = {}
        for k, shp in CONST_SHAPES.items():
            if k in ("resetm", "alk", "alq"):
                continue
            C[k] = mk(gst, "sb", shp, F32, "c" + k)
            P.dma("sp", (lambda e, k=k: e.dma_start(out=C[k][:], in_=I[k])), writes=["c_" + k])
        identb = mk(gst, "sb", [128, 128], BF16, "identb")
        P.op("dve", lambda e: e.tensor_copy(out=identb[:], in_=C["ident"][:]), reads=["c_ident"], writes=["identb"])
        msi_b = mk(gst, "sb", [128, 256], BF16, "msib")
        P.op("dve", lambda e: e.tensor_copy(out=msi_b[:], in_=C["mask_si"][:]), reads=["c_mask_si"], writes=["msib"])
        blk_b = mk(gst, "sb", [128, 128], BF16, "blkb")
        P.op("dve", lambda e: e.tensor_copy(out=blk_b[:], in_=C["blkones"][:]), reads=["c_blkones"], writes=["blkb"])
        pvec = mk(gst, "sb", [128, 4, 7], F32, "pvec")
        P.dma("sp", lambda e: e.dma_start(out=pvec[:], in_=I["pvec"]), writes=["pvec"])
        qkg = mk(gst, "sb", [128, 2], F32, "qkg")
        P.dma("sp", lambda e: e.dma_start(out=qkg[:], in_=I["qkg"]), writes=["qkg"])
        P.barrier()

        def mods_piece(st, b, slot, dest, destname, mode, gvec=None):
            cb = mk(st, "sb", [128, 8], F32, "cb")
            cbs = mk(st, "sb", [128, 8], F32, "cbs")
            CB = mk(st, "sb", [128, 8, 128], BF16, "CB")
            wp = [mk(st, "sb", [128, 8, 512], BF16, "wadap") for _ in range(2)]
            bb = [mk(st, "sb", [128, 512], F32, "badab") for _ in range(2)]
            pp = [mk(st, "ps", [128, 512], F32, "modps") for _ in range(2)]
            u = "m%d_%d_" % (b, slot)
            P.dma("sp", lambda e: e.dma_start(out=cb[:], in_=I["cT"][b]), writes=[u + "cb"])
            P.op("act", lambda e: e.activation(out=cbs[:], in_=cb[:], func=AF.Silu), reads=[u + "cb"], writes=[u + "cbs"])
            P.op("dve", lambda e: e.tensor_copy(out=CB[:], in_=cbs[:].unsqueeze(2).to_broadcast([128, 8, 128])),
                 reads=[u + "cbs"], writes=[u + "CB"])
            gb = None
            if mode == "scale":
                gb = mk(st, "sb", [128, 1024], F32, "gvecb")
                P.dma("sp", lambda e: e.dma_start(out=gb[:], in_=gvec.partition_broadcast(128)), writes=[u + "gb"])
            wv = I["w_ada"].rearrange("(k p) n -> p k n", p=128)
            for half in range(2):
                c0 = slot * 1024 + half * 512
                P.dma("pool", (lambda e, half=half, c0=c0: e.dma_start(out=wp[half][:], in_=wv[:, :, c0:c0 + 512])),
                      writes=[u + "wp%d" % half])
                P.dma("sp", (lambda e, half=half, c0=c0: e.dma_start(out=bb[half][:],
                                                                      in_=I["b_ada"][c0:c0 + 512].partition_broadcast(128))),
                      writes=[u + "bb%d" % half])
                for k in range(8):
                    P.op("pe", (lambda e, half=half, k=k: e.matmul(pp[half][:], lhsT=CB[:, k, :], rhs=wp[half][:, k, :],
                                                                   start=(k == 0), stop=(k == 7))),
                         reads=[u + "CB", u + "wp%d" % half], writes=[u + "pp%d" % half])
                dsl = dest[:, half * 512:(half + 1) * 512]
                if mode == "plain":
                    P.op("dve", (lambda e, half=half, dsl=dsl: e.tensor_tensor(out=dsl, in0=pp[half][:], in1=bb[half][:], op=ALU.add)),
                         reads=[u + "pp%d" % half, u + "bb%d" % half], writes=[destname])
                else:
                    P.op("dve", (lambda e, half=half: e.tensor_tensor(out=bb[half][:], in0=pp[half][:], in1=bb[half][:], op=ALU.add)),
                         reads=[u + "pp%d" % half, u + "bb%d" % half], writes=[u + "bb%d" % half])
                    P.op("dve", (lambda e, half=half, dsl=dsl: e.scalar_tensor_tensor(
                        out=dsl, in0=bb[half][:], scalar=1.0, in1=gb[:, half * 512:(half + 1) * 512], op0=ALU.add, op1=ALU.mult)),
                         reads=[u + "bb%d" % half, u + "gb"], writes=[destname])

        def norm_tile(st_bufs, src_ap, Gt, Bt, Gn, Bn, tag, hb_out, extra_reads=()):
            xt, junk, ss, t1 = st_bufs
            P.dma("sp", lambda e: e.dma_start(out=xt[:], in_=src_ap), reads=list(extra_reads), writes=[tag + "xt"])
            P.op("act", lambda e: e.activation(out=junk[:], in_=xt[:], func=AF.Square, accum_out=ss[:, 0:1]),
                 reads=[tag + "xt"], writes=[tag + "junk", tag + "ss"])
            P.op("dve", lambda e: e.tensor_scalar(out=ss[:, 1:2], in0=ss[:, 0:1], scalar1=1.0 / D, scalar2=1e-6,
                                                  op0=ALU.mult, op1=ALU.add), reads=[tag + "ss"], writes=[tag + "ss"])
            P.op("act", lambda e: e.activation(out=ss[:, 1:2], in_=ss[:, 1:2], func=AF.Sqrt), reads=[tag + "ss"], writes=[tag + "ss"])
            P.op("dve", lambda e: e.reciprocal(out=ss[:, 1:2], in_=ss[:, 1:2]), reads=[tag + "ss"], writes=[tag + "ss"])
            P.op("dve", lambda e: e.scalar_tensor_tensor(out=t1[:], in0=xt[:], scalar=ss[:, 1:2], in1=Gt[:], op0=ALU.mult, op1=ALU.mult),
                 reads=[tag + "xt", tag + "ss", Gn], writes=[tag + "t1"])
            P.op("pool", lambda e: e.tensor_tensor(out=hb_out[0][:], in0=t1[:], in1=Bt[:], op=ALU.add),
                 reads=[tag + "t1", Bn], writes=[hb_out[1]])

        HM8 = mk(gst, "sb", [128, 2], F32, "HM8")
        for hh_ in range(2):
            P.op("dve", (lambda e, hh_=hh_: e.tensor_scalar(out=HM8[:, hh_:hh_ + 1], in0=C["blkones"][:, hh_ * 64:hh_ * 64 + 1], scalar1=0.125, scalar2=None,
                                                            op0=ALU.mult)), reads=["c_blkones"], writes=["HM8"])
        idsel = mk(gst, "sb", [128, 64], F32, "idsel")
        P.op("dve", lambda e: e.tensor_tensor(out=idsel[:], in0=C["ident"][:, 0:64], in1=C["ident"][:, 64:128], op=ALU.add),
             reads=["c_ident"], writes=["idsel"])
        P.barrier()

        def interleave(*gens):
            gens = list(gens)
            while gens:
                for g in list(gens):
                    try:
                        next(g)
                    except StopIteration:
                        gens.remove(g)

        def st2_rwkv(b):
            with contextlib.ExitStack() as st:
                RM = mk(st, "sb", [128, S], F32, "RM")
                P.dma("sp", lambda e: e.dma_start(out=RM[:], in_=I["resetm"]), writes=["c_resetm"])
                LA = mk(st, "sb", [128, S], BF16, "LA")
                SG1 = mk(st, "sb", [128, S], BF16, "SG1")
                SG2 = mk(st, "sb", [32, S], BF16, "SG2")
                WUP = mk(st, "sb", [128, 512], BF16, "WUP")
                GUP1 = mk(st, "sb", [128, 512], BF16, "GUP1")
                GUP2 = mk(st, "sb", [32, 512], BF16, "GUP2")
                T = [mk(st, "sb", [128, S], F32, "T%d" % i) for i in range(8)]
                T1, T2, T3, T4, T5, T6, T7 = T[1:8]
                AFt, BFt, KFt, RFt, VFt, Qt = [mk(st, "sb", [128, S], BF16, n) for n in ("AFt", "BFt", "KFt", "RFt", "VFt", "Qt")]
                bank = {n: mk(st, "ps", [128, 512], F32, "bk" + n) for n in "BCDEFGH"}
                pp = [bank["E"], bank["F"]]
                ppn = ["bkE", "bkF"]
                bankA = mk(st, "ps", [128, 1024], BF16, "bkA")
                ppi = [0]

                def mm_tg(lhsT_fn, rhs_fn, reads, evac_fn, nacc=1):
                    for tg in range(4):
                        i = ppi[0] % 2
                        ppi[0] += 1
                        for a in range(nacc):
                            P.op("pe", (lambda e, i=i, a=a, tg=tg: e.matmul(pp[i][:], lhsT=lhsT_fn(a), rhs=rhs_fn(a, tg),
                                                                             start=(a == 0), stop=(a == nacc - 1))),
                                 reads=reads, writes=[ppn[i]])
                        evac_fn(pp[i], ppn[i], tg)

                sl = lambda tg: slice(tg * 512, (tg + 1) * 512)
                P.dma("sp", lambda e: e.dma_start(out=T1[:], in_=projF[b, 1536:1664, :]), reads=[], writes=["T1"])
                P.op("act", lambda e: e.activation(out=LA[0:64, :], in_=T1[0:64, :], func=AF.Tanh), reads=["T1"], writes=["LA"])
                P.op("act", lambda e: e.copy(out=LA[64:128, :], in_=T1[64:128, :]), reads=["T1"], writes=["LA"])
                P.dma("sp", lambda e: e.dma_start(out=T2[:], in_=projF[b, 1664:1792, :]), reads=[], writes=["T2"])
                P.op("act", lambda e: e.activation(out=SG1[:], in_=T2[:], func=AF.Sigmoid), reads=["T2"], writes=["SG1"])
                P.dma("sp", lambda e: e.dma_start(out=T3[0:32, :], in_=projF[b, 1792:1824, :]), reads=[], writes=["T3"])
                P.op("act", lambda e: e.activation(out=SG2[:], in_=T3[0:32, :], func=AF.Sigmoid), reads=["T3"], writes=["SG2"])
                WUP2 = mk(st, "sb", [128, 512], BF16, "WUP2")
                P.op("pool", lambda e: e.memset(WUP[:], 0.0), writes=["WUP"])
                P.op("pool", lambda e: e.memset(WUP2[:], 0.0), writes=["WUP2"])
                P.dma("pool", lambda e: e.dma_start(out=WUP[0:64, :], in_=I["w_up"]), writes=["WUP"])
                P.dma("pool", lambda e: e.dma_start(out=WUP2[64:128, :], in_=I["a_up"]), writes=["WUP2"])
                P.dma("pool", lambda e: e.dma_start(out=GUP1[:], in_=I["g_up"][0:128, :]), writes=["GUP1"])
                P.dma("pool", lambda e: e.dma_start(out=GUP2[:], in_=I["g_up"][128:160, :]), writes=["GUP2"])

                def do_hp(hp):
                    cs = slice(hp * 128, (hp + 1) * 128)
                    pv = lambda i: pvec[:, hp, i:i + 1]
                    mm_tg(lambda a: WUP[:, cs], lambda a, tg: LA[:, sl(tg)], ["WUP", "LA"],
                          lambda ps, pn, tg: P.op("act", lambda e: e.activation(out=T1[:, sl(tg)], in_=ps[:], func=AF.Sigmoid, bias=pv(0)),
                                                  reads=[pn, "pvec"], writes=["T1"]))
                    P.op("act", lambda e: e.activation(out=T1[:], in_=T1[:], func=AF.Identity, scale=-0.6065306597126334),
                         reads=["T1"], writes=["T1"])
                    P.op("dve", lambda e: e.tensor_tensor_scan(out=T2[:], data0=RM[:], data1=T1[:], initial=0.0,
                                                               op0=ALU.mult, op1=ALU.add), reads=["T1", "c_resetm"], writes=["T2"])
                    P.op("dve", lambda e: e.tensor_tensor(out=T3[:], in0=T2[:], in1=T1[:], op=ALU.subtract), reads=["T1", "T2"], writes=["T3"])
                    P.op("act", lambda e: e.activation(out=T3[:], in_=T3[:], func=AF.Exp), reads=["T3"], writes=["T3"])
                    P.op("act", lambda e: e.activation(out=T4[:], in_=T2[:], func=AF.Exp), reads=["T2"], writes=["T4"])
                    P.op("act", lambda e: e.activation(out=T2[:], in_=T2[:], func=AF.Exp, scale=-1.0), reads=["T2"], writes=["T2"])
                    mm_tg(lambda a: WUP2[:, cs], lambda a, tg: LA[:, sl(tg)], ["WUP2", "LA"],
                          lambda ps, pn, tg: P.op("act", lambda e: e.activation(out=T1[:, sl(tg)], in_=ps[:], func=AF.Sigmoid, bias=pv(1)),
                                                  reads=[pn, "pvec"], writes=["T1"]))
                    P.dma("sp", lambda e: e.dma_start(out=T5[:], in_=projF[b, 512 + hp * 128:512 + (hp + 1) * 128, :]),
                          reads=[], writes=["T5"])
                    P.op("dve", lambda e: e.tensor_scalar(out=T6[:], in0=T5[:], scalar1=pv(2), scalar2=None, op0=ALU.mult),
                         reads=["T5", "pvec"], writes=["T6"])
                    P.op("dve", lambda e: e.tensor_tensor(out=Qt[:], in0=T6[:], in1=T6[:], op=ALU.mult), reads=["T6"], writes=["Qt"])
                    mm_tg(lambda a: blk_b[:], lambda a, tg: Qt[:, sl(tg)], ["blkb", "Qt"],
                          lambda ps, pn, tg: P.op("dve", lambda e: e.tensor_scalar(out=T7[:, sl(tg)], in0=ps[:], scalar1=1e-24, scalar2=None, op0=ALU.max),
                                                  reads=[pn], writes=["T7"]))
                    P.op("act", lambda e: e.activation(out=T7[:], in_=T7[:], func=AF.Ln), reads=["T7"], writes=["T7"])
                    P.op("act", lambda e: e.activation(out=T7[:], in_=T7[:], func=AF.Exp, scale=-0.5), reads=["T7"], writes=["T7"])
                    P.op("dve", lambda e: e.tensor_tensor(out=T6[:], in0=T6[:], in1=T7[:], op=ALU.mult), reads=["T6", "T7"], writes=["T6"])
                    P.op("dve", lambda e: e.scalar_tensor_tensor(out=AFt[:], in0=T6[:], scalar=-1.0, in1=T3[:], op0=ALU.mult, op1=ALU.mult),
                         reads=["T6", "T3"], writes=["AFt"])
                    P.op("dve", lambda e: e.tensor_tensor(out=T7[:], in0=T6[:], in1=T1[:], op=ALU.mult), reads=["T6", "T1"], writes=["T7"])
                    P.op("dve", lambda e: e.tensor_tensor(out=BFt[:], in0=T7[:], in1=T2[:], op=ALU.mult), reads=["T7", "T2"], writes=["BFt"])
                    P.op("dve", lambda e: e.tensor_scalar(out=T7[:], in0=T1[:], scalar1=-1.0, scalar2=pv(3), op0=ALU.add, op1=ALU.mult),
                         reads=["T1", "pvec"], writes=["T7"])
                    P.op("dve", lambda e: e.scalar_tensor_tensor(out=T5[:], in0=T7[:], scalar=1.0, in1=T5[:], op0=ALU.add, op1=ALU.mult),
                         reads=["T7", "T5"], writes=["T5"])
                    P.op("dve", lambda e: e.tensor_tensor(out=KFt[:], in0=T5[:], in1=T2[:], op=ALU.mult), reads=["T5", "T2"], writes=["KFt"])
                    P.dma("sp", lambda e: e.dma_start(out=T1[:], in_=projF[b, hp * 128:(hp + 1) * 128, :]), reads=[], writes=["T1"])
                    P.op("dve", lambda e: e.tensor_tensor(out=RFt[:], in0=T1[:], in1=T4[:], op=ALU.mult), reads=["T1", "T4"], writes=["RFt"])
                    P.op("dve", lambda e: e.tensor_tensor(out=T6[:], in0=T1[:], in1=T5[:], op=ALU.mult), reads=["T1", "T5"], writes=["T6"])
                    P.op("dve", lambda e: e.tensor_scalar(out=Qt[:], in0=T6[:], scalar1=pv(4), scalar2=None, op0=ALU.mult),
                         reads=["T6", "pvec"], writes=["Qt"])
                    P.dma("sp", lambda e: e.dma_start(out=T3[:], in_=projF[b, 1024 + hp * 128:1024 + (hp + 1) * 128, :]),
                          reads=[], writes=["T3"])
                    P.op("act", lambda e: e.copy(out=VFt[:], in_=T3[:]), reads=["T3"], writes=["VFt"])
                    mm_tg(lambda a: blk_b[:], lambda a, tg: Qt[:, sl(tg)], ["blkb", "Qt"],
                          lambda ps, pn, tg: P.op("dve", lambda e: e.tensor_tensor(out=T7[:, sl(tg)], in0=ps[:], in1=T3[:, sl(tg)], op=ALU.mult),
                                                  reads=[pn, "T3"], writes=["T7"]))
                    mm_tg(lambda a: (GUP1[:, cs] if a == 0 else GUP2[:, cs]), lambda a, tg: (SG1[:, sl(tg)] if a == 0 else SG2[:, sl(tg)]),
                          ["GUP1", "GUP2", "SG1", "SG2"],
                          lambda ps, pn, tg: P.op("act", lambda e: e.copy(out=T2[:, sl(tg)], in_=ps[:]), reads=[pn], writes=["T2"]),
                          nacc=2)

                    with contextlib.ExitStack() as sc:
                        sbc = lambda shape, dt, n: mk(sc, "sb", shape, dt, n)
                        hm = lambda hh: C["blkones"][:, hh * 64:hh * 64 + 1]
                        AFh = [sbc([128, S], BF16, "AFh") for _ in range(2)]
                        RFh = [sbc([128, S], BF16, "RFh") for _ in range(2)]
                        BFh = [sbc([128, S], BF16, "BFh") for _ in range(2)]
                        for hh in range(2):
                            P.op("act", (lambda e, hh=hh: e.activation(out=AFh[hh][:], in_=AFt[:], func=AF.Identity, scale=hm(hh))),
                                 reads=["AFt", "c_blkones"], writes=["AFh%d" % hh])
                            P.op("act", (lambda e, hh=hh: e.activation(out=RFh[hh][:], in_=RFt[:], func=AF.Identity, scale=hm(hh))),
                                 reads=["RFt", "c_blkones"], writes=["RFh%d" % hh])
                            P.op("act", (lambda e, hh=hh: e.activation(out=BFh[hh][:], in_=BFt[:], func=AF.Identity, scale=hm(hh))),
                                 reads=["BFt", "c_blkones"], writes=["BFh%d" % hh])
                        TT = sbc([128, 4, 128], BF16, "TT")
                        PADT = sbc([128, 3, 384], BF16, "PADT")
                        ZZ = sbc([128, 2, 384], BF16, "ZZ")
                        XM1 = sbc([128, 2, 256], BF16, "XM1")
                        XM2 = sbc([128, 2, 256], BF16, "XM2")
                        LM = sbc([128, 2, 128], BF16, "LM")
                        Z = [sbc([128, 2, 128], BF16, "Z%d" % i) for i in range(KLV + 1)]
                        XPw = {k: sbc([128, 2, 128], BF16, "XP%d" % k) for k in range(1, KLV)}
                        LPw = {k: sbc([128, 2, 128], BF16, "LP%d" % k) for k in range(1, KLV - 1)}
                        BX, UX, VX = [sbc([128, 2, NCH * 64], BF16, n) for n in ("BX", "UX", "VX")]
                        RH = [sbc([128, 128], F32, "RH") for _ in range(2)]
                        YH = [sbc([128, 128], F32, "YH") for _ in range(2)]
                        GT = [sbc([128, NCH, 128], F32, "GT") for _ in range(2)]
                        HG = [sbc([128, NCH, 128], F32, "HG") for _ in range(2)]
                        STt = [sbc([128, 128], F32, "ST") for _ in range(2)]
                        P.op("pool", lambda e: e.memset(STt[0][:], 0.0), writes=["ST0"])
                        P.op("pool", lambda e: e.memset(PADT[:], 0.0), writes=["PADT"])
                        P.op("pool", lambda e: e.memset(ZZ[:], 0.0), writes=["ZZ"])
                        for q_ in range(2):
                            P.op("pool", (lambda e, q_=q_: e.memset(GT[q_][:], 0.0)), writes=["GT%d" % q_])
                            P.op("pool", (lambda e, q_=q_: e.memset(HG[q_][:], 0.0)), writes=["HG%d" % q_])
                        PT = bankA[:].rearrange("p (a c) -> p a c", c=128)[:, 0:4, :]
                        P1 = bank["B"][:].rearrange("p (h c) -> p h c", c=256)
                        P2 = bank["C"][:].rearrange("p (h c) -> p h c", c=256)
                        P3 = bank["D"][:, 0:256].rearrange("p (h c) -> p h c", c=128)
                        P4 = bank["D"][:, 256:384].rearrange("p (h c) -> p h c", c=64)
                        rot = [bank["E"], bank["F"], bank["G"]]
                        rotn = ["bkE", "bkF", "bkG"]
                        ri = [0]
                        cm3 = C["cmask"][:].rearrange("p (n j) -> p n j", j=64)
                        HS = [slice(0, 64), slice(64, 128)]
                        HB = [slice(0, 128), slice(128, 256)]

                        def pre(bl):
                            blk = slice(bl * 128, (bl + 1) * 128)
                            q = bl % 2
                            for a, src in enumerate((AFt, BFt, KFt, VFt)):
                                P.op("pe", (lambda e, a=a, src=src: e.transpose(PT[:, a, :], src[:, blk], identb[:])),
                                     reads=[("AFt", "BFt", "KFt", "VFt")[a], "identb"], writes=["bkA"])
                            P.op("act", lambda e: e.copy(out=TT[:], in_=PT), reads=["bkA"], writes=["TT"])
                            P.op("dve", lambda e: e.tensor_copy(
                                out=PADT[:].rearrange("p a (h c) -> p a h c", c=192)[:, :, :, 0:64],
                                in_=TT[:, 1:4, :].rearrange("p a (h c) -> p a h c", c=64)), reads=["TT"], writes=["PADT"])
                            for hh in range(2):
                                hc = slice(hh * 64, (hh + 1) * 64)
                                P.op("pool", (lambda e, hh=hh, hc=hc: e.tensor_tensor(
                                    out=BX[:, hh, :].rearrange("p (n j) -> p n j", j=64),
                                    in0=TT[:, 1, hc].unsqueeze(1).to_broadcast([128, NCH, 64]), in1=cm3, op=ALU.mult)),
                                     reads=["TT", "c_cmask"], writes=["BX"])
                                P.op("pool", (lambda e, hh=hh, hc=hc: e.tensor_tensor(
                                    out=VX[:, hh, :].rearrange("p (n j) -> p n j", j=64),
                                    in0=TT[:, 3, hc].unsqueeze(1).to_broadcast([128, NCH, 64]), in1=cm3, op=ALU.mult)),
                                     reads=["TT", "c_cmask"], writes=["VX"])
                            yield
                            for hh in range(2):
                                P.op("pe", (lambda e, hh=hh: e.matmul(P1[:, hh, 0:128], lhsT=BFt[:, blk], rhs=AFh[hh][:, blk], start=True, stop=True)),
                                     reads=["BFt", "AFh%d" % hh], writes=["bkB"])
                                P.op("pe", (lambda e, hh=hh: e.matmul(P1[:, hh, 128:256], lhsT=BFt[:, blk], rhs=RFh[hh][:, blk], start=True, stop=True)),
                                     reads=["BFt", "RFh%d" % hh], writes=["bkB"])
                                P.op("pe", (lambda e, hh=hh: e.matmul(P2[:, hh, 0:128], lhsT=KFt[:, blk], rhs=AFh[hh][:, blk], start=True, stop=True)),
                                     reads=["KFt", "AFh%d" % hh], writes=["bkC"])
                                P.op("pe", (lambda e, hh=hh: e.matmul(P2[:, hh, 128:256], lhsT=KFt[:, blk], rhs=RFh[hh][:, blk], start=True, stop=True)),
                                     reads=["KFt", "RFh%d" % hh], writes=["bkC"])
                                P.op("pe", (lambda e, hh=hh: e.matmul(P3[:, hh, :], lhsT=AFt[:, blk], rhs=BFh[hh][:, blk], start=True, stop=True)),
                                     reads=["AFt", "BFh%d" % hh], writes=["bkD"])
                            msk = C["mask_si"][:].unsqueeze(1).to_broadcast([128, 2, 256])
                            P.op("dve", lambda e: e.tensor_tensor(out=XM1[:], in0=P1, in1=msk, op=ALU.mult), reads=["bkB", "c_mask_si"], writes=["XM1"])
                            P.op("dve", lambda e: e.tensor_tensor(out=XM2[:], in0=P2, in1=msk, op=ALU.mult), reads=["bkC", "c_mask_si"], writes=["XM2"])
                            P.op("dve", lambda e: e.tensor_tensor(out=LM[:], in0=P3, in1=C["mask_l"][:].unsqueeze(1).to_broadcast([128, 2, 128]),
                                                                  op=ALU.mult), reads=["bkD", "c_mask_l"], writes=["LM"])
                            yield
                            Xab = lambda hh: XM1[:, hh, 0:128]
                            Mrb = lambda hh: XM1[:, hh, 128:256]
                            Xak = lambda hh: XM2[:, hh, 0:128]
                            Mrk = lambda hh: XM2[:, hh, 128:256]
                            VT = lambda hh: TT[:, 3, hh * 64:(hh + 1) * 64]
                            for hh in range(2):
                                P.op("pe", (lambda e, hh=hh: e.matmul(P4[:, hh, :], lhsT=Xak(hh), rhs=VT(hh), start=True, stop=True)),
                                     reads=["XM2", "TT"], writes=["bkD"])
                            P.op("dve", lambda e: e.tensor_copy(out=Z[0][:, :, 0:64], in_=TT[:, 0, :].rearrange("p (h c) -> p h c", c=64)),
                                 reads=["TT"], writes=["Z0a"])
                            P.op("act", lambda e: e.copy(out=Z[0][:, :, 64:128], in_=P4), reads=["bkD"], writes=["Z0b"])
                            yield

                            def mm2(dst_i, lfn, rfn, reads):
                                for hh in range(2):
                                    P.op("pe", (lambda e, hh=hh: e.matmul(rot[dst_i][:, hh * 128:(hh + 1) * 128], lhsT=lfn(hh), rhs=rfn(hh), start=True, stop=True)),
                                         reads=reads, writes=[rotn[dst_i]])

                            def rv(i):
                                return rot[i][:, 0:256].rearrange("p (h c) -> p h c", c=128)

                            def tl(Xt):
                                return Xt if callable(Xt) else (lambda hh: Xt[:, hh, :])

                            def zstep(k, Xt, xn):
                                i = ri[0] % len(rot)
                                ri[0] += 1
                                zr = ["Z%da" % k, "Z%db" % k] if k == 0 else ["Z%d" % k]
                                mm2(i, tl(Xt), lambda hh: Z[k][:, hh, :], [xn] + zr)
                                P.op("dve", (lambda e, i=i: e.tensor_tensor(out=Z[k + 1][:], in0=rv(i), in1=Z[k][:], op=ALU.add)),
                                     reads=[rotn[i]] + zr, writes=["Z%d" % (k + 1)])

                            def sq(dst, dn, Lt, ln_, Rt, rn_):
                                i = ri[0] % len(rot)
                                ri[0] += 1
                                mm2(i, tl(Lt), tl(Rt), [ln_, rn_])
                                P.op("act", (lambda e, i=i: e.copy(out=dst[:], in_=rv(i))), reads=[rotn[i]], writes=[dn])

                            Lab = lambda hh: LM[:, hh, :]
                            Xk, Xn_, Lk, Ln_ = Xab, "XM1", Lab, "LM"
                            for k in range(KLV):
                                zstep(k, Xk, Xn_)
                                if k < KLV - 1:
                                    sq(XPw[k + 1], "XP%d" % (k + 1), Lk, Ln_, Xk, Xn_)
                                    if k < KLV - 2:
                                        sq(LPw[k + 1], "LP%d" % (k + 1), Xk, Xn_, Lk, Ln_)
                                        Lk, Ln_ = LPw[k + 1], "LP%d" % (k + 1)
                                    Xk, Xn_ = XPw[k + 1], "XP%d" % (k + 1)
                                if k < KLV - 1:
                                    yield
                            P.op("act", lambda e: e.copy(
                                out=ZZ[:].rearrange("p a (h c) -> p a h c", c=192)[:, :, :, 0:64],
                                in_=Z[KLV][:].rearrange("p h (a c) -> p a h c", c=64)), reads=["Z%d" % KLV], writes=["ZZ"])
                            for hh in range(2):
                                P.op("dve", (lambda e, hh=hh: e.tensor_tensor(
                                    out=UX[:, hh, :].rearrange("p (n j) -> p n j", j=64),
                                    in0=Z[KLV][:, hh, 64:128].unsqueeze(1).to_broadcast([128, NCH, 64]), in1=cm3, op=ALU.mult)),
                                     reads=["Z%d" % KLV, "c_cmask"], writes=["UX"])
                            yield
                            Uh = lambda hh: Z[KLV][:, hh, 64:128]
                            PR = bank["B"][:, 0:128]
                            PY = bank["B"][:, 128:256]
                            for hh in range(2):
                                P.op("pe", (lambda e, hh=hh: e.matmul(PR, lhsT=ZZ[:, 0, HB[hh]], rhs=Mrb(hh), start=(hh == 0), stop=(hh == 1))),
                                     reads=["ZZ", "XM1"], writes=["bkB"])
                            for hh in range(2):
                                P.op("pe", (lambda e, hh=hh: e.matmul(PY, lhsT=ZZ[:, 1, HB[hh]], rhs=Mrb(hh), start=(hh == 0), stop=False)),
                                     reads=["ZZ", "XM1"], writes=["bkB"])
                                P.op("pe", (lambda e, hh=hh: e.matmul(PY, lhsT=PADT[:, 2, HB[hh]], rhs=Mrk(hh), start=False, stop=(hh == 1))),
                                     reads=["PADT", "XM2"], writes=["bkB"])
                            P.op("dve", lambda e: e.tensor_tensor(out=RH[q][:], in0=PR, in1=RFt[:, blk], op=ALU.add),
                                 reads=["bkB", "RFt"], writes=["RH%d" % q])
                            P.op("act", lambda e: e.copy(out=YH[q][:], in_=PY), reads=["bkB"], writes=["YH%d" % q])
                            yield
                            PG = bankA[:].bitcast(F32)[:, 0:NCH * 64]
                            PH = bank["C"][:, 0:NCH * 64]
                            for hh in range(2):
                                P.op("pe", (lambda e, hh=hh: e.matmul(PG, lhsT=ZZ[:, 0, HB[hh]], rhs=BX[:, hh, :], start=(hh == 0), stop=(hh == 1))),
                                     reads=["ZZ", "BX"], writes=["bkA"])
                            for hh in range(2):
                                P.op("pe", (lambda e, hh=hh: e.matmul(PH[:], lhsT=PADT[:, 0, HB[hh]], rhs=UX[:, hh, :], start=(hh == 0), stop=False)),
                                     reads=["PADT", "UX"], writes=["bkC"])
                                P.op("pe", (lambda e, hh=hh: e.matmul(PH[:], lhsT=PADT[:, 1, HB[hh]], rhs=VX[:, hh, :], start=False, stop=(hh == 1))),
                                     reads=["PADT", "VX"], writes=["bkC"])
                            gc = T4[:, blk].rearrange("p (n c) -> p n c", c=CH)[:, :, CH - 1]
                            for hh in range(2):
                                hs = HS[hh]
                                P.op("dve", (lambda e, hs=hs: e.tensor_tensor(out=GT[q][hs, :, hs], in0=PG[hs, :].rearrange("p (n j) -> p n j", j=64),
                                                                              in1=idsel[hs, :].unsqueeze(1).to_broadcast([64, NCH, 64]), op=ALU.add)),
                                     reads=["bkA", "idsel"], writes=["GT%d" % q])
                                P.op("dve", (lambda e, hs=hs: e.tensor_tensor(out=HG[q][hs, :, hs], in0=PH[hs, :].rearrange("p (n j) -> p n j", j=64),
                                                                              in1=gc[hs, :].unsqueeze(2).to_broadcast([64, NCH, 64]), op=ALU.mult)),
                                     reads=["bkC", "T4"], writes=["HG%d" % q])
                            yield

                        def chain(bl):
                            blk = slice(bl * 128, (bl + 1) * 128)
                            q = bl % 2
                            PYc = bank["H"][:, 0:128]
                            PS = bank["H"][:, 128:256]
                            for n in range(NCH):
                                g = bl * NCH + n
                                cur, nxt = STt[g % 2], STt[(g + 1) % 2]
                                cn, nn = "ST%d" % (g % 2), "ST%d" % ((g + 1) % 2)
                                P.op("pe", (lambda e, n=n, cur=cur: e.matmul(PYc[:, n * CH:(n + 1) * CH], lhsT=cur[:], rhs=RH[q][:, n * CH:(n + 1) * CH],
                                                                             start=True, stop=True)),
                                     reads=[cn, "RH%d" % q], writes=["bkH"])
                                P.op("pe", (lambda e, n=n, cur=cur: e.matmul(PS, lhsT=GT[q][:, n, :], rhs=cur[:], start=True, stop=True)),
                                     reads=[cn, "GT%d" % q], writes=["bkH"])
                                for _w in range(WARM):
                                    P.op("pe", lambda e: e.matmul(bank["G"][:], lhsT=identb[:], rhs=AFt[:, 0:512], start=True, stop=True),
                                         reads=[], writes=["bkG"])
                                gcol = T4[:, bl * 128 + n * CH + CH - 1:bl * 128 + n * CH + CH]
                                P.op("dve", (lambda e, n=n, nxt=nxt, gcol=gcol: e.scalar_tensor_tensor(out=nxt[:], in0=PS, scalar=gcol, in1=HG[q][:, n, :],
                                                                                                        op0=ALU.mult, op1=ALU.add)),
                                     reads=["bkH", "T4", "HG%d" % q], writes=[nn])
                                yield
                            P.op("dve", lambda e: e.tensor_tensor(out=T5[:, blk], in0=PYc, in1=YH[q][:], op=ALU.add),
                                 reads=["bkH", "YH%d" % q], writes=["T5"])
                            yield

                        nblk = T2_NBLK
                        if T2_UPTO == 2:
                            g_ = pre(0)
                            for _ in range(t2.get("pre", 99)):
                                try:
                                    next(g_)
                                except StopIteration:
                                    break
                        elif T2_UPTO >= 2:
                            interleave(pre(0))
                        if T2_UPTO >= 3:
                            for bl in range(nblk):
                                if bl + 1 < nblk:
                                    interleave(chain(bl), pre(bl + 1))
                                else:
                                    interleave(chain(bl))

                    if T2_UPTO < 4:
                        P.barrier()
                        return
                    P.op("act", lambda e: e.copy(out=Qt[:], in_=T5[:]), reads=["T5"], writes=["Qt"])
                    mm_tg(lambda a: blk_b[:], lambda a, tg: Qt[:, sl(tg)], ["blkb", "Qt"],
                          lambda ps, pn, tg: P.op("dve", lambda e: e.scalar_tensor_tensor(out=T3[:, sl(tg)], in0=ps[:], scalar=-1.0 / 64, in1=T5[:, sl(tg)],
                                                                                          op0=ALU.mult, op1=ALU.add),
                                                  reads=[pn, "T5"], writes=["T3"]))
                    P.op("dve", lambda e: e.tensor_tensor(out=Qt[:], in0=T3[:], in1=T3[:], op=ALU.mult), reads=["T3"], writes=["Qt"])
                    mm_tg(lambda a: blk_b[:], lambda a, tg: Qt[:, sl(tg)], ["blkb", "Qt"],
                          lambda ps, pn, tg: P.op("dve", lambda e: e.tensor_scalar(out=T6[:, sl(tg)], in0=ps[:], scalar1=1.0 / 64, scalar2=64e-5,
                                                                                   op0=ALU.mult, op1=ALU.add),
                                                  reads=[pn], writes=["T6"]))
                    P.op("act", lambda e: e.activation(out=T6[:], in_=T6[:], func=AF.Ln), reads=["T6"], writes=["T6"])
                    P.op("act", lambda e: e.activation(out=T6[:], in_=T6[:], func=AF.Exp, scale=-0.5), reads=["T6"], writes=["T6"])
                    P.op("dve", lambda e: e.tensor_tensor(out=T3[:], in0=T3[:], in1=T6[:], op=ALU.mult), reads=["T3", "T6"], writes=["T3"])
                    P.op("dve", lambda e: e.tensor_scalar(out=T3[:], in0=T3[:], scalar1=pv(5), scalar2=pv(6), op0=ALU.mult, op1=ALU.add),
                         reads=["T3", "pvec"], writes=["T3"])
                    P.op("dve", lambda e: e.tensor_tensor(out=T3[:], in0=T3[:], in1=T7[:], op=ALU.add), reads=["T3", "T7"], writes=["T3"])
                    P.op("dve", lambda e: e.tensor_tensor(out=Qt[:], in0=T3[:], in1=T2[:], op=ALU.mult), reads=["T3", "T2"], writes=["Qt"])
                    P.dma("sp", lambda e: e.dma_start(out=ymixT[b, hp * 128:(hp + 1) * 128, :], in_=Qt[:]), reads=["Qt"], writes=[])
                    if debug and b == 0:
                        P.dma("sp", lambda e: e.dma_start(out=dbg["d_yr"][hp * 128:(hp + 1) * 128, :], in_=T3[:]), reads=["T3"])
                        P.dma("sp", lambda e: e.dma_start(out=dbg["d_yraw"][hp * 128:(hp + 1) * 128, :], in_=T5[:]), reads=["T5"])
                    P.barrier()

                for hp in range(T2_NHP):
                    do_hp(hp)
            P.barrier()

        def st3_moba(b):
            with contextlib.ExitStack() as st:
                TQ, TK, TV, RR = [mk(st, "sb", [128, S], F32, n) for n in ("TQ", "TK", "TV", "RR")]
                SQ = mk(st, "sb", [128, S], BF16, "SQ")
                KNBh = [mk(st, "sb", [128, S], BF16, "KNBh") for _ in range(2)]
                QH = [mk(st, "sb", [128, S], BF16, "QH") for _ in range(2)]
                VA = mk(st, "sb", [128, 16, 2, 65], BF16, "VA")
                KM = mk(st, "sb", [128, 8], F32, "KM")
                KMH = [mk(st, "sb", [128, 8], F32, "KMH") for _ in range(2)]
                GATE = mk(st, "sb", [128, 32, 8], F32, "GATE")
                MX = mk(st, "sb", [128, 32, 8], F32, "MX")
                SEL = mk(st, "sb", [128, 32, 8], F32, "SEL")
                ET = [mk(st, "sb", [128, 2, 128], BF16, "ET") for _ in range(4)]
                ACC = [mk(st, "sb", [128, 65], F32, "ACC") for _ in range(2)]
                RC = [mk(st, "sb", [128, 1], F32, "RC") for _ in range(2)]
                YT = mk(st, "sb", [128, 16, 128], BF16, "YT")
                ppx = [mk(st, "ps", [128, 512], F32, "PSpp") for _ in range(2)]
                PTR = mk(st, "ps", [128, 8, 128], BF16, "PStr")
                PGt = mk(st, "ps", [128, 512], F32, "PSg")
                PSs = [mk(st, "ps", [128, 512], F32, "PSs") for _ in range(2)]
                POs = [mk(st, "ps", [128, 512], F32, "PSo") for _ in range(2)]
                sl = lambda tg: slice(tg * 512, (tg + 1) * 512)
                hm = lambda hh: C["blkones"][:, hh * 64:hh * 64 + 1]
                P.op("pool", lambda e: e.memset(VA[:, :, :, 64:65], 1.0), writes=["VA1"])
                cnt = {"pp": 0, "s": 0, "o": 0, "e": 0}

                def rmsn(Tt, tn, gcol):
                    P.op("pool", lambda e: e.tensor_tensor(out=SQ[:], in0=Tt[:], in1=Tt[:], op=ALU.mult), reads=[tn], writes=["SQ"])
                    for tg in range(4):
                        i = cnt["pp"] % 2
                        cnt["pp"] += 1
                        P.op("pe", (lambda e, i=i, tg=tg: e.matmul(ppx[i][:], lhsT=blk_b[:], rhs=SQ[:, sl(tg)], start=True, stop=True)),
                             reads=["blkb", "SQ"], writes=["PSpp%d" % i])
                        P.op("dve", (lambda e, i=i, tg=tg: e.tensor_scalar(out=RR[:, sl(tg)], in0=ppx[i][:], scalar1=1.0 / 64, scalar2=1e-6,
                                                                         op0=ALU.mult, op1=ALU.add)), reads=["PSpp%d" % i], writes=["RR"])
                    P.op("act", lambda e: e.activation(out=RR[:], in_=RR[:], func=AF.Ln), reads=["RR"], writes=["RR"])
                    P.op("act", lambda e: e.activation(out=RR[:], in_=RR[:], func=AF.Exp, scale=-0.5), reads=["RR"], writes=["RR"])
                    P.op("dve", lambda e: e.scalar_tensor_tensor(out=Tt[:], in0=Tt[:], scalar=gcol, in1=RR[:], op0=ALU.mult, op1=ALU.mult),
                         reads=[tn, "RR", "qkg"], writes=[tn])

                def do_hp(hp):
                    r0 = 1920 + hp * 128
                    P.dma("sp", lambda e: e.dma_start(out=TQ[:], in_=projF[b, r0:r0 + 128, :]), reads=[], writes=["TQ"])
                    P.dma("sp", lambda e: e.dma_start(out=TK[:], in_=projF[b, r0 + 512:r0 + 640, :]), reads=[], writes=["TK"])
                    P.dma("sp", lambda e: e.dma_start(out=TV[:], in_=projF[b, r0 + 1024:r0 + 1152, :]), reads=[], writes=["TV"])
                    rmsn(TQ, "TQ", qkg[:, 0:1])
                    for hh in range(2):
                        P.op("act", (lambda e, hh=hh: e.activation(out=QH[hh][:], in_=TQ[:], func=AF.Identity, scale=HM8[:, hh:hh + 1])),
                             reads=["TQ", "HM8"], writes=["QH%d" % hh])
                    rmsn(TK, "TK", qkg[:, 1:2])
                    for hh in range(2):
                        hg_ = hp * 2 + hh
                        o_ = 64 * (1 - hh)
                        P.op("act", (lambda e, hh=hh: e.copy(out=KNBh[hh][:], in_=TK[:])), reads=["TK"], writes=["KNB%d" % hh])
                        P.dma("pool", (lambda e, hh=hh, hg_=hg_, o_=o_: e.dma_start(out=KNBh[hh][o_:o_ + 3, :], in_=I["alk"][hg_ * 3:hg_ * 3 + 3, :])),
                              writes=["KNB%d" % hh])
                        P.dma("pool", (lambda e, hh=hh, hg_=hg_, o_=o_: e.dma_start(out=QH[hh][o_:o_ + 3, :], in_=I["alq"][hg_ * 3:hg_ * 3 + 3, :])),
                              writes=["QH%d" % hh])
                    P.op("dve", lambda e: e.tensor_reduce(out=KM[:], in_=TK[:].rearrange("p (n k) -> p n k", k=256), axis=AX.X, op=ALU.add),
                         reads=["TK"], writes=["KM"])
                    for hh in range(2):
                        P.op("dve", (lambda e, hh=hh: e.tensor_scalar(out=KMH[hh][:], in0=KM[:], scalar1=hm(hh), scalar2=1.0 / 256, op0=ALU.mult, op1=ALU.mult)),
                             reads=["KM", "c_blkones"], writes=["KMH%d" % hh])
                    P.op("act", lambda e: e.copy(out=SQ[:], in_=TV[:]), reads=["TV"], writes=["SQ"])
                    for g8 in range(2):
                        for k8 in range(8):
                            kt = g8 * 8 + k8
                            P.op("pe", (lambda e, kt=kt, k8=k8: e.transpose(PTR[:, k8, :], SQ[:, kt * 128:(kt + 1) * 128], identb[:])),
                                 reads=["SQ", "identb"], writes=["PStr"])
                        P.op("act", (lambda e, g8=g8: e.copy(out=VA[:, g8 * 8:(g8 + 1) * 8, :, 0:64],
                                                             in_=PTR[:].rearrange("p k (h c) -> p k h c", c=64))),
                             reads=["PStr"], writes=["VA"])
                    PG3 = PGt[:, 0:256].rearrange("p (g n) -> p g n", n=8)
                    for qt in range(NT):
                        for hh in range(2):
                            P.op("pe", (lambda e, qt=qt, hh=hh: e.matmul(PG3[:, qt * 2 + hh, :], lhsT=TQ[:, qt * 128:(qt + 1) * 128], rhs=KMH[hh][:],
                                                                         start=True, stop=True)),
                                 reads=["TQ", "KMH%d" % hh], writes=["PSg"])
                    pm4 = C["pastm"][:].rearrange("p (j n) -> p j n", n=8).unsqueeze(2).to_broadcast([128, 8, 4, 8])
                    pi4 = C["pasti"][:].rearrange("p (j n) -> p j n", n=8).unsqueeze(2).to_broadcast([128, 8, 4, 8])
                    g4 = lambda T_: T_[:].rearrange("p (j f) n -> p j f n", f=4)
                    P.op("dve", lambda e: e.tensor_tensor(out=g4(GATE), in0=PGt[:, 0:256].rearrange("p (j f n) -> p j f n", f=4, n=8), in1=pm4, op=ALU.add),
                         reads=["PSg", "c_pastm"], writes=["GATE"])
                    for g in range(32):
                        P.op("dve", (lambda e, g=g: e.max(out=MX[:, g, :], in_=GATE[:, g, :])), reads=["GATE"], writes=["MX"])
                    P.op("dve", lambda e: e.tensor_tensor(out=SEL[:], in0=GATE[:], in1=MX[:, :, 2:3].to_broadcast([128, 32, 8]), op=ALU.is_ge),
                         reads=["GATE", "MX"], writes=["SEL"])
                    P.op("dve", lambda e: e.tensor_tensor(out=g4(SEL), in0=g4(SEL), in1=pi4, op=ALU.mult), reads=["SEL", "c_pasti"], writes=["SEL"])

                    Sb = [(PSs[0], "PSs0"), (PSs[1], "PSs1"), (ppx[0], "PSpp0"), (ppx[1], "PSpp1")]
                    Ob = [(POs[0], "PSo0"), (POs[1], "PSo1"), (PGt, "PSg"), (PTR[:].rearrange("p k c -> p (k c)").bitcast(F32), "PStr")]

                    def front(hh, qt, kts):
                        si = cnt["s"] % 4
                        cnt["s"] += 1
                        ei = cnt["e"] % 4
                        cnt["e"] += 1
                        PSs_, sn = Sb[si]
                        qs = slice(qt * 128, (qt + 1) * 128)
                        nk = len(kts)
                        for idx, kt in enumerate(kts):
                            P.op("pe", (lambda e, idx=idx, kt=kt: e.matmul(PSs_[:, idx * 128:(idx + 1) * 128], lhsT=KNBh[hh][:, kt * 128:(kt + 1) * 128],
                                                                           rhs=QH[hh][:, qs], start=True, stop=True)),
                                 reads=["KNB%d" % hh, "QH%d" % hh], writes=[sn])
                        P.op("act", lambda e: e.activation(out=ET[ei][:, 0:nk, :], in_=PSs_[:, 0:nk * 128].rearrange("p (k c) -> p k c", c=128), func=AF.Exp),
                             reads=[sn], writes=["ET%d" % ei])
                        for idx, kt in enumerate(kts):
                            if kt == qt:
                                P.op("pool", (lambda e, idx=idx: e.tensor_tensor(out=ET[ei][:, idx, :], in0=ET[ei][:, idx, :], in1=C["causal"][:], op=ALU.mult)),
                                     reads=["ET%d" % ei, "c_causal"], writes=["ET%d" % ei])
                        return ei

                    def back(hh, qt, kts, sel_col, first, last, ei):
                        oi = cnt["o"] % 4
                        cnt["o"] += 1
                        POs_, on = Ob[oi]
                        a = qt % 2
                        nk = len(kts)
                        for idx, kt in enumerate(kts):
                            P.op("pe", (lambda e, idx=idx, kt=kt: e.matmul(POs_[:, 0:65], lhsT=ET[ei][:, idx, :], rhs=VA[:, kt, hh, :],
                                                                           start=(idx == 0), stop=(idx == nk - 1))),
                                 reads=["ET%d" % ei, "VA", "VA1"], writes=[on])
                        if first:
                            P.op("act", lambda e: e.copy(out=ACC[a][:], in_=POs_[:, 0:65]), reads=[on], writes=["ACC%d" % a])
                        else:
                            P.op("dve", lambda e: e.scalar_tensor_tensor(out=ACC[a][:], in0=POs_[:, 0:65], scalar=sel_col, in1=ACC[a][:],
                                                                         op0=ALU.mult, op1=ALU.add),
                                 reads=[on, "SEL", "ACC%d" % a], writes=["ACC%d" % a])
                        if last:
                            P.op("dve", lambda e: e.reciprocal(out=RC[a][:], in_=ACC[a][:, 64:65]), reads=["ACC%d" % a], writes=["RC%d" % a])
                            P.op("dve", lambda e: e.tensor_scalar(out=YT[:, qt, hh * 64:(hh + 1) * 64], in0=ACC[a][:, 0:64],
                                                                  scalar1=RC[a][:, 0:1], scalar2=None, op0=ALU.mult),
                                 reads=["ACC%d" % a, "RC%d" % a], writes=["YT"])

                    work = []
                    for hh in range(2):
                        for qt in range(NT):
                            j = qt // 2
                            items = [([qt] if qt % 2 == 0 else [qt - 1, qt], None, True)]
                            for n in range(j):
                                items.append(([2 * n, 2 * n + 1], SEL[:, qt * 2 + hh, n:n + 1], False))
                            for ii, (kts, sc, first) in enumerate(items):
                                work.append((hh, qt, kts, sc, first, ii == len(items) - 1))
                    SKEW = 3
                    pend = []
                    for wi in range(len(work) + SKEW):
                        if wi < len(work):
                            hh, qt, kts, sc, first, last = work[wi]
                            pend.append((work[wi], front(hh, qt, kts)))
                        if wi >= SKEW:
                            (hh, qt, kts, sc, first, last), ei = pend.pop(0)
                            back(hh, qt, kts, sc, first, last, ei)
                    P.dma("sp", lambda e: e.dma_start(out=ymoba[b].rearrange("(t p) c -> p t c", p=128)[:, :, hp * 128:(hp + 1) * 128], in_=YT[:]),
                          reads=["YT"], writes=[])
                    if debug and b == 0:
                        P.dma("sp", lambda e: e.dma_start(out=dbg["d_ym"].rearrange("(t p) c -> p t c", p=128)[:, :, hp * 128:(hp + 1) * 128], in_=YT[:]),
                              reads=["YT"])

                for hp in range(t3.get("nhp", 4)):
                    do_hp(hp)
            P.barrier()

        def st456(b):
            with contextlib.ExitStack() as st:
                h2T = mk(st, "sb", [128, 8, S], BF16, "h2T")
                ACCM = mk(st, "sb", [128, NT, 1024], F32, "ACCM")
                GW = mk(st, "sb", [128, NT, NEXP], F32, "GW")
                GT1, G2, B2, GT2 = [mk(st, "sb", [128, 1024], F32, n) for n in ("GT1", "G2", "B2", "GT2")]
                for slot, dest, dn, mode, gv in ((2, GT1, "GT1", "plain", None), (4, G2, "G2", "scale", I["norm2_g"]),
                                                 (3, B2, "B2", "plain", None), (5, GT2, "GT2", "plain", None)):
                    with contextlib.ExitStack() as st2:
                        mods_piece(st2, b, slot, dest, dn, mode, gv)
                        P.barrier()
                with contextlib.ExitStack() as s4:
                    YR = mk(s4, "sb", [128, 4, S], BF16, "YR")
                    YM = mk(s4, "sb", [128, NT, 512], BF16, "YM")
                    WO = mk(s4, "sb", [128, 8, 1024], BF16, "WO")
                    WR = mk(s4, "sb", [128, 8, NEXP], F32, "WR")
                    BRb = mk(s4, "sb", [128, NEXP], F32, "BRb")
                    BD = mk(s4, "sb", [32, 1024], F32, "BD")
                    P.dma("sp", lambda e: e.dma_start(out=YR[:], in_=ymixT[b].rearrange("(c p) t -> p c t", p=128)), reads=[], writes=["YR"])
                    P.dma("sp", lambda e: e.dma_start(out=YM[:], in_=ymoba[b].rearrange("(t p) c -> p t c", p=128)), reads=[], writes=["YM"])
                    P.dma("pool", lambda e: e.dma_start(out=WO[:], in_=I["w_out"].rearrange("(k p) n -> p k n", p=128)), writes=["WO"])
                    P.dma("sp", lambda e: e.dma_start(out=WR[:], in_=I["w_router"].rearrange("(k p) n -> p k n", p=128)), writes=["WR"])
                    P.dma("sp", lambda e: e.dma_start(out=BRb[:], in_=I["b_router"].partition_broadcast(128)), writes=["BRb"])
                    P.dma("sp", lambda e: e.dma_start(out=BD[:], in_=I["b_down"]), writes=["BD"])
                    nb2 = 2
                    xt1 = mk(s4, "sb", [128, 1024], F32, "xt4")
                    xt = [xt1, xt1]
                    xm = [mk(s4, "sb", [128, 1024], F32, "xm4") for _ in range(nb2)]
                    h2f = [mk(s4, "sb", [128, 1024], F32, "h2f") for _ in range(nb2)]
                    ss = [mk(s4, "sb", [128, 2], F32, "ss4") for _ in range(nb2)]
                    YMT = [mk(s4, "sb", [128, 4, 128], BF16, "YMT") for _ in range(nb2)]
                    h2Tf1 = mk(s4, "sb", [128, 8, 128], F32, "h2Tf")
                    h2Tf = [h2Tf1, h2Tf1]
                    LG = [mk(s4, "sb", [128, NEXP], F32, "LG") for _ in range(nb2)]
                    EX = [mk(s4, "sb", [128, NEXP], F32, "EX") for _ in range(nb2)]
                    MK = [mk(s4, "sb", [128, NEXP], F32, "MK") for _ in range(nb2)]
                    MX8 = [mk(s4, "sb", [128, 8], F32, "MX8") for _ in range(nb2)]
                    sm = [mk(s4, "sb", [128, 4], F32, "sm4") for _ in range(nb2)]
                    gTs = [mk(s4, "sb", [32, 128], F32, "gTs") for _ in range(nb2)]
                    PO = [mk(s4, "ps", [128, 512], F32, "PSo4") for _ in range(2)]
                    PTm = mk(s4, "ps", [128, 8, 128], BF16, "PStm")
                    PTf = [mk(s4, "ps", [128, 4, 128], F32, "PStf") for _ in range(2)]
                    PL = mk(s4, "ps", [128, 512], F32, "PSl")
                    PGt_ = mk(s4, "ps", [128, 512], F32, "PSgt")
                    hsl = lambda h: slice(h * 512, (h + 1) * 512)
                    for tt in range(NT):
                        i = tt % nb2
                        u = "s4_%d_" % i
                        tsl = slice(tt * 128, (tt + 1) * 128)
                        for c in range(4):
                            P.op("pe", (lambda e, c=c, tt=tt: e.transpose(PTm[:, c, :], YM[:, tt, c * 128:(c + 1) * 128], identb[:])),
                                 reads=["YM", "identb"], writes=["PStm"])
                        P.op("act", (lambda e, i=i: e.copy(out=YMT[i][:], in_=PTm[:, 0:4, :])), reads=["PStm"], writes=[u + "YMT"])
                        P.dma("sp", (lambda e, i=i, tt=tt: e.dma_start(out=xt[i][:], in_=I["x"][b, tt * 128:(tt + 1) * 128, :])), writes=["s4_xt"])
                        for h in range(2):
                            for kc in range(8):
                                P.op("pe", (lambda e, i=i, h=h, kc=kc, tsl=tsl: e.matmul(
                                    PO[h][:], lhsT=(YR[:, kc, tsl] if kc < 4 else YMT[i][:, kc - 4, :]), rhs=WO[:, kc, hsl(h)],
                                    start=(kc == 0), stop=(kc == 7))), reads=["YR", u + "YMT", "WO"], writes=["PSo4%d" % h])
                            P.op("dve", (lambda e, i=i, h=h: e.tensor_tensor(out=xm[i][:, hsl(h)], in0=PO[h][:], in1=GT1[:, hsl(h)], op=ALU.mult)),
                                 reads=["PSo4%d" % h, "GT1"], writes=[u + "xm"])
                        P.op("pool", (lambda e, i=i: e.tensor_tensor(out=xm[i][:], in0=xm[i][:], in1=xt[i][:], op=ALU.add)),
                             reads=[u + "xm", "s4_xt"], writes=[u + "xm"])
                        P.dma("sp", (lambda e, i=i, tt=tt: e.dma_start(out=xmid[b, tt * 128:(tt + 1) * 128, :], in_=xm[i][:])),
                              reads=[u + "xm"], writes=[])
                        P.op("act", (lambda e, i=i: e.activation(out=h2f[i][:], in_=xm[i][:], func=AF.Square, accum_out=ss[i][:, 0:1])),
                             reads=[u + "xm"], writes=[u + "h2f", u + "ss"])
                        P.op("dve", (lambda e, i=i: e.tensor_scalar(out=ss[i][:, 1:2], in0=ss[i][:, 0:1], scalar1=1.0 / D, scalar2=1e-6,
                                                                    op0=ALU.mult, op1=ALU.add)), reads=[u + "ss"], writes=[u + "ss"])
                        P.op("act", (lambda e, i=i: e.activation(out=ss[i][:, 1:2], in_=ss[i][:, 1:2], func=AF.Sqrt)), reads=[u + "ss"], writes=[u + "ss"])
                        P.op("dve", (lambda e, i=i: e.reciprocal(out=ss[i][:, 1:2], in_=ss[i][:, 1:2])), reads=[u + "ss"], writes=[u + "ss"])
                        P.op("dve", (lambda e, i=i: e.scalar_tensor_tensor(out=h2f[i][:], in0=xm[i][:], scalar=ss[i][:, 1:2], in1=G2[:],
                                                                           op0=ALU.mult, op1=ALU.mult)), reads=[u + "xm", u + "ss", "G2"], writes=[u + "h2f"])
                        P.op("pool", (lambda e, i=i: e.tensor_tensor(out=h2f[i][:], in0=h2f[i][:], in1=B2[:], op=ALU.add)),
                             reads=[u + "h2f", "B2"], writes=[u + "h2f"])
                        for k in range(8):
                            P.op("pe", (lambda e, i=i, k=k: e.transpose(PTf[k // 4][:, k % 4, :], h2f[i][:, k * 128:(k + 1) * 128], C["ident"][:])),
                                 reads=[u + "h2f", "c_ident"], writes=["PStf%d" % (k // 4)])
                        for g in range(2):
                            P.op("act", (lambda e, i=i, g=g: e.copy(out=h2Tf[i][:, g * 4:(g + 1) * 4, :], in_=PTf[g][:])),
                                 reads=["PStf%d" % g], writes=["s4_h2Tf"])
                            P.op("dve", (lambda e, g=g, tsl=tsl: e.tensor_copy(out=h2T[:, g * 4:(g + 1) * 4, tsl], in_=PTf[g][:])),
                                 reads=["PStf%d" % g], writes=["h2T"])
                        for k in range(8):
                            P.op("pe", (lambda e, i=i, k=k: e.matmul(PL[:, 0:NEXP], lhsT=h2Tf[i][:, k, :], rhs=WR[:, k, :], start=(k == 0), stop=(k == 7))),
                                 reads=["s4_h2Tf", "WR"], writes=["PSl"])
                        P.op("dve", (lambda e, i=i: e.tensor_tensor(out=LG[i][:], in0=PL[:, 0:NEXP], in1=BRb[:], op=ALU.add)),
                             reads=["PSl", "BRb"], writes=[u + "LG"])
                        P.op("dve", (lambda e, i=i: e.max(out=MX8[i][:], in_=LG[i][:])), reads=[u + "LG"], writes=[u + "MX8"])
                        P.op("dve", (lambda e, i=i: e.tensor_scalar(out=sm[i][:, 0:1], in0=MX8[i][:, 0:1], scalar1=-1.0, scalar2=None, op0=ALU.mult)),
                             reads=[u + "MX8"], writes=[u + "sm"])
                        P.op("act", (lambda e, i=i: e.activation(out=EX[i][:], in_=LG[i][:], func=AF.Exp, bias=sm[i][:, 0:1])),
                             reads=[u + "LG", u + "sm"], writes=[u + "EX"])
                        P.op("dve", (lambda e, i=i: e.tensor_scalar(out=MK[i][:], in0=LG[i][:], scalar1=MX8[i][:, 3:4], scalar2=None, op0=ALU.is_ge)),
                             reads=[u + "LG", u + "MX8"], writes=[u + "MK"])
                        P.op("dve", (lambda e, i=i: e.tensor_tensor(out=EX[i][:], in0=EX[i][:], in1=MK[i][:], op=ALU.mult)),
                             reads=[u + "EX", u + "MK"], writes=[u + "EX"])
                        P.op("dve", (lambda e, i=i: e.tensor_reduce(out=sm[i][:, 1:2], in_=EX[i][:], axis=AX.X, op=ALU.add)),
                             reads=[u + "EX"], writes=[u + "sm"])
                        P.op("dve", (lambda e, i=i: e.reciprocal(out=sm[i][:, 2:3], in_=sm[i][:, 1:2])), reads=[u + "sm"], writes=[u + "sm"])
                        P.op("dve", (lambda e, i=i, tt=tt: e.tensor_scalar(out=GW[:, tt, :], in0=EX[i][:], scalar1=sm[i][:, 2:3], scalar2=None, op0=ALU.mult)),
                             reads=[u + "EX", u + "sm"], writes=["GW"])
                        P.op("pe", (lambda e, tt=tt: e.transpose(PGt_[0:32, 0:128], GW[:, tt, :], C["ident"][:])), reads=["GW", "c_ident"], writes=["PSgt"])
                        P.op("act", (lambda e, i=i: e.copy(out=gTs[i][:], in_=PGt_[0:32, 0:128])), reads=["PSgt"], writes=[u + "gTs"])
                        for h in range(2):
                            P.op("pe", (lambda e, i=i, h=h: e.matmul(PO[h][:], lhsT=gTs[i][:], rhs=BD[:, hsl(h)], start=True, stop=True)),
                                 reads=[u + "gTs", "BD"], writes=["PSo4%d" % h])
                            P.op("act", (lambda e, h=h, tt=tt: e.copy(out=ACCM[:, tt, hsl(h)], in_=PO[h][:])), reads=["PSo4%d" % h], writes=["ACCM%d" % tt])
                        if debug and b == 0:
                            P.dma("sp", (lambda e, i=i, tt=tt: e.dma_start(out=dbg["d_xm"][tt * 128:(tt + 1) * 128, :], in_=xm[i][:])), reads=[u + "xm"])
                            P.dma("sp", (lambda e, tt=tt: e.dma_start(out=dbg["d_gw"][tt * 128:(tt + 1) * 128, :], in_=GW[:, tt, :])), reads=["GW"])
                    P.barrier()
                if stages <= 4:
                    P.barrier()
                    return
                with contextlib.ExitStack() as s5:
                    ACT_T = mk(s5, "sb", [128, 8, S], BF16, "ACT_T")
                    NG, NDR = 3, 11
                    WGUr = [mk(s5, "sb", [128, 8, 256], BF16, "WGUr") for _ in range(NG)]
                    WDr = [mk(s5, "sb", [128, 1024], BF16, "WDr") for _ in range(NDR)]
                    BGU = mk(s5, "sb", [128, NEXP, 16], F32, "BGU")
                    BGS = mk(s5, "sb", [128, NEXP, 16], F32, "BGS")
                    P.dma("sp", lambda e: e.dma_start(out=BGU[:], in_=I["bgu_l"]), writes=["BGU"])
                    P.op("dve", lambda e: e.tensor_scalar(out=BGS[:], in0=BGU[:], scalar1=1.702, scalar2=None, op0=ALU.mult), reads=["BGU"], writes=["BGS"])
                    sgt = [mk(s5, "sb", [128, 512], F32, "sgt") for _ in range(2)]
                    gtt = [mk(s5, "sb", [128, 512], F32, "gtt") for _ in range(2)]
                    upt = [mk(s5, "sb", [128, 512], F32, "upt") for _ in range(2)]
                    PGm = [mk(s5, "ps", [128, 512], F32, "PSgm") for _ in range(2)]
                    PUm = [mk(s5, "ps", [128, 512], F32, "PSum") for _ in range(2)]
                    POm = [mk(s5, "ps", [128, 512], F32, "PSom") for _ in range(2)]
                    SIGC = 0.9999933243
                    uc = [0]
                    oc = [0]
                    nexp = t5.get("nexp", NEXP)

                    def load_piece(idx):
                        e_, fc = idx // 8, idx % 8
                        if e_ >= nexp:
                            return
                        P.dma("pool", (lambda e, e_=e_, fc=fc, idx=idx: e.dma_start(out=WGUr[idx % NG][:], in_=I["wgu_l"][e_, fc])),
                              writes=["WGU%d" % (idx % NG)])
                        P.dma("pool", (lambda e, e_=e_, fc=fc, idx=idx: e.dma_start(out=WDr[idx % NDR][:], in_=I["w_down"][e_, fc * 128:(fc + 1) * 128, :])),
                              writes=["WD%d" % (idx % NDR)])
                    LOOK = 2
                    for idx in range(LOOK):
                        load_piece(idx)
                    for ex in range(nexp):
                        for fc in range(8):
                            idx = ex * 8 + fc
                            load_piece(idx + LOOK)
                            wg = WGUr[idx % NG]
                            wn = "WGU%d" % (idx % NG)
                            for tg in range(4):
                                j = uc[0] % 2
                                uc[0] += 1
                                tsl = slice(tg * 512, (tg + 1) * 512)
                                for k in range(8):
                                    P.op("pe", (lambda e, wg=wg, k=k, tsl=tsl, j=j: e.matmul(PGm[j][:], lhsT=wg[:, k, 0:128], rhs=h2T[:, k, tsl],
                                                                                           start=(k == 0), stop=(k == 7))),
                                         reads=[wn, "h2T"], writes=["PSgm%d" % j])
                                for k in range(8):
                                    P.op("pe", (lambda e, wg=wg, k=k, tsl=tsl, j=j: e.matmul(PUm[j][:], lhsT=wg[:, k, 128:256], rhs=h2T[:, k, tsl],
                                                                                           start=(k == 0), stop=(k == 7))),
                                         reads=[wn, "h2T"], writes=["PSum%d" % j])
                                bg = BGU[:, ex, fc:fc + 1]
                                bu = BGU[:, ex, 8 + fc:9 + fc]
                                bs = BGS[:, ex, fc:fc + 1]
                                P.op("act", (lambda e, j=j, bs=bs: e.activation(out=sgt[j][:], in_=PGm[j][:], func=AF.Sigmoid, bias=bs, scale=1.702)),
                                     reads=["PSgm%d" % j, "BGS"], writes=["sgt%d" % j])
                                P.op("dve", (lambda e, j=j, bg=bg: e.tensor_scalar(out=gtt[j][:], in0=PGm[j][:], scalar1=bg, scalar2=7.0, op0=ALU.add, op1=ALU.min)),
                                     reads=["PSgm%d" % j, "BGU"], writes=["gtt%d" % j])
                                P.op("dve", (lambda e, j=j, bu=bu: e.tensor_scalar(out=upt[j][:], in0=PUm[j][:], scalar1=bu, scalar2=-7.0, op0=ALU.add, op1=ALU.max)),
                                     reads=["PSum%d" % j, "BGU"], writes=["upt%d" % j])
                                P.op("dve", (lambda e, j=j: e.tensor_scalar(out=upt[j][:], in0=upt[j][:], scalar1=7.0, scalar2=1.0, op0=ALU.min, op1=ALU.add)),
                                     reads=["upt%d" % j], writes=["upt%d" % j])
                                P.op("dve", (lambda e, j=j: e.scalar_tensor_tensor(out=gtt[j][:], in0=sgt[j][:], scalar=SIGC, in1=gtt[j][:], op0=ALU.min, op1=ALU.mult)),
                                     reads=["sgt%d" % j, "gtt%d" % j], writes=["gtt%d" % j])
                                P.op("dve", (lambda e, j=j, fc=fc, tsl=tsl: e.tensor_tensor(out=ACT_T[:, fc, tsl], in0=gtt[j][:], in1=upt[j][:], op=ALU.mult)),
                                     reads=["gtt%d" % j, "upt%d" % j], writes=["ACT_T%d" % fc])
                        for tt in range(NT):
                            tsl = slice(tt * 128, (tt + 1) * 128)
                            for h in range(2):
                                j = oc[0] % 2
                                oc[0] += 1
                                for fc in range(8):
                                    wd = WDr[(ex * 8 + fc) % NDR]
                                    P.op("pe", (lambda e, wd=wd, fc=fc, tsl=tsl, j=j, h=h: e.matmul(POm[j][:], lhsT=ACT_T[:, fc, tsl], rhs=wd[:, hsl(h)],
                                                                                                  start=(fc == 0), stop=(fc == 7))),
                                         reads=["ACT_T%d" % fc, "WD%d" % ((ex * 8 + fc) % NDR)], writes=["PSom%d" % j])
                                P.op("dve", (lambda e, j=j, tt=tt, h=h, ex=ex: e.scalar_tensor_tensor(out=ACCM[:, tt, hsl(h)], in0=POm[j][:], scalar=GW[:, tt, ex:ex + 1],
                                                                                                     in1=ACCM[:, tt, hsl(h)], op0=ALU.mult, op1=ALU.add)),
                                     reads=["PSom%d" % j, "GW", "ACCM%d" % tt], writes=["ACCM%d" % tt])
                    P.barrier()
                with contextlib.ExitStack() as s6:
                    xm6 = [mk(s6, "sb", [128, 1024], F32, "xm6") for _ in range(2)]
                    o6 = [mk(s6, "sb", [128, 1024], F32, "o6") for _ in range(2)]
                    for tt in range(NT):
                        i = tt % 2
                        P.dma("sp", (lambda e, i=i, tt=tt: e.dma_start(out=xm6[i][:], in_=xmid[b, tt * 128:(tt + 1) * 128, :])), reads=[], writes=["xm6%d" % i])
                        P.op("dve", (lambda e, i=i, tt=tt: e.tensor_tensor(out=o6[i][:], in0=ACCM[:, tt, :], in1=GT2[:], op=ALU.mult)),
                             reads=["ACCM%d" % tt, "GT2"], writes=["o6%d" % i])
                        P.op("pool", (lambda e, i=i: e.tensor_tensor(out=o6[i][:], in0=o6[i][:], in1=xm6[i][:], op=ALU.add)),
                             reads=["o6%d" % i, "xm6%d" % i], writes=["o6%d" % i])
                        P.dma("sp", (lambda e, i=i, tt=tt: e.dma_start(out=out[b, tt * 128:(tt + 1) * 128, :], in_=o6[i][:])), reads=["o6%d" % i], writes=[])
                    P.barrier()
            P.barrier()

        BASE = mk(gst, "sb", [128, NEXP], F32, "BASE")
        GWa = mk(gst, "sb", [128, NB * NT, NEXP], F32, "GWa")
        SLu = mk(gst, "sb", [128, NB * NT, 4], U32, "SLu")
        GKa = mk(gst, "sb", [128, NB * NT, 4], F32, "GKa")
        GT2a = [mk(gst, "sb", [128, 1024], F32, "GT2a") for _ in range(NB)]
        trib = mk(gst, "sb", [128, 128], BF16, "trib")
        onesb = mk(gst, "sb", [128, 128], BF16, "onesb")
        P.op("dve", lambda e: e.tensor_copy(out=trib[:], in_=C["tri"][:]), reads=["c_tri"], writes=["trib"])
        P.op("pool", lambda e: e.memset(onesb[:], 1.0), writes=["onesb"])
        P.op("dve", lambda e: e.tensor_copy(out=BASE[:], in_=C["ecap"][:]), reads=["c_ecap"], writes=["BASE"])
        ECMAX = mk(gst, "sb", [128, NEXP], F32, "ECMAX")
        P.op("dve", lambda e: e.tensor_scalar(out=ECMAX[:], in0=C["ecap"][:], scalar1=float(CAP - 1), scalar2=None, op0=ALU.add), reads=["c_ecap"], writes=["ECMAX"])
        P.barrier()

        def st4s(b):
            with contextlib.ExitStack() as st:
                GT1, G2, B2 = [mk(st, "sb", [128, 1024], F32, n) for n in ("GT1", "G2", "B2")]
                with contextlib.ExitStack() as st2:
                    for slot, dest, dn, mode, gv in ((2, GT1, "GT1", "plain", None), (4, G2, "G2", "scale", I["norm2_g"]),
                                                     (3, B2, "B2", "plain", None), (5, GT2a[b], "GT2a%d" % b, "plain", None)):
                        mods_piece(st2, b, slot, dest, dn, mode, gv)
                    P.barrier()
                with contextlib.ExitStack() as s4:
                    YR = mk(s4, "sb", [128, 4, S], BF16, "YR")
                    YM = mk(s4, "sb", [128, NT, 512], BF16, "YM")
                    WO = mk(s4, "sb", [128, 8, 1024], BF16, "WO")
                    WR = mk(s4, "sb", [128, 8, NEXP], F32, "WR")
                    BRb = mk(s4, "sb", [128, NEXP], F32, "BRb")
                    P.dma("sp", lambda e: e.dma_start(out=YR[:], in_=ymixT[b].rearrange("(c p) t -> p c t", p=128)), reads=[], writes=["YR"])
                    P.dma("sp", lambda e: e.dma_start(out=YM[:], in_=ymoba[b].rearrange("(t p) c -> p t c", p=128)), reads=[], writes=["YM"])
                    P.dma("pool", lambda e: e.dma_start(out=WO[:], in_=I["w_out"].rearrange("(k p) n -> p k n", p=128)), writes=["WO"])
                    P.dma("sp", lambda e: e.dma_start(out=WR[:], in_=I["w_router"].rearrange("(k p) n -> p k n", p=128)), writes=["WR"])
                    P.dma("sp", lambda e: e.dma_start(out=BRb[:], in_=I["b_router"].partition_broadcast(128)), writes=["BRb"])
                    nb2 = 2
                    xt1 = mk(s4, "sb", [128, 1024], F32, "xt4")
                    xm = [mk(s4, "sb", [128, 1024], F32, "xm4") for _ in range(nb2)]
                    h2f = [mk(s4, "sb", [128, 1024], F32, "h2f") for _ in range(nb2)]
                    h2b = [mk(s4, "sb", [128, 1024], BF16, "h2b") for _ in range(nb2)]
                    ss = [mk(s4, "sb", [128, 2], F32, "ss4") for _ in range(nb2)]
                    YMT = [mk(s4, "sb", [128, 4, 128], BF16, "YMT") for _ in range(nb2)]
                    h2Tf = mk(s4, "sb", [128, 8, 128], F32, "h2Tf")
                    LG = [mk(s4, "sb", [128, NEXP], F32, "LG") for _ in range(nb2)]
                    EX = [mk(s4, "sb", [128, NEXP], F32, "EX") for _ in range(nb2)]
                    MK = [mk(s4, "sb", [128, NEXP], F32, "MK") for _ in range(nb2)]
                    MKb = [mk(s4, "sb", [128, NEXP], BF16, "MKb") for _ in range(nb2)]
                    POSE = [mk(s4, "sb", [128, NEXP], F32, "POSE") for _ in range(nb2)]
                    TMP = [mk(s4, "sb", [128, NEXP], F32, "TMP") for _ in range(nb2)]
                    MX8 = [mk(s4, "sb", [128, 8], F32, "MX8") for _ in range(nb2)]
                    IX8 = [mk(s4, "sb", [128, 8], U32, "IX8") for _ in range(nb2)]
                    IXf = [mk(s4, "sb", [128, 8], F32, "IXf") for _ in range(nb2)]
                    SLf = [mk(s4, "sb", [128, 4], F32, "SLf") for _ in range(nb2)]
                    sm = [mk(s4, "sb", [128, 4], F32, "sm4") for _ in range(nb2)]
                    PO = [mk(s4, "ps", [128, 512], F32, "PSo4") for _ in range(2)]
                    PTm = mk(s4, "ps", [128, 8, 128], BF16, "PStm")
                    PTf = [mk(s4, "ps", [128, 4, 128], F32, "PStf") for _ in range(2)]
                    PL = mk(s4, "ps", [128, 512], F32, "PSl")
                    PC = mk(s4, "ps", [128, 512], F32, "PSc")
                    hsl = lambda h: slice(h * 512, (h + 1) * 512)
                    def phaseA(tt):
                        i = tt % nb2
                        gt = b * NT + tt
                        u = "s4_%d_" % i
                        tsl = slice(tt * 128, (tt + 1) * 128)
                        for c in range(4):
                            P.op("pe", (lambda e, c=c, tt=tt: e.transpose(PTm[:, c, :], YM[:, tt, c * 128:(c + 1) * 128], identb[:])),
                                 reads=["YM", "identb"], writes=["PStm"])
                        P.op("act", (lambda e, i=i: e.copy(out=YMT[i][:], in_=PTm[:, 0:4, :])), reads=["PStm"], writes=[u + "YMT"])
                        P.dma("sp", (lambda e, tt=tt: e.dma_start(out=xt1[:], in_=I["x"][b, tt * 128:(tt + 1) * 128, :])), writes=["s4_xt"])
                        for h in range(2):
                            for kc in range(8):
                                P.op("pe", (lambda e, i=i, h=h, kc=kc, tsl=tsl: e.matmul(
                                    PO[h][:], lhsT=(YR[:, kc, tsl] if kc < 4 else YMT[i][:, kc - 4, :]), rhs=WO[:, kc, hsl(h)],
                                    start=(kc == 0), stop=(kc == 7))), reads=["YR", u + "YMT", "WO"], writes=["PSo4%d" % h])
                            P.op("dve", (lambda e, i=i, h=h: e.tensor_tensor(out=xm[i][:, hsl(h)], in0=PO[h][:], in1=GT1[:, hsl(h)], op=ALU.mult)),
                                 reads=["PSo4%d" % h, "GT1"], writes=[u + "xm"])
                        P.op("dve", (lambda e, i=i: e.tensor_tensor(out=xm[i][:], in0=xm[i][:], in1=xt1[:], op=ALU.add)),
                             reads=[u + "xm", "s4_xt"], writes=[u + "xm"])
                        P.dma("sp", (lambda e, i=i, tt=tt: e.dma_start(out=xmid[b, tt * 128:(tt + 1) * 128, :], in_=xm[i][:])),
                              reads=[u + "xm"], writes=[])
                        P.op("act", (lambda e, i=i: e.activation(out=h2f[i][:], in_=xm[i][:], func=AF.Square, accum_out=ss[i][:, 0:1])),
                             reads=[u + "xm"], writes=[u + "h2f", u + "ss"])
                        P.op("dve", (lambda e, i=i: e.tensor_scalar(out=ss[i][:, 1:2], in0=ss[i][:, 0:1], scalar1=1.0 / D, scalar2=1e-6,
                                                                    op0=ALU.mult, op1=ALU.add)), reads=[u + "ss"], writes=[u + "ss"])
                        P.op("act", (lambda e, i=i: e.activation(out=ss[i][:, 1:2], in_=ss[i][:, 1:2], func=AF.Sqrt)), reads=[u + "ss"], writes=[u + "ss"])
                        P.op("dve", (lambda e, i=i: e.reciprocal(out=ss[i][:, 1:2], in_=ss[i][:, 1:2])), reads=[u + "ss"], writes=[u + "ss"])
                        P.op("dve", (lambda e, i=i: e.scalar_tensor_tensor(out=h2f[i][:], in0=xm[i][:], scalar=ss[i][:, 1:2], in1=G2[:],
                                                                           op0=ALU.mult, op1=ALU.mult)), reads=[u + "xm", u + "ss", "G2"], writes=[u + "h2f"])
                        P.op("dve", (lambda e, i=i: e.tensor_tensor(out=h2f[i][:], in0=h2f[i][:], in1=B2[:], op=ALU.add)),
                             reads=[u + "h2f", "B2"], writes=[u + "h2f"])
                        P.op("act", (lambda e, i=i: e.copy(out=h2b[i][:], in_=h2f[i][:])), reads=[u + "h2f"], writes=[u + "h2b"])
                        for k in range(8):
                            P.op("pe", (lambda e, i=i, k=k: e.transpose(PTf[k // 4][:, k % 4, :], h2f[i][:, k * 128:(k + 1) * 128], C["ident"][:])),
                                 reads=[u + "h2f", "c_ident"], writes=["PStf%d" % (k // 4)])
                        for g in range(2):
                            P.op("act", (lambda e, g=g: e.copy(out=h2Tf[:, g * 4:(g + 1) * 4, :], in_=PTf[g][:])),
                                 reads=["PStf%d" % g], writes=["s4_h2Tf"])
                        for k in range(8):
                            P.op("pe", (lambda e, k=k: e.matmul(PL[:, 0:NEXP], lhsT=h2Tf[:, k, :], rhs=WR[:, k, :], start=(k == 0), stop=(k == 7))),
                                 reads=["s4_h2Tf", "WR"], writes=["PSl"])
                        P.op("dve", (lambda e, i=i: e.tensor_tensor(out=LG[i][:], in0=PL[:, 0:NEXP], in1=BRb[:], op=ALU.add)),
                             reads=["PSl", "BRb"], writes=[u + "LG"])

                    def phaseB(tt):
                        i = tt % nb2
                        gt = b * NT + tt
                        u = "s4_%d_" % i
                        P.op("dve", (lambda e, i=i: e.max(out=MX8[i][:], in_=LG[i][:])), reads=[u + "LG"], writes=[u + "MX8"])
                        P.op("dve", (lambda e, i=i: e.max_index(out=IX8[i][:], in_max=MX8[i][:], in_values=LG[i][:])),
                             reads=[u + "LG", u + "MX8"], writes=[u + "IX8"])
                        P.op("dve", (lambda e, i=i: e.tensor_copy(out=IXf[i][:], in_=IX8[i][:])), reads=[u + "IX8"], writes=[u + "IXf"])
                        P.op("dve", (lambda e, i=i: e.tensor_scalar(out=sm[i][:, 0:1], in0=MX8[i][:, 0:1], scalar1=-1.0, scalar2=None, op0=ALU.mult)),
                             reads=[u + "MX8"], writes=[u + "sm"])
                        P.op("act", (lambda e, i=i: e.activation(out=EX[i][:], in_=LG[i][:], func=AF.Exp, bias=sm[i][:, 0:1])),
                             reads=[u + "LG", u + "sm"], writes=[u + "EX"])
                        P.op("dve", (lambda e, i=i: e.tensor_scalar(out=MK[i][:], in0=LG[i][:], scalar1=MX8[i][:, 3:4], scalar2=None, op0=ALU.is_ge)),
                             reads=[u + "LG", u + "MX8"], writes=[u + "MK"])
                        P.op("dve", (lambda e, i=i: e.tensor_copy(out=MKb[i][:], in_=MK[i][:])), reads=[u + "MK"], writes=[u + "MKb"])
                        P.op("dve", (lambda e, i=i: e.tensor_tensor(out=EX[i][:], in0=EX[i][:], in1=MK[i][:], op=ALU.mult)),
                             reads=[u + "EX", u + "MK"], writes=[u + "EX"])
                        P.op("dve", (lambda e, i=i: e.tensor_reduce(out=sm[i][:, 1:2], in_=EX[i][:], axis=AX.X, op=ALU.add)),
                             reads=[u + "EX"], writes=[u + "sm"])
                        P.op("dve", (lambda e, i=i: e.reciprocal(out=sm[i][:, 2:3], in_=sm[i][:, 1:2])), reads=[u + "sm"], writes=[u + "sm"])
                        P.op("dve", (lambda e, i=i, gt=gt: e.tensor_scalar(out=GWa[:, gt, :], in0=EX[i][:], scalar1=sm[i][:, 2:3], scalar2=None, op0=ALU.mult)),
                             reads=[u + "EX", u + "sm"], writes=["GWa"])
                        P.op("pe", (lambda e, i=i: e.matmul(PC[:, 0:NEXP], lhsT=trib[:], rhs=MKb[i][:], start=True, stop=True)),
                             reads=["trib", u + "MKb"], writes=["PSc"])
                        P.op("pe", (lambda e, i=i: e.matmul(PC[:, NEXP:2 * NEXP], lhsT=onesb[:], rhs=MKb[i][:], start=True, stop=True)),
                             reads=["onesb", u + "MKb"], writes=["PSc"])
                        P.op("dve", (lambda e, i=i: e.tensor_tensor(out=POSE[i][:], in0=PC[:, 0:NEXP], in1=BASE[:], op=ALU.add)),
                             reads=["PSc", "BASE"], writes=[u + "POSE"])
                        P.op("dve", lambda e: e.tensor_tensor(out=BASE[:], in0=PC[:, NEXP:2 * NEXP], in1=BASE[:], op=ALU.add),
                             reads=["PSc", "BASE"], writes=["BASE"])
                        P.op("dve", (lambda e, i=i: e.tensor_tensor(out=POSE[i][:], in0=POSE[i][:], in1=ECMAX[:], op=ALU.min)),
                             reads=[u + "POSE", "ECMAX"], writes=[u + "POSE"])
                        P.op("dve", (lambda e, i=i: e.memset(SLf[i][:], 0.0)), writes=[u + "SLf"])
                        P.op("dve", (lambda e, gt=gt: e.memset(GKa[:, gt, :], 0.0)), writes=["GKa"])
                        for k in range(4):
                            P.op("dve", (lambda e, i=i, k=k: e.scalar_tensor_tensor(out=TMP[i][:], in0=C["iota32"][:], scalar=IXf[i][:, k:k + 1], in1=POSE[i][:],
                                                                                    op0=ALU.is_equal, op1=ALU.mult, accum_out=SLf[i][:, k:k + 1])),
                                 reads=["c_iota32", u + "IXf", u + "POSE", u + "SLf"], writes=[u + "TMP", u + "SLf"])
                            P.op("dve", (lambda e, i=i, k=k, gt=gt: e.scalar_tensor_tensor(out=TMP[i][:], in0=C["iota32"][:], scalar=IXf[i][:, k:k + 1], in1=GWa[:, gt, :],
                                                                                           op0=ALU.is_equal, op1=ALU.mult, accum_out=GKa[:, gt, k:k + 1])),
                                 reads=["c_iota32", u + "IXf", "GWa", "GKa"], writes=[u + "TMP", "GKa"])
                        P.op("dve", (lambda e, i=i, gt=gt: e.tensor_copy(out=SLu[:, gt, :], in_=SLf[i][:])), reads=[u + "SLf"], writes=["SLu"])
                        for k in range(4):
                            P.dma("pool", (lambda e, i=i, k=k, gt=gt: e.indirect_dma_start(
                                out=XS, out_offset=bass.IndirectOffsetOnAxis(ap=SLu[:, gt, k:k + 1], axis=0), in_=h2b[i][:], in_offset=None)),
                                  reads=[u + "h2b", "SLu"], writes=[])
                        if debug and b == 0:
                            P.dma("sp", (lambda e, i=i, tt=tt: e.dma_start(out=dbg["d_xm"][tt * 128:(tt + 1) * 128, :], in_=xm[i][:])), reads=[u + "xm"])
                            P.dma("sp", (lambda e, gt=gt, tt=tt: e.dma_start(out=dbg["d_gw"][tt * 128:(tt + 1) * 128, :], in_=GWa[:, gt, :])), reads=["GWa"])

                    phaseA(0)
                    for tt in range(NT):
                        if tt + 1 < NT:
                            phaseA(tt + 1)
                        phaseB(tt)
                    P.barrier()
            P.barrier()

        def moe_sparse():
            with contextlib.ExitStack() as s5:
                XT = mk(s5, "sb", [128, 8, CAP], BF16, "XT")
                ACT_T = mk(s5, "sb", [128, 8, CAP], BF16, "ACT_T")
                NG, NDR = 3, 11
                WGUr = [mk(s5, "sb", [128, 8, 256], BF16, "WGUr") for _ in range(NG)]
                WDr = [mk(s5, "sb", [128, 1024], BF16, "WDr") for _ in range(NDR)]
                BGU = mk(s5, "sb", [128, NEXP, 16], F32, "BGU")
                BGS = mk(s5, "sb", [128, NEXP, 16], F32, "BGS")
                P.dma("sp", lambda e: e.dma_start(out=BGU[:], in_=I["bgu_l"]), writes=["BGU"])
                P.op("dve", lambda e: e.tensor_scalar(out=BGS[:], in0=BGU[:], scalar1=1.702, scalar2=None, op0=ALU.mult), reads=["BGU"], writes=["BGS"])
                BG1 = mk(s5, "sb", [128, NEXP, 16], F32, "BG1")
                P.op("dve", lambda e: e.tensor_scalar(out=BG1[:], in0=BGU[:], scalar1=1.0, scalar2=None, op0=ALU.add), reads=["BGU"], writes=["BG1"])
                XR = [mk(s5, "sb", [128, 1024], BF16, "XR") for _ in range(2)]
                YO = [mk(s5, "sb", [128, 1024], F32, "YO") for _ in range(2)]
                sgt = [mk(s5, "sb", [128, 512], F32, "sgt") for _ in range(2)]
                gtt = [mk(s5, "sb", [128, 512], F32, "gtt") for _ in range(2)]
                upt = [mk(s5, "sb", [128, 512], F32, "upt") for _ in range(2)]
                PGm = [mk(s5, "ps", [128, 512], F32, "PSgm") for _ in range(2)]
                PUm = [mk(s5, "ps", [128, 512], F32, "PSum") for _ in range(2)]
                POm = [mk(s5, "ps", [128, 512], F32, "PSom") for _ in range(2)]
                PTx = mk(s5, "ps", [128, 8, 128], BF16, "PStx")
                SIGC = 0.9999933243
                hsl = lambda h: slice(h * 512, (h + 1) * 512)
                uc = [0]
                oc = [0]
                xc = [0]
                nexp = t5.get("nexp", NEXP)
                NST = CAP // 128

                def load_piece(idx):
                    e_, fc = idx // 8, idx % 8
                    if e_ >= nexp:
                        return
                    P.dma("pool", (lambda e, e_=e_, fc=fc, idx=idx: e.dma_start(out=WGUr[idx % NG][:], in_=I["wgu_l"][e_, fc])),
                          writes=["WGU%d" % (idx % NG)])
                    P.dma("pool", (lambda e, e_=e_, fc=fc, idx=idx: e.dma_start(out=WDr[idx % NDR][:], in_=I["w_down"][e_, fc * 128:(fc + 1) * 128, :])),
                          writes=["WD%d" % (idx % NDR)])
                LOOK = 2
                for idx in range(LOOK):
                    load_piece(idx)
                def build_xt(ex, st_):
                    i = xc[0] % 2
                    xc[0] += 1
                    r0 = ex * CAP + st_ * 128
                    P.dma("sp", (lambda e, i=i, r0=r0: e.dma_start(out=XR[i][:], in_=XS[r0:r0 + 128, :])), reads=[], writes=["XR%d" % i])
                    for k in range(8):
                        P.op("pe", (lambda e, i=i, k=k: e.transpose(PTx[:, k, :], XR[i][:, k * 128:(k + 1) * 128], identb[:])),
                             reads=["XR%d" % i, "identb"], writes=["PStx"])
                    P.op("act", (lambda e, st_=st_: e.copy(out=XT[:, :, st_ * 128:(st_ + 1) * 128], in_=PTx[:])), reads=["PStx"], writes=["XT"])

                for st_ in range(NST):
                    build_xt(0, st_)
                for ex in range(nexp):
                    for fc in range(8):
                        idx = ex * 8 + fc
                        load_piece(idx + LOOK)
                        wg = WGUr[idx % NG]
                        wn = "WGU%d" % (idx % NG)
                        for tg in range(CAP // 512):
                            j = uc[0] % 2
                            uc[0] += 1
                            tsl = slice(tg * 512, (tg + 1) * 512)
                            for k in range(8):
                                P.op("pe", (lambda e, wg=wg, k=k, tsl=tsl, j=j: e.matmul(PGm[j][:], lhsT=wg[:, k, 0:128], rhs=XT[:, k, tsl],
                                                                                       start=(k == 0), stop=(k == 7))),
                                     reads=[wn, "XT"], writes=["PSgm%d" % j])
                            for k in range(8):
                                P.op("pe", (lambda e, wg=wg, k=k, tsl=tsl, j=j: e.matmul(PUm[j][:], lhsT=wg[:, k, 128:256], rhs=XT[:, k, tsl],
                                                                                       start=(k == 0), stop=(k == 7))),
                                     reads=[wn, "XT"], writes=["PSum%d" % j])
                            bg = BGU[:, ex, fc:fc + 1]
                            bu = BG1[:, ex, 8 + fc:9 + fc]
                            bs = BGS[:, ex, fc:fc + 1]
                            P.op("act", (lambda e, j=j, bs=bs: e.activation(out=sgt[j][:], in_=PGm[j][:], func=AF.Sigmoid, bias=bs, scale=1.702)),
                                 reads=["PSgm%d" % j, "BGS"], writes=["sgt%d" % j])
                            P.op("dve", (lambda e, j=j, bg=bg: e.tensor_scalar(out=gtt[j][:], in0=PGm[j][:], scalar1=bg, scalar2=7.0, op0=ALU.add, op1=ALU.min)),
                                 reads=["PSgm%d" % j, "BGU"], writes=["gtt%d" % j])
                            P.op("act", (lambda e, j=j, bu=bu: e.activation(out=upt[j][:], in_=PUm[j][:], func=AF.Identity, bias=bu)),
                                 reads=["PSum%d" % j, "BG1"], writes=["upt%d" % j])
                            P.op("dve", (lambda e, j=j: e.tensor_scalar(out=upt[j][:], in0=upt[j][:], scalar1=-6.0, scalar2=8.0, op0=ALU.max, op1=ALU.min)),
                                 reads=["upt%d" % j], writes=["upt%d" % j])
                            P.op("dve", (lambda e, j=j: e.scalar_tensor_tensor(out=gtt[j][:], in0=sgt[j][:], scalar=SIGC, in1=gtt[j][:], op0=ALU.min, op1=ALU.mult)),
                                 reads=["sgt%d" % j, "gtt%d" % j], writes=["gtt%d" % j])
                            P.op("dve", (lambda e, j=j, fc=fc, tsl=tsl: e.tensor_tensor(out=ACT_T[:, fc, tsl], in0=gtt[j][:], in1=upt[j][:], op=ALU.mult)),
                                 reads=["gtt%d" % j, "upt%d" % j], writes=["ACT_T%d" % fc])
                    for st_ in range(NST):
                        tsl = slice(st_ * 128, (st_ + 1) * 128)
                        yi = st_ % 2
                        for h in range(2):
                            j = oc[0] % 2
                            oc[0] += 1
                            for fc in range(8):
                                wd = WDr[(ex * 8 + fc) % NDR]
                                P.op("pe", (lambda e, wd=wd, fc=fc, tsl=tsl, j=j, h=h: e.matmul(POm[j][:], lhsT=ACT_T[:, fc, tsl], rhs=wd[:, hsl(h)],
                                                                                              start=(fc == 0), stop=(fc == 7))),
                                     reads=["ACT_T%d" % fc, "WD%d" % ((ex * 8 + fc) % NDR)], writes=["PSom%d" % j])
                            P.op("act", (lambda e, j=j, h=h, yi=yi: e.copy(out=YO[yi][:, hsl(h)], in_=POm[j][:])), reads=["PSom%d" % j], writes=["YO%d" % yi])
                        r0 = ex * CAP + st_ * 128
                        P.dma("act", (lambda e, yi=yi, r0=r0: e.dma_start(out=YS[r0:r0 + 128, :], in_=YO[yi][:])), reads=["YO%d" % yi], writes=[])
                        if ex + 1 < nexp:
                            build_xt(ex + 1, st_)
                P.barrier()

        def final_sparse():
            with contextlib.ExitStack() as s6:
                BD = mk(s6, "sb", [32, 1024], F32, "BD")
                P.dma("sp", lambda e: e.dma_start(out=BD[:], in_=I["b_down"]), writes=["BD"])
                gTs = [mk(s6, "sb", [32, 128], F32, "gTs") for _ in range(2)]
                acc = [mk(s6, "sb", [128, 1024], F32, "acc6") for _ in range(2)]
                GB = [mk(s6, "sb", [128, 1024], F32, "GB6") for _ in range(8)]
                xm6 = [mk(s6, "sb", [128, 1024], F32, "xm6") for _ in range(2)]
                PGt_ = mk(s6, "ps", [128, 512], F32, "PSgt")
                PO = [mk(s6, "ps", [128, 512], F32, "PSo6") for _ in range(2)]
                hsl = lambda h: slice(h * 512, (h + 1) * 512)
                gc_ = [0]
                for gt in range(NB * NT):
                    b, tt = gt // NT, gt % NT
                    i = gt % 2
                    P.op("pe", (lambda e, gt=gt: e.transpose(PGt_[0:32, 0:128], GWa[:, gt, :], C["ident"][:])), reads=["GWa", "c_ident"], writes=["PSgt"])
                    P.op("act", (lambda e, i=i: e.copy(out=gTs[i][:], in_=PGt_[0:32, 0:128])), reads=["PSgt"], writes=["gTs%d" % i])
                    for h in range(2):
                        P.op("pe", (lambda e, i=i, h=h: e.matmul(PO[h][:], lhsT=gTs[i][:], rhs=BD[:, hsl(h)], start=True, stop=True)),
                             reads=["gTs%d" % i, "BD"], writes=["PSo6%d" % h])
                        P.op("act", (lambda e, i=i, h=h: e.copy(out=acc[i][:, hsl(h)], in_=PO[h][:])), reads=["PSo6%d" % h], writes=["acc%d" % i])
                    P.dma("sp", (lambda e, i=i, b=b, tt=tt: e.dma_start(out=xm6[i][:], in_=xmid[b, tt * 128:(tt + 1) * 128, :])), reads=[], writes=["xm6%d" % i])
                    for k in range(4):
                        g = gc_[0] % 8
                        gc_[0] += 1
                        P.dma("pool", (lambda e, g=g, gt=gt, k=k: e.indirect_dma_start(
                            out=GB[g][:], out_offset=None, in_=YS, in_offset=bass.IndirectOffsetOnAxis(ap=SLu[:, gt, k:k + 1], axis=0))),
                              reads=["SLu"], writes=["GB%d" % g])
                        P.op("dve", (lambda e, g=g, i=i, gt=gt, k=k: e.scalar_tensor_tensor(out=acc[i][:], in0=GB[g][:], scalar=GKa[:, gt, k:k + 1], in1=acc[i][:],
                                                                                            op0=ALU.mult, op1=ALU.add)),
                             reads=["GB%d" % g, "GKa", "acc%d" % i], writes=["acc%d" % i])
                    P.op("dve", (lambda e, i=i, b=b: e.tensor_tensor(out=acc[i][:], in0=acc[i][:], in1=GT2a[b][:], op=ALU.mult)),
                         reads=["acc%d" % i, "GT2a%d" % b], writes=["acc%d" % i])
                    P.op("dve", (lambda e, i=i: e.tensor_tensor(out=acc[i][:], in0=acc[i][:], in1=xm6[i][:], op=ALU.add)),
                         reads=["acc%d" % i, "xm6%d" % i], writes=["acc%d" % i])
                    P.dma("act", (lambda e, i=i, b=b, tt=tt: e.dma_start(out=out[b, tt * 128:(tt + 1) * 128, :], in_=acc[i][:])), reads=["acc%d" % i], writes=[])
                P.barrier()

        def st1(b):
            with contextlib.ExitStack() as st:
                G1 = mk(st, "sb", [128, 1024], F32, "G1")
                B1 = mk(st, "sb", [128, 1024], F32, "B1")
                with contextlib.ExitStack() as st2:
                    mods_piece(st2, b, 1, G1, "G1", "scale", I["norm1_g"])
                    mods_piece(st2, b, 0, B1, "B1", "plain")
                    P.barrier()
                hT = mk(st, "sb", [128, 8, S + 1], BF16, "hT")
                P.op("pool", lambda e: e.memset(hT[:, :, 0:1], 0.0), writes=["hT0"])
                if True:
                    bufs = [(mk(st, "sb", [128, 1024], F32, "xt"), mk(st, "sb", [128, 1024], BF16, "junk"),
                             mk(st, "sb", [128, 2], F32, "ss"), mk(st, "sb", [128, 1024], F32, "t1")) for _ in range(2)]
                    hbs = [mk(st, "sb", [128, 1024], BF16, "hb") for _ in range(2)]
                    pts = [mk(st, "ps", [128, 8, 128], BF16, "pt") for _ in range(2)]
                    for tt in range(NT):
                        i = tt % 2
                        tag = "n1_%d_" % i
                        norm_tile(bufs[i], I["x"][b, tt * 128:(tt + 1) * 128, :], G1, B1, "G1", "B1", tag, (hbs[i], tag + "hb"))
                        for k in range(8):
                            P.op("pe", (lambda e, i=i, k=k: e.transpose(pts[i][:, k, :], hbs[i][:, k * 128:(k + 1) * 128], identb[:])),
                                 reads=[tag + "hb", "identb"], writes=[tag + "pt"])
                        P.op("act", (lambda e, i=i, tt=tt: e.copy(out=hT[:, :, 1 + tt * 128:1 + (tt + 1) * 128], in_=pts[i][:])),
                             reads=[tag + "pt"], writes=["hT_%d" % (tt // 4)])
                with contextlib.ExitStack() as st2:
                    mub = mk(st2, "sb", [128, 1920], F32, "mub")
                    P.dma("sp", lambda e: e.dma_start(out=mub[:], in_=I["mu_p"].partition_broadcast(128)), writes=["mub"])
                    wf = [mk(st2, "sb", [128, 8, 128], F32, "wf") for _ in range(2)]
                    w1 = [mk(st2, "sb", [128, 8, 128], BF16, "w1") for _ in range(2)]
                    w2 = [mk(st2, "sb", [128, 8, 128], BF16, "w2") for _ in range(2)]
                    stg = [mk(st2, "sb", [128, S], F32, "stg") for _ in range(2)]
                    pps = [mk(st2, "ps", [128, 512], F32, "pps") for _ in range(4)]
                    for cc in range(27):
                        i = cc % 2
                        tg_ = "pj%d_" % i
                        rw = cc < 15
                        if rw:
                            P.dma("sp", (lambda e, i=i, cc=cc: e.dma_start(out=wf[i][:], in_=I["win_l"][cc])), writes=[tg_ + "wf"])
                            P.op("pool", (lambda e, i=i, cc=cc: e.tensor_tensor(
                                out=w2[i][:], in0=wf[i][:], in1=mub[:, cc * 128:(cc + 1) * 128].unsqueeze(1).to_broadcast([128, 8, 128]),
                                op=ALU.mult)), reads=[tg_ + "wf", "mub"], writes=[tg_ + "w2"])
                            P.op("dve", (lambda e, i=i: e.tensor_tensor(out=w1[i][:], in0=wf[i][:], in1=w2[i][:], op=ALU.subtract)),
                                 reads=[tg_ + "wf", tg_ + "w2"], writes=[tg_ + "w1"])
                        else:
                            P.dma("pool", (lambda e, i=i, cc=cc: e.dma_start(out=w1[i][:], in_=I["win_l"][cc])), writes=[tg_ + "w1"])
                        for tg in range(4):
                            pi_ = (cc * 4 + tg) % 4
                            nmm = 16 if rw else 8
                            for k in range(8):
                                P.op("pe", (lambda e, i=i, k=k, tg=tg, pi_=pi_, nmm=nmm: e.matmul(
                                    pps[pi_][:], lhsT=w1[i][:, k, :], rhs=hT[:, k, 1 + tg * 512:1 + (tg + 1) * 512],
                                    start=(k == 0), stop=(k == 7 and nmm == 8))),
                                     reads=[tg_ + "w1", "hT_%d" % tg], writes=["pps%d" % pi_])
                            if rw:
                                rd = ["hT_%d" % tg, "hT0"] + (["hT_%d" % (tg - 1)] if tg > 0 else [])
                                for k in range(8):
                                    P.op("pe", (lambda e, i=i, k=k, tg=tg, pi_=pi_: e.matmul(
                                        pps[pi_][:], lhsT=w2[i][:, k, :], rhs=hT[:, k, tg * 512:(tg + 1) * 512],
                                        start=False, stop=(k == 7))),
                                         reads=[tg_ + "w2"] + rd, writes=["pps%d" % pi_])
                            P.op("act", (lambda e, i=i, tg=tg, pi_=pi_: e.copy(out=stg[i][:, tg * 512:(tg + 1) * 512], in_=pps[pi_][:])),
                                 reads=["pps%d" % pi_], writes=[tg_ + "stg"])
                        P.dma("act", (lambda e, i=i, cc=cc: e.dma_start(out=projF[b, cc * 128:(cc + 1) * 128, :], in_=stg[i][:])),
                              reads=[tg_ + "stg"], writes=[])
                        if debug and b == 0:
                            P.dma("sp", (lambda e, i=i, cc=cc: e.dma_start(out=dbg["d_proj"][cc * 128:(cc + 1) * 128, :], in_=stg[i][:])),
                                  reads=[tg_ + "stg"])
                    P.barrier()
        if t2:
            st2_rwkv(0)
        if t3only:
            st3_moba(0)
        for b in range(NB if not (t2 or t3only) else 0):
            st1(b)
            if stages <= 1:
                continue
            st2_rwkv(b)
            if stages <= 2:
                continue
            st3_moba(b)
            if stages <= 3:
                continue
            if SPARSE:
                st4s(b)
            else:
                st456(b)
        if SPARSE and not (t2 or t3only) and stages > 4:
            moe_sparse()
            final_sparse()
        P.barrier()
        global LAST_NOPS
        LAST_NOPS = P.nops
        P.emit()
    return nc


def host_layout(inputs):
    f = lambda k: np.ascontiguousarray(np.asarray(inputs[k], dtype=np.float32)[0])
    x = np.asarray(inputs["x"], dtype=np.float32)
    c = np.asarray(inputs["c"], dtype=np.float32)
    w_in = f("w_in")
    win_p = np.zeros((D, 27 * 128), np.float32)
    win_p[:, 0:1824] = w_in[:, 0:1824]
    win_p[:, 1920:1920 + 1536] = w_in[:, 1824:3360]
    win_l = np.ascontiguousarray(win_p.reshape(8, 128, 27, 128).transpose(2, 1, 0, 3))
    mu_p = np.zeros((1920,), np.float32)
    mu_p[0:1824] = f("rwkv_mu")
    names = ["rwkv_w0", "rwkv_a0", "rwkv_k_k", "rwkv_k_a", "rwkv_r_k", "rwkv_ln_g", "rwkv_ln_b"]
    pvec = np.stack([f(n).reshape(4, 128) for n in names], axis=-1)
    pvec = np.ascontiguousarray(pvec.transpose(1, 0, 2))
    qkg = np.stack([np.tile(f("q_norm_g"), 2), np.tile(f("k_norm_g"), 2)], axis=-1)
    wgu = f("w_gate_up")
    wg = wgu[:, :, :1024].reshape(NEXP, 8, 128, 8, 128)
    wu = wgu[:, :, 1024:].reshape(NEXP, 8, 128, 8, 128)
    wgu_l = np.ascontiguousarray(np.concatenate([wg, wu], axis=-1).transpose(0, 3, 2, 1, 4))
    bgu = f("b_gate_up")
    bgu_l = np.ascontiguousarray(bgu.reshape(NEXP, 16, 128).transpose(2, 0, 1))
    shared = {
        "w_ada": f("w_ada"), "b_ada": f("b_ada"), "norm1_g": f("norm1_g"), "win_l": win_l, "mu_p": mu_p, "pvec": pvec,
        "w_up": f("rwkv_w_up"), "a_up": f("rwkv_a_up"), "g_up": f("rwkv_g_up"), "qkg": np.ascontiguousarray(qkg),
        "w_out": f("w_out"), "norm2_g": f("norm2_g"), "w_router": f("w_router"), "b_router": f("b_router"),
        "wgu_l": wgu_l, "bgu_l": bgu_l, "w_down": f("w_down"), "b_down": f("b_down"),
    }
    for k, v in host_consts().items():
        shared["c_" + k] = v
    in_maps = []
    for i in range(NCORES):
        m = dict(shared)
        m["x"] = np.ascontiguousarray(x[i * NB:(i + 1) * NB])
        cc = c[i * NB:(i + 1) * NB]
        m["cT"] = np.ascontiguousarray(cc.reshape(NB, 8, 128).transpose(0, 2, 1))
        in_maps.append(m)
    return in_maps


def kernel(**inputs):
    in_maps = host_layout(inputs)
    nc = build_program()
    res = run_bass_kernel_spmd(nc, in_maps, core_ids=list(range(NCORES)))
    return np.concatenate([r["out"] for r in res.results], axis=0)
```
